# Optimizing a Trainium2 kernel written in Bass

```python
import jax, jax.numpy as jnp
from jax import lax
import numpy as np

D_MODEL = 2048
BATCH = 4
SEQ = 4096
DEPTH = 2

GRID_W = 64
CTX_LEN = 256
EPS = 1e-6

MLA_HEADS = 8
MLA_Q_RANK = 512
MLA_KV_RANK = 512
MLA_NOPE = 128
MLA_ROPE = 64
MLA_V = 128
MLA_QK = MLA_NOPE + MLA_ROPE
MLA_SCALE = MLA_QK ** -0.5
ROPE_THETA = 10000.0
ATTN_BLOCK = 128

SGU_WIDTH = 512
SGU_GROUPS = 4
SGU_GROUP_DIM = SGU_WIDTH // SGU_GROUPS
SGU_CHUNK = 128

GLA_HEADS = 4
GLA_DK = 64
GLA_DV = 128
GLA_GATE_RANK = 16
GLA_GATE_NORM = 16.0
GLA_CHUNK = 64

MIX_WIDTH = MLA_HEADS * MLA_V + SGU_WIDTH + GLA_HEADS * GLA_DV

IN_SPLITS = (MLA_Q_RANK, MLA_KV_RANK, MLA_ROPE,
             SGU_WIDTH, SGU_WIDTH,
             GLA_HEADS * GLA_DK, GLA_HEADS * GLA_DK,
             GLA_HEADS * GLA_DV, GLA_HEADS * GLA_DV,
             GLA_GATE_RANK, GLA_GATE_RANK)
D_IN = sum(IN_SPLITS)

MOE_GROUPS = 4
MOE_PER_GROUP = 4
MOE_EXPERTS = MOE_GROUPS * MOE_PER_GROUP
MOE_TOPK = 2
D_EXPERT = 1024

kernel_name = 'hybrid_mla_sgu_gla_hmoe_dit'


def rmsnorm(x, gain):
    xf = x.astype(jnp.float32)
    y = xf * lax.rsqrt(jnp.mean(xf * xf, axis=-1, keepdims=True) + EPS)
    return (y * gain.astype(jnp.float32)).astype(x.dtype)


def modulate(h, shift, scale):
    return h * (1 + scale) + shift


def split_in(z):
    return jnp.split(z, np.cumsum(IN_SPLITS)[:-1].tolist(), axis=-1)


def axial_rope_tables(n_tokens):
    rows = n_tokens // GRID_W
    row = jnp.repeat(jnp.arange(rows, dtype=jnp.int32), GRID_W)
    col = jnp.tile(jnp.arange(GRID_W, dtype=jnp.int32), rows)
    n_freq = MLA_ROPE // 4
    inv_freq = ROPE_THETA ** (-jnp.arange(n_freq, dtype=jnp.float32) / n_freq)
    ang = jnp.concatenate([row[:, None].astype(jnp.float32) * inv_freq,
                           col[:, None].astype(jnp.float32) * inv_freq], axis=-1)
    return jnp.cos(ang), jnp.sin(ang)


def apply_axial_rope(t, cos, sin):
    n_freq = MLA_ROPE // 4

    def rot(u, c, s):
        u1, u2 = jnp.split(u, 2, axis=-1)
        return jnp.concatenate([u1 * c - u2 * s, u1 * s + u2 * c], axis=-1)

    t_row, t_col = jnp.split(t, 2, axis=-1)
    return jnp.concatenate([rot(t_row, cos[..., :n_freq], sin[..., :n_freq]),
                            rot(t_col, cos[..., n_freq:], sin[..., n_freq:])], axis=-1)


def mla_qkv(c_q, c_kv, k_pe_shared, q_norm, w_uq, kv_norm, w_ukv, q_gain, k_gain, rope):
    B, N, _ = c_q.shape
    q = (rmsnorm(c_q, q_norm) @ w_uq).reshape(B, N, MLA_HEADS, MLA_QK)
    kv = (rmsnorm(c_kv, kv_norm) @ w_ukv).reshape(B, N, MLA_HEADS, MLA_NOPE + MLA_V)
    k_nope, v = jnp.split(kv, [MLA_NOPE], axis=-1)
    k_pe = jnp.broadcast_to(k_pe_shared[:, :, None, :], (B, N, MLA_HEADS, MLA_ROPE))
    k = jnp.concatenate([k_nope, k_pe], axis=-1)
    q = rmsnorm(q, q_gain)
    k = rmsnorm(k, k_gain)
    if rope is not None:
        cos, sin = rope
        q = jnp.concatenate([q[..., :MLA_NOPE], apply_axial_rope(q[..., MLA_NOPE:], cos, sin)], axis=-1)
        k = jnp.concatenate([k[..., :MLA_NOPE], apply_axial_rope(k[..., MLA_NOPE:], cos, sin)], axis=-1)
    return q, k, v


def latent_attention(q, k, v, k_ctx, v_ctx):
    B, S, H, _ = q.shape
    keys = jnp.concatenate([k_ctx, k], axis=1)
    vals = jnp.concatenate([v_ctx, v], axis=1)
    n_blk = S // ATTN_BLOCK
    q_blocks = q.reshape(B, n_blk, ATTN_BLOCK, H, MLA_QK).transpose(1, 0, 2, 3, 4)

    def one_block(qb):
        s = jnp.einsum('bqhd,bkhd->bhqk', qb, keys).astype(jnp.float32) * MLA_SCALE
        p = jax.nn.softmax(s, axis=-1).astype(vals.dtype)
        return jnp.einsum('bhqk,bkhd->bqhd', p, vals)

    o = lax.map(one_block, q_blocks)
    return o.transpose(1, 0, 2, 3, 4).reshape(B, S, H * MLA_V)


def context_attention(q, k, v):
    B, L, H, _ = q.shape
    s = jnp.einsum('bqhd,bkhd->bhqk', q, k).astype(jnp.float32) * MLA_SCALE
    p = jax.nn.softmax(s, axis=-1).astype(v.dtype)
    return jnp.einsum('bhqk,bkhd->bqhd', p, v).reshape(B, L, H * MLA_V)


def spatial_gating(zu, zv, v_gain, w_s, b_s):
    B, N, _ = zu.shape
    shape = (B, N // SGU_CHUNK, SGU_CHUNK, SGU_GROUPS, SGU_GROUP_DIM)
    u = jax.nn.gelu(zu).reshape(shape)
    v = rmsnorm(jax.nn.gelu(zv).reshape(shape), v_gain)
    v = jnp.einsum('gts,bcsgd->bctgd', w_s, v) + b_s.T[:, :, None]
    return (u * v).reshape(B, N, SGU_WIDTH)


def gla_inputs(zq, zk, zv, zg_f, zg_b, wg_f, bg_f, wg_b, bg_b):
    B, N, _ = zq.shape
    q = zq.reshape(B, N, GLA_HEADS, GLA_DK) * GLA_DK ** -0.5
    k = zk.reshape(B, N, GLA_HEADS, GLA_DK)
    v = zv.reshape(B, N, GLA_HEADS, GLA_DV)
    g_f = (jax.nn.log_sigmoid((zg_f @ wg_f + bg_f).astype(jnp.float32)) / GLA_GATE_NORM).reshape(B, N, GLA_HEADS, GLA_DK)
    g_b = (jax.nn.log_sigmoid((zg_b @ wg_b + bg_b).astype(jnp.float32)) / GLA_GATE_NORM).reshape(B, N, GLA_HEADS, GLA_DK)
    return q, k, v, g_f, g_b


def gla_scan(q, k, v, g, s0):
    B, N, H, _ = q.shape
    n_chunk = N // GLA_CHUNK

    def to_chunks(t):
        return t.reshape(B, n_chunk, GLA_CHUNK, H, t.shape[-1]).transpose(1, 0, 3, 2, 4)

    incl = jnp.tril(jnp.ones((GLA_CHUNK, GLA_CHUNK), dtype=bool))[:, :, None]

    def step(S, inp):
        qi, ki, vi, gi = inp
        qf, kf, vf = qi.astype(jnp.float32), ki.astype(jnp.float32), vi.astype(jnp.float32)
        b = jnp.cumsum(gi, axis=2)
        o_inter = jnp.einsum('bhtd,bhde->bhte', qf * jnp.exp(b), S)
        diff = b[:, :, :, None, :] - b[:, :, None, :, :]
        decay = jnp.exp(jnp.where(incl, diff, -jnp.inf))
        a = jnp.einsum('bhtd,bhsd,bhtsd->bhts', qf, kf, decay)
        o = o_inter + jnp.einsum('bhts,bhse->bhte', a, vf)
        b_last = b[:, :, -1:, :]
        S = jnp.exp(b_last[:, :, 0, :])[..., None] * S + jnp.einsum('bhsd,bhse->bhde', kf * jnp.exp(b_last - b), vf)
        return S, o

    S, o = lax.scan(step, s0, (to_chunks(q), to_chunks(k), to_chunks(v), to_chunks(g)))
    o = o.transpose(1, 0, 3, 2, 4).reshape(B, N, H, GLA_DV).astype(v.dtype)
    return o, S


def gla_bidirectional(q, k, v, g_f, g_b, s0_f, s0_b):
    flip = lambda t: jnp.flip(t, axis=1)
    o_f, s_f = gla_scan(q, k, v, g_f, s0_f)
    o_b, s_b = gla_scan(flip(q), flip(k), flip(v), flip(g_b), s0_b)
    return o_f + flip(o_b), s_f, s_b


def gla_final_state(k, v, g):
    G = jnp.cumsum(g, axis=1)
    w = jnp.exp(G[:, -1:] - G)
    return jnp.einsum('bnhd,bnhe->bhde', k.astype(jnp.float32) * w, v.astype(jnp.float32))


def gla_output(o, r, gain):
    B, N = o.shape[:2]
    return rmsnorm(o, gain).reshape(B, N, GLA_HEADS * GLA_DV) * jax.nn.silu(r)


def hier_moe(h, w_group, b_group, w_expert, b_expert, w1, w3, w2):
    g_prob = jax.nn.softmax((h @ w_group + b_group).astype(jnp.float32), axis=-1)
    g_w, g_sel = lax.top_k(g_prob, 1)
    e_logits = (h @ w_expert + b_expert).astype(jnp.float32)
    e_logits = e_logits.reshape(e_logits.shape[:-1] + (MOE_GROUPS, MOE_PER_GROUP))
    e_in = jnp.take_along_axis(e_logits, g_sel[..., None], axis=-2)[..., 0, :]
    e_w, e_sel = lax.top_k(jax.nn.softmax(e_in, axis=-1), MOE_TOPK)
    e_w = e_w / jnp.sum(e_w, axis=-1, keepdims=True)
    expert_id = g_sel * MOE_PER_GROUP + e_sel
    combine = jnp.sum(jax.nn.one_hot(expert_id, MOE_EXPERTS, dtype=jnp.float32)
                      * (g_w * e_w)[..., None], axis=-2).astype(h.dtype)
    out = jnp.zeros_like(h)
    for e in range(MOE_EXPERTS):
        act = jax.nn.silu(h @ w1[e]) * (h @ w3[e])
        out = out + combine[..., e:e + 1] * (act @ w2[e])
    return out


def setup_inputs(seed: int = 0) -> dict:
    key = jax.random.key(seed)
    keys = jax.random.split(key, 40)
    counter = [0]

    def nrm(shape, scale=1.0):
        k = keys[counter[0]]
        counter[0] += 1
        return jax.random.normal(k, shape, jnp.float32) * scale

    def gain(shape):
        return 1.0 + nrm(shape, 0.02)

    D = D_MODEL
    return {
        'x': nrm((BATCH, SEQ, D)),
        'c': nrm((BATCH, D)),
        'ctx': nrm((BATCH, CTX_LEN, D)),
        'c_ctx': nrm((D,)),
        'norm1_g': gain((DEPTH, D)),
        'norm2_g': gain((DEPTH, D)),
        'ada_w': nrm((DEPTH, D, 6 * D), 0.5 * D ** -0.5),
        'ada_b': nrm((DEPTH, 6 * D), 0.02),
        'w_in': nrm((DEPTH, D, D_IN), D ** -0.5),
        'mla_q_norm': gain((DEPTH, MLA_Q_RANK)),
        'mla_w_uq': nrm((DEPTH, MLA_Q_RANK, MLA_HEADS * MLA_QK), MLA_Q_RANK ** -0.5),
        'mla_kv_norm': gain((DEPTH, MLA_KV_RANK)),
        'mla_w_ukv': nrm((DEPTH, MLA_KV_RANK, MLA_HEADS * (MLA_NOPE + MLA_V)), MLA_KV_RANK ** -0.5),
        'mla_q_gain': gain((DEPTH, MLA_QK)),
        'mla_k_gain': gain((DEPTH, MLA_QK)),
        'sgu_norm': gain((DEPTH, SGU_GROUPS, SGU_GROUP_DIM)),
        'sgu_w': nrm((DEPTH, SGU_GROUPS, SGU_CHUNK, SGU_CHUNK), SGU_CHUNK ** -0.5),
        'sgu_b': gain((DEPTH, SGU_GROUPS, SGU_CHUNK)),
        'gla_wg_f': nrm((DEPTH, GLA_GATE_RANK, GLA_HEADS * GLA_DK), GLA_GATE_RANK ** -0.5),
        'gla_bg_f': nrm((DEPTH, GLA_HEADS * GLA_DK), 0.1),
        'gla_wg_b': nrm((DEPTH, GLA_GATE_RANK, GLA_HEADS * GLA_DK), GLA_GATE_RANK ** -0.5),
        'gla_bg_b': nrm((DEPTH, GLA_HEADS * GLA_DK), 0.1),
        'gla_out_norm': gain((DEPTH, GLA_DV)),
        'w_out': nrm((DEPTH, MIX_WIDTH, D), MIX_WIDTH ** -0.5),
        'moe_w_group': nrm((DEPTH, D, MOE_GROUPS), D ** -0.5),
        'moe_b_group': nrm((DEPTH, MOE_GROUPS), 0.01),
        'moe_w_expert': nrm((DEPTH, D, MOE_EXPERTS), D ** -0.5),
        'moe_b_expert': nrm((DEPTH, MOE_EXPERTS), 0.01),
        'moe_w1': nrm((DEPTH, MOE_EXPERTS, D, D_EXPERT), D ** -0.5),
        'moe_w3': nrm((DEPTH, MOE_EXPERTS, D, D_EXPERT), D ** -0.5),
        'moe_w2': nrm((DEPTH, MOE_EXPERTS, D_EXPERT, D), D_EXPERT ** -0.5),
    }


def reference(x, c, ctx, c_ctx, norm1_g, norm2_g, ada_w, ada_b, w_in,
              mla_q_norm, mla_w_uq, mla_kv_norm, mla_w_ukv, mla_q_gain, mla_k_gain,
              sgu_norm, sgu_w, sgu_b,
              gla_wg_f, gla_bg_f, gla_wg_b, gla_bg_b, gla_out_norm,
              w_out, moe_w_group, moe_b_group, moe_w_expert, moe_b_expert, moe_w1, moe_w3, moe_w2):
    B, S, _ = x.shape
    cos, sin = axial_rope_tables(S)
    rope = (cos[:, None, :].astype(x.dtype), sin[:, None, :].astype(x.dtype))
    zero_state = jnp.zeros((B, GLA_HEADS, GLA_DK, GLA_DV), jnp.float32)

    for l in range(DEPTH):
        ctx_out = l < DEPTH - 1
        mod_x = jnp.split((jax.nn.silu(c) @ ada_w[l] + ada_b[l])[:, None, :], 6, axis=-1)
        mod_c = jnp.split((jax.nn.silu(c_ctx) @ ada_w[l] + ada_b[l])[None, None, :], 6, axis=-1)

        h = modulate(rmsnorm(x, norm1_g[l]), mod_x[0], mod_x[1])
        hc = modulate(rmsnorm(ctx, norm1_g[l]), mod_c[0], mod_c[1])
        (cq_x, ckv_x, kpe_x, zu_x, zv_x, gq_x, gk_x, gv_x, gr_x, ggf_x, ggb_x) = split_in(h @ w_in[l])
        (cq_c, ckv_c, kpe_c, zu_c, zv_c, gq_c, gk_c, gv_c, gr_c, ggf_c, ggb_c) = split_in(hc @ w_in[l])

        q_x, k_x, v_x = mla_qkv(cq_x, ckv_x, kpe_x, mla_q_norm[l], mla_w_uq[l], mla_kv_norm[l],
                                mla_w_ukv[l], mla_q_gain[l], mla_k_gain[l], rope)
        q_c, k_c, v_c = mla_qkv(cq_c, ckv_c, kpe_c, mla_q_norm[l], mla_w_uq[l], mla_kv_norm[l],
                                mla_w_ukv[l], mla_q_gain[l], mla_k_gain[l], None)
        a_x = latent_attention(q_x, k_x, v_x, k_c, v_c)

        b_x = spatial_gating(zu_x, zv_x, sgu_norm[l], sgu_w[l], sgu_b[l])

        q_gc, k_gc, v_gc, gf_c, gb_c = gla_inputs(gq_c, gk_c, gv_c, ggf_c, ggb_c,
                                                  gla_wg_f[l], gla_bg_f[l], gla_wg_b[l], gla_bg_b[l])
        if ctx_out:
            o_gc, s_f, s_b = gla_bidirectional(q_gc, k_gc, v_gc, gf_c, gb_c, zero_state, zero_state)
        else:
            s_f = gla_final_state(k_gc, v_gc, gf_c)
            s_b = gla_final_state(jnp.flip(k_gc, axis=1), jnp.flip(v_gc, axis=1), jnp.flip(gb_c, axis=1))
        q_gx, k_gx, v_gx, gf_x, gb_x = gla_inputs(gq_x, gk_x, gv_x, ggf_x, ggb_x,
                                                  gla_wg_f[l], gla_bg_f[l], gla_wg_b[l], gla_bg_b[l])
        o_gx, _, _ = gla_bidirectional(q_gx, k_gx, v_gx, gf_x, gb_x, s_f, s_b)
        c_x = gla_output(o_gx, gr_x, gla_out_norm[l])

        mix_x = jnp.concatenate([a_x, b_x, c_x], axis=-1)
        x = x + mod_x[2] * (mix_x @ w_out[l])

        h2 = modulate(rmsnorm(x, norm2_g[l]), mod_x[3], mod_x[4])
        x = x + mod_x[5] * hier_moe(h2, moe_w_group[l], moe_b_group[l], moe_w_expert[l], moe_b_expert[l],
                                    moe_w1[l], moe_w3[l], moe_w2[l])

        if ctx_out:
            a_c = context_attention(q_c, k_c, v_c)
            b_c = spatial_gating(zu_c, zv_c, sgu_norm[l], sgu_w[l], sgu_b[l])
            c_c = gla_output(o_gc, gr_c, gla_out_norm[l])
            mix_c = jnp.concatenate([a_c, b_c, c_c], axis=-1)
            ctx = ctx + mod_c[2] * (mix_c @ w_out[l])
            h2c = modulate(rmsnorm(ctx, norm2_g[l]), mod_c[3], mod_c[4])
            ctx = ctx + mod_c[5] * hier_moe(h2c, moe_w_group[l], moe_b_group[l], moe_w_expert[l], moe_b_expert[l],
                                            moe_w1[l], moe_w3[l], moe_w2[l])
    return x
```

```python
from contextlib import ExitStack
import numpy as np
import concourse.bass as bass
import concourse.mybir as mybir
from concourse.bass_utils import run_bass_kernel_spmd

F32, BF16 = mybir.dt.float32, mybir.dt.bfloat16
AF = mybir.ActivationFunctionType
ALU = mybir.AluOpType
AX = mybir.AxisListType

D = 2048
SEQ = 4096
CTX = 256
NXT = SEQ // 128
NCT = CTX // 128
NT = NXT + NCT
NTOK = NT * 128
DEPTH = 2
EPS = 1e-6
MLA_SCALE = 192 ** -0.5
NE = 16
DE = 1024
NCORES = 4


class Sem:
    def __init__(self, h, k):
        self.h, self.k, self.n = h, k, 0


class Res:
    __slots__ = ("w", "r")

    def __init__(self):
        self.w = {}
        self.r = {}


def _merge(dst, src):
    for k, (s, v) in src.items():
        if k not in dst or dst[k][1] < v:
            dst[k] = (s, v)


class Buf:
    def __init__(self, t):
        self.t = t
        self.res = Res()

    def __getitem__(self, key):
        return self.t[key]


class Prog:
    ENG = ("pe", "act", "dve", "pool", "sp")

    def __init__(self, nc):
        self.nc = nc
        self.es = ExitStack()
        self.nsem = 0
        self.streams = {e: [] for e in self.ENG}
        self.esem = {e: self.newsem() for e in self.ENG}
        self.waited = {e: {} for e in self.ENG}
        self.dsems = []
        self.ninstr = 0

    def newsem(self):
        h = self.es.enter_context(self.nc.semaphore(f"sm{self.nsem}"))
        s = Sem(h, self.nsem)
        self.nsem += 1
        return s

    def slot(self):
        s = self.newsem()
        self.dsems.append(s)
        return s

    def gs(self, j):
        if not hasattr(self, "_gs"):
            self._gs = {}
        if j not in self._gs:
            self._gs[j] = self.slot()
        return self._gs[j]

    def one(self):
        if not hasattr(self, "_ones"):
            self._ones = [self.slot() for _ in range(20)]
            self._onei = 0
        s = self._ones[self._onei % 20]
        self._onei += 1
        return s

    def _emit(self, eng, fn, sem, inc, reads, writes, awrites):
        deps = {}
        for r in reads:
            _merge(deps, r.w)
        for w in writes:
            _merge(deps, w.w)
            _merge(deps, w.r)
        for w in awrites:
            _merge(deps, w.r)
        st = self.streams[eng]
        wd = self.waited[eng]
        for k, (s, v) in deps.items():
            if eng == "pe" and s is self.esem["pe"]:
                continue
            if wd.get(k, 0) >= v:
                continue
            wd[k] = v
            st.append(("w", s, v))
        sem.n += inc
        tok = {sem.k: (sem, sem.n)}
        st.append(("i", fn, sem, inc))
        self.ninstr += 1
        for r in reads:
            _merge(r.r, tok)
        for w in writes:
            w.w = dict(tok)
            w.r = {}
        for w in awrites:
            _merge(w.w, tok)
        return tok

    def op(self, eng, fn, reads=(), writes=(), awrites=()):
        if self.esem[eng].n > 12000:
            self.dsems.append(self.esem[eng])
            self.esem[eng] = self.newsem()
        return self._emit(eng, fn, self.esem[eng], 1, reads, writes, awrites)

    def dma(self, eng, slot, out, in_, reads=(), writes=(), awrites=(), **kw):
        return self._emit(eng, lambda E: E.dma_start(out=out, in_=in_, **kw), slot, 16, reads, writes, awrites)

    def barrier(self):
        toks = {}
        for e in self.ENG:
            s = self.esem[e]
            if s.n:
                toks[s.k] = (s, s.n)
        for s in self.dsems:
            if s.n:
                toks[s.k] = (s, s.n)
        for e in self.ENG:
            wd = self.waited[e]
            for k, (s, v) in toks.items():
                if wd.get(k, 0) >= v:
                    continue
                wd[k] = v
                self.streams[e].append(("w", s, v))

    def flush(self):
        nc = self.nc
        streams = self.streams
        self.streams = {e: [] for e in self.ENG}

        def replay(E, st):
            for it in st:
                if it[0] == "w":
                    E.wait_ge(it[1].h, it[2])
                else:
                    it[1](E).then_inc(it[2].h, it[3])

        with nc.Block() as block:
            @block.tensor
            def _(E):
                replay(E, streams["pe"])

            @block.scalar
            def _(E):
                replay(E, streams["act"])

            @block.vector
            def _(E):
                replay(E, streams["dve"])

            @block.gpsimd
            def _(E):
                replay(E, streams["pool"])

            @block.sync
            def _(E):
                replay(E, streams["sp"])


class Pool8:
    def __init__(self, banks):
        self.b = banks
        self.i = 0

    def next(self):
        b = self.b[self.i % len(self.b)]
        self.i += 1
        return b


_UID = [0]


def sb(ps, nc, name, shape, dt):
    _UID[0] += 1
    return Buf(ps.enter_context(nc.sbuf_tensor(f"s{_UID[0]}_{name}", shape, dt)))


def sbn(ps, nc, name, shape, dt, n):
    return [sb(ps, nc, f"{name}{i}", shape, dt) for i in range(n)]


class Ctx:
    pass


def rstd_ops(P, st, ssres_cols, out_cols, n, scale):
    a, b = ssres_cols, out_cols
    P.op("act", lambda E: E.activation(out=st.t[:, b:b + n], in_=st.t[:, a:a + n], func=AF.Ln, scale=scale, bias=EPS),
         reads=[st.res], writes=[st.res])
    P.op("act", lambda E: E.activation(out=st.t[:, b:b + n], in_=st.t[:, b:b + n], func=AF.Exp, scale=-0.5),
         reads=[st.res], writes=[st.res])


def build(nlayers=DEPTH, dbg=(), lite=False, upto=99):
    nc = bass.Bass("TRN2", target_bir_lowering=False)
    P = Prog(nc)
    cx = Ctx()
    L = DEPTH

    def din(name, shape, dt=F32):
        return nc.dram_tensor(name, list(shape), dt, kind="ExternalInput").ap()

    def dscr(name, shape, dt):
        kind = "ExternalOutput" if name in dbg else "Internal"
        return nc.dram_tensor(name, list(shape), dt, kind=kind).ap()

    x_in = din("x", [SEQ, D])
    c_in = din("ctx", [CTX, D])
    cT = din("cT", [128, 16, 2])
    ada_w = din("ada_w", [L, D, 6 * D])
    ada_b = din("ada_b", [L, 6 * D])
    g1 = din("g1", [L, D])
    g2 = din("g2", [L, D])
    wk_d = din("wk", [L, D, 1536])
    wq_d = din("wq", [L, D, 2304])
    wukv_d = din("wukv", [L, 512, 2048])
    wuq_d = din("wuq", [L, 512, 2048])
    qnorm = din("qnorm", [L, 512])
    kvnorm = din("kvnorm", [L, 512])
    qgn = din("qgn", [L, 128, 1])
    kgn = din("kgn", [L, 128, 1])
    qgpe = din("qgpe", [L, 1024])
    kgpe = din("kgpe", [L, 128])
    sgun = din("sgun", [L, 512])
    wsT_d = din("wsT", [L, 128, 4, 128])
    sgub = din("sgub", [L, 512])
    wgF_d = din("wgF", [L, 17, 256])
    wgB_d = din("wgB", [L, 17, 256])
    glaon = din("glaon", [L, 512])
    wout_d = din("w_out", [L, D, D])
    wr_d = din("wr", [L, D, 20])
    br_d = din("br", [L, 20])
    nE = 1 if lite else NE
    w1_d = din("w1", [L, nE, D, DE])
    w3_d = din("w3", [L, nE, D, DE])
    w2_d = din("w2", [L, nE, DE, D])
    tabk = din("tabk", [NT, 128, 128])
    tabq = din("tabq", [NT, 128, 1024])
    ident_d = din("ident", [128, 128])
    gcon = din("gcon", [128, 1412])
    y_out = nc.dram_tensor("y", [SEQ, D], F32, kind="ExternalOutput").ap()

    modbuf = dscr("modbuf", [L, 2, 6 * D], F32)
    xbuf = dscr("xbuf", [SEQ, D], F32)
    cbuf = dscr("cbuf", [CTX, D], F32)
    x1buf = dscr("x1buf", [NTOK, D], F32)
    hTbuf = dscr("hTbuf", [NT, 128, 16, 128], BF16)
    h2Tbuf = dscr("h2Tbuf", [NT, 128, 16, 128], BF16)
    Vb = dscr("Vb", [NTOK, 1024], BF16)
    KnT = dscr("KnT", [NT, 128, 8, 128], BF16)
    KpeT = dscr("KpeT", [128, NTOK], BF16)
    rkb = dscr("rkb", [NTOK, 8], F32)
    QnT = dscr("QnT", [NT, 128, 8, 128], BF16)
    QpeT = dscr("QpeT", [NT, 128, 4, 128], BF16)
    glaKV = dscr("glaKV", [NTOK, 768], BF16)
    glaG = dscr("glaG", [NTOK, 512], F32)
    glaQ = dscr("glaQ", [NTOK, 256], BF16)
    glaR = dscr("glaR", [NT, 128, 4, 128], BF16)
    glaO = dscr("glaO", [NT, 128, 4, 128], F32)
    mixT = dscr("mixT", [NT, 128, 16, 128], BF16)
    combb = dscr("combb", [NTOK, 16], F32)
    R = {n: Res() for n in ("modbuf", "xbuf", "cbuf", "x1buf", "hTbuf", "h2Tbuf", "Vb", "KnT", "KpeT", "rkb", "QnT", "QpeT",
                            "glaKV", "glaG", "glaQ", "glaR", "glaO", "mixT", "combb", "y")}

    gs = ExitStack()
    banks = []
    for i in range(8):
        banks.append(Buf(gs.enter_context(nc.psum_tensor(f"bank{i}", [128, 512], F32))))
    ident = sb(gs, nc, "ident", [128, 128], BF16)
    ones_bf = sb(gs, nc, "ones_bf", [128, 128], BF16)
    P.dma("pool", P.one(), ident.t[:], ident_d, writes=[ident.res])
    P.op("pool", lambda E: E.memset(ones_bf.t[:], 1.0), writes=[ones_bf.res])

    def bf(bank, n=1024):
        return bank.t[:].bitcast(BF16)[:, 0:n]

    def phase0(l):
        with ExitStack() as ps:
            sil = sb(ps, nc, "sil", [128, 16, 2], F32)
            mod = sb(ps, nc, "mod", [2, 6 * D], F32)
            adab = sb(ps, nc, "adab", [2, 6 * D], F32)
            gg = sb(ps, nc, "gg", [2, 2 * D], F32)
            blk = sbn(ps, nc, "adablk", [128, 16, 512], F32, 2)
            bslot = [P.gs(20), P.gs(21)]
            P.dma("sp", P.one(), sil.t[:], cT, writes=[sil.res])
            P.dma("sp", P.one(), adab.t[:], ada_b[l].partition_broadcast(2), writes=[adab.res])
            P.dma("sp", P.one(), gg.t[:, 0:D], g1[l].partition_broadcast(2), writes=[gg.res])
            P.dma("sp", P.one(), gg.t[:, D:2 * D], g2[l].partition_broadcast(2), writes=[gg.res])
            P.op("act", lambda E: E.activation(out=sil.t[:], in_=sil.t[:], func=AF.Silu), reads=[sil.res], writes=[sil.res])
            pp = Pool8(banks)
            src = ada_w[l].rearrange("(kc p) n -> p kc n", p=128)
            for j in range(24):
                b = blk[j % 2]
                P.dma("sp", bslot[j % 2], b.t[:], src[:, :, j * 512:(j + 1) * 512], writes=[b.res])
                bk = pp.next()

                def mm(E, b=b, bk=bk):
                    for kc in range(16):
                        ins = E.matmul(bk.t[0:2, :], lhsT=sil.t[:, kc, :], rhs=b.t[:, kc, :], start=(kc == 0), stop=(kc == 15))
                    return ins
                P.op("pe", mm, reads=[sil.res, b.res], writes=[bk.res])
                P.op("dve", lambda E, bk=bk, j=j: E.tensor_tensor(out=mod.t[:, j * 512:(j + 1) * 512], in0=bk.t[0:2, :],
                                                                  in1=adab.t[:, j * 512:(j + 1) * 512], op=ALU.add),
                     reads=[bk.res, adab.res], awrites=[mod.res])
            P.op("dve", lambda E: E.scalar_tensor_tensor(out=mod.t[:, D:2 * D], in0=mod.t[:, D:2 * D], scalar=1.0, in1=gg.t[:, 0:D],
                                                          op0=ALU.add, op1=ALU.mult), reads=[mod.res, gg.res], writes=[mod.res])
            P.op("dve", lambda E: E.scalar_tensor_tensor(out=mod.t[:, 4 * D:5 * D], in0=mod.t[:, 4 * D:5 * D], scalar=1.0, in1=gg.t[:, D:2 * D],
                                                          op0=ALU.add, op1=ALU.mult), reads=[mod.res, gg.res], writes=[mod.res])
            P.dma("sp", P.one(), modbuf[l], mod.t[:], reads=[mod.res], awrites=[R["modbuf"]])
            P.barrier()
            P.flush()

    def modrow(l, r, i):
        return modbuf[l, r, i * D:(i + 1) * D].partition_broadcast(128)

    def tile_src(l, t):
        xs = x_in if l == 0 else xbuf
        cs = c_in if l == 0 else cbuf
        if t < NXT:
            return xs[t * 128:(t + 1) * 128, :], (R["xbuf"] if l else None)
        return cs[(t - NXT) * 128:(t - NXT + 1) * 128, :], (R["cbuf"] if l else None)

    def phaseA1(l, tiles):
        with ExitStack() as ps:
            wk = sb(ps, nc, "wk", [128, 16, 1536], BF16)
            wukv = sb(ps, nc, "wukv", [128, 4, 2048], BF16)
            G1 = sbn(ps, nc, "G1_", [128, D], F32, 2)
            S1 = sbn(ps, nc, "S1_", [128, D], F32, 2)
            kvn = sb(ps, nc, "kvn", [128, 512], F32)
            kgn_s = sb(ps, nc, "kgn_s", [128, 1], F32)
            kgpe_s = sb(ps, nc, "kgpe_s", [128, 128], F32)
            wgF = sb(ps, nc, "wgF", [17, 256], BF16)
            wgB = sb(ps, nc, "wgB", [17, 256], BF16)
            xt = sbn(ps, nc, "xt", [128, D], F32, 3)
            tab = sbn(ps, nc, "tab", [128, 128], F32, 2)
            junk = sb(ps, nc, "junk", [128, D], BF16)
            tmp = sbn(ps, nc, "tmp", [128, D], F32, 2)
            hb = sbn(ps, nc, "hb", [128, D], BF16, 2)
            hT = sbn(ps, nc, "hT", [128, 16, 128], BF16, 2)
            st = sbn(ps, nc, "st", [128, 64], F32, 2)
            ckvn = sbn(ps, nc, "ckvn", [128, 512], BF16, 2)
            ckvnT = sbn(ps, nc, "ckvnT", [128, 4, 128], BF16, 2)
            Vt = sbn(ps, nc, "Vt", [128, 1024], BF16, 2)
            knb = sbn(ps, nc, "knb", [128, 1024], BF16, 2)
            KnTt = sbn(ps, nc, "KnTt", [128, 8, 128], BF16, 2)
            rkt = sbn(ps, nc, "rkt", [128, 8], F32, 2)
            rp = sbn(ps, nc, "rp", [128, 192], F32, 2)
            krb = sbn(ps, nc, "krb", [128, 128], BF16, 2)
            KpeTt = sbn(ps, nc, "KpeTt", [128, 128], BF16, 2)
            gkv = sbn(ps, nc, "gkv", [128, 768], BF16, 2)
            ggT = [sbn(ps, nc, "ggTF", [17, 128], BF16, 2), sbn(ps, nc, "ggTB", [17, 128], BF16, 2)]
            ge = sbn(ps, nc, "ge", [128, 512], F32, 2)
            gr_ = sbn(ps, nc, "grr", [128, 512], F32, 2)
            for kq in range(4):
                P.dma("pool", P.one(), wk.t[:, kq * 4:(kq + 1) * 4, :],
                      wk_d[l, kq * 512:(kq + 1) * 512, :].rearrange("(kc p) n -> p kc n", p=128), writes=[wk.res])
            P.dma("pool", P.one(), wukv.t[:], wukv_d[l].rearrange("(kc p) n -> p kc n", p=128), writes=[wukv.res])
            P.dma("pool", P.one(), wgF.t[:], wgF_d[l], writes=[wgF.res])
            P.dma("pool", P.one(), wgB.t[:], wgB_d[l], writes=[wgB.res])
            for r in range(2):
                P.dma("sp", P.one(), G1[r].t[:], modrow(l, r, 1), reads=[R["modbuf"]], writes=[G1[r].res])
                P.dma("sp", P.one(), S1[r].t[:], modrow(l, r, 0), reads=[R["modbuf"]], writes=[S1[r].res])
            P.dma("sp", P.one(), kvn.t[:], kvnorm[l].partition_broadcast(128), writes=[kvn.res])
            P.dma("sp", P.one(), kgn_s.t[:], kgn[l], writes=[kgn_s.res])
            P.dma("sp", P.one(), kgpe_s.t[:], kgpe[l].partition_broadcast(128), writes=[kgpe_s.res])
            for i in range(2):
                for d_ in range(2):
                    P.op("pool", lambda E, t_=ggT[d_][i]: E.memset(t_.t[:], 1.0), writes=[ggT[d_][i].res])
            xsl = [P.gs(20 + j) for j in range(3)]
            tsl = [P.gs(23 + j) for j in range(2)]
            pp = Pool8(banks[4:8])
            pz = Pool8(banks[0:4])
            for n, t in enumerate(tiles):
                i = n % 2
                r = 0 if t < NXT else 1
                X = xt[n % 3]
                src, sres = tile_src(l, t)
                P.dma("sp", xsl[n % 3], X.t[:], src, reads=([sres] if sres else []), writes=[X.res])
                P.dma("sp", tsl[i], tab[i].t[:], tabk[t], writes=[tab[i].res])
                S = st[i]
                P.op("act", lambda E, X=X, S=S: E.activation(out=junk.t[:], in_=X.t[:], func=AF.Square, accum_out=S.t[:, 0:1]),
                     reads=[X.res], writes=[junk.res, S.res])
                rstd_ops(P, S, 0, 1, 1, 1.0 / D)
                T_ = tmp[i]
                P.op("dve", lambda E, X=X, S=S, T_=T_, r=r: E.scalar_tensor_tensor(out=T_.t[:], in0=X.t[:], scalar=S.t[:, 1:2], in1=G1[r].t[:],
                                                                                    op0=ALU.mult, op1=ALU.mult),
                     reads=[X.res, S.res, G1[r].res], writes=[T_.res])
                H = hb[i]
                P.op("pool", lambda E, T_=T_, H=H, r=r: E.tensor_tensor(out=H.t[:], in0=T_.t[:], in1=S1[r].t[:], op=ALU.add),
                     reads=[T_.res, S1[r].res], writes=[H.res])
                HT = hT[i]
                for half in range(2):
                    bk = pp.next()

                    def tr(E, H=H, bk=bk, half=half):
                        for j in range(8):
                            kc = half * 8 + j
                            ins = E.transpose(out=bf(bk)[:, j * 128:(j + 1) * 128], in_=H.t[:, kc * 128:(kc + 1) * 128], identity=ident.t[:])
                        return ins
                    P.op("pe", tr, reads=[H.res, ident.res], writes=[bk.res])
                    eng = "act" if half == 0 else "dve"
                    if eng == "act":
                        P.op("act", lambda E, HT=HT, bk=bk, half=half: E.copy(out=HT.t[:, half * 8:(half + 1) * 8, :], in_=bf(bk)),
                             reads=[bk.res], awrites=[HT.res])
                    else:
                        P.op("dve", lambda E, HT=HT, bk=bk, half=half: E.tensor_copy(out=HT.t[:, half * 8:(half + 1) * 8, :], in_=bf(bk)),
                             reads=[bk.res], awrites=[HT.res])
                P.dma("sp", P.gs(0 + i), hTbuf[t], HT.t[:], reads=[HT.res], awrites=[R["hTbuf"]])
                if upto < 1:
                    continue

                def zmm(c0, c1, M=None):
                    bk = pz.next()

                    def f(E, bk=bk, HT=HT):
                        for kc in range(16):
                            ins = E.matmul(bk.t[:, 0:c1 - c0], lhsT=HT.t[:, kc, :], rhs=wk.t[:, kc, c0:c1], start=(kc == 0), stop=(kc == 15))
                        return ins
                    P.op("pe", f, reads=[HT.res, wk.res], writes=[bk.res])
                    return bk
                b_ckv = zmm(0, 512)
                b_mid = zmm(512, 896)
                b_gv = zmm(1024, 1536)
                b_g = pz.next()

                def gmm(E, bk=b_g, HT=HT):
                    for d_ in range(2):
                        for kc in range(16):
                            ins = E.matmul(bk.t[0:16, d_ * 128:(d_ + 1) * 128], lhsT=wk.t[:, kc, 896 + 16 * d_:912 + 16 * d_], rhs=HT.t[:, kc, :],
                                           start=(kc == 0), stop=(kc == 15))
                    return ins
                P.op("pe", gmm, reads=[HT.res, wk.res], writes=[b_g.res])
                if upto < 2:
                    continue
                P.op("act", lambda E, S=S, bk=b_ckv: E.activation(out=junk.t[:, 0:512], in_=bk.t[:], func=AF.Square, accum_out=S.t[:, 2:3]),
                     reads=[bk.res if False else b_ckv.res], writes=[junk.res, S.res])
                rstd_ops(P, S, 2, 3, 1, 1.0 / 512)
                CK = ckvn[i]
                P.op("dve", lambda E, S=S, bk=b_ckv, CK=CK: E.scalar_tensor_tensor(out=CK.t[:], in0=bk.t[:], scalar=S.t[:, 3:4], in1=kvn.t[:],
                                                                                   op0=ALU.mult, op1=ALU.mult),
                     reads=[b_ckv.res, S.res, kvn.res], writes=[CK.res])
                bk = pp.next()

                def tr2(E, CK=CK, bk=bk):
                    for j in range(4):
                        ins = E.transpose(out=bf(bk)[:, j * 128:(j + 1) * 128], in_=CK.t[:, j * 128:(j + 1) * 128], identity=ident.t[:])
                    return ins
                P.op("pe", tr2, reads=[CK.res, ident.res], writes=[bk.res])
                CT = ckvnT[i]
                P.op("act", lambda E, CT=CT, bk=bk: E.copy(out=CT.t[:], in_=bf(bk, 512)), reads=[bk.res], writes=[CT.res])
                if upto < 3:
                    continue
                kvb = []
                for q4 in range(4):
                    bk = pp.next()

                    def f(E, bk=bk, CT=CT, q4=q4):
                        for kc in range(4):
                            ins = E.matmul(bk.t[:], lhsT=CT.t[:, kc, :], rhs=wukv.t[:, kc, q4 * 512:(q4 + 1) * 512], start=(kc == 0), stop=(kc == 3))
                        return ins
                    P.op("pe", f, reads=[CT.res, wukv.res], writes=[bk.res])
                    kvb.append(bk)
                V = Vt[i]
                P.op("act", lambda E, V=V, bk=kvb[2]: E.copy(out=V.t[:, 0:512], in_=bk.t[:]), reads=[kvb[2].res], awrites=[V.res])
                P.op("dve", lambda E, V=V, bk=kvb[3]: E.tensor_copy(out=V.t[:, 512:1024], in_=bk.t[:]), reads=[kvb[3].res], awrites=[V.res])
                P.dma("sp", P.gs(2 + i), Vb[t * 128:(t + 1) * 128, :], V.t[:], reads=[V.res], awrites=[R["Vb"]])
                if upto < 4:
                    continue
                for h in range(8):
                    P.op("act", lambda E, S=S, bk=kvb[h // 4], h=h: E.activation(out=junk.t[:, 0:128], in_=bk.t[:, (h % 4) * 128:(h % 4 + 1) * 128],
                                                                                func=AF.Square, accum_out=S.t[:, 8 + h:9 + h]),
                         reads=[kvb[h // 4].res], writes=[junk.res], awrites=[S.res])
                P.op("act", lambda E, S=S, bk=b_mid: E.activation(out=junk.t[:, 0:64], in_=bk.t[:, 0:64], func=AF.Square, accum_out=S.t[:, 6:7]),
                     reads=[b_mid.res], writes=[junk.res, S.res])
                P.op("dve", lambda E, S=S: E.tensor_scalar(out=S.t[:, 8:16], in0=S.t[:, 8:16], scalar1=S.t[:, 6:7], scalar2=None, op0=ALU.add),
                     reads=[S.res], writes=[S.res])
                rstd_ops(P, S, 8, 16, 8, 1.0 / 192)
                RK = rkt[i]
                P.op("dve", lambda E, S=S, RK=RK: E.tensor_scalar(out=RK.t[:], in0=S.t[:, 16:24], scalar1=MLA_SCALE, scalar2=None, op0=ALU.mult),
                     reads=[S.res], writes=[RK.res])
                P.dma("sp", P.gs(4 + i), rkb[t * 128:(t + 1) * 128, :], RK.t[:], reads=[RK.res], awrites=[R["rkb"]])
                if upto < 5:
                    continue
                KB = knb[i]
                P.op("dve", lambda E, KB=KB, bk=kvb[0]: E.tensor_copy(out=KB.t[:, 0:512], in_=bk.t[:]), reads=[kvb[0].res], awrites=[KB.res])
                P.op("act", lambda E, KB=KB, bk=kvb[1]: E.copy(out=KB.t[:, 512:1024], in_=bk.t[:]), reads=[kvb[1].res], awrites=[KB.res])
                bk = pp.next()

                def tr3(E, KB=KB, bk=bk):
                    for h in range(8):
                        ins = E.transpose(out=bf(bk)[:, h * 128:(h + 1) * 128], in_=KB.t[:, h * 128:(h + 1) * 128], identity=ident.t[:])
                    return ins
                P.op("pe", tr3, reads=[KB.res, ident.res], writes=[bk.res])
                if upto < 5.1:
                    continue
                KT = KnTt[i]
                P.op("dve", lambda E, KT=KT, bk=bk: E.tensor_scalar(out=KT.t[:].rearrange("p h t -> p (h t)"), in0=bf(bk), scalar1=kgn_s.t[:, 0:1],
                                                                     scalar2=None, op0=ALU.mult),
                     reads=[bk.res, kgn_s.res], writes=[KT.res])
                if upto < 5.2:
                    continue
                P.dma("sp", P.gs(6 + i), KnT[t], KT.t[:], reads=[KT.res], awrites=[R["KnT"]])
                if upto < 6:
                    continue
                RP = rp[i]
                TB = tab[i]
                P.op("dve", lambda E, RP=RP, TB=TB, bk=b_mid: E.tensor_tensor(out=RP.t[:, 0:128], in0=bk.t[:, 0:128], in1=TB.t[:], op=ALU.mult),
                     reads=[b_mid.res, TB.res], writes=[RP.res])
                P.op("dve", lambda E, RP=RP: E.tensor_tensor(out=RP.t[:, 0:128], in0=RP.t[:, 0:128], in1=kgpe_s.t[:], op=ALU.mult),
                     reads=[RP.res, kgpe_s.res], writes=[RP.res])
                KR = krb[i]
                for hh in range(2):
                    P.op("dve", lambda E, RP=RP, KR=KR, hh=hh: E.tensor_tensor(out=KR.t[:, hh * 64:(hh + 1) * 64], in0=RP.t[:, 0:64], in1=RP.t[:, 64:128], op=ALU.add),
                         reads=[RP.res], awrites=[KR.res])
                bk = pp.next()
                P.op("pe", lambda E, KR=KR, bk=bk: E.transpose(out=bf(bk)[:, 0:128], in_=KR.t[:], identity=ident.t[:]),
                     reads=[KR.res, ident.res], writes=[bk.res])
                KP = KpeTt[i]
                P.op("act", lambda E, KP=KP, bk=bk: E.copy(out=KP.t[:], in_=bf(bk)[:, 0:128]), reads=[bk.res], writes=[KP.res])
                P.dma("sp", P.gs(8 + i), KpeT[:, t * 128:(t + 1) * 128], KP.t[:], reads=[KP.res], awrites=[R["KpeT"]])
                if upto < 7:
                    continue
                GK = gkv[i]
                P.op("dve", lambda E, GK=GK, bk=b_mid: E.tensor_copy(out=GK.t[:, 0:256], in_=bk.t[:, 128:384]), reads=[b_mid.res], awrites=[GK.res])
                P.op("act", lambda E, GK=GK, bk=b_gv: E.copy(out=GK.t[:, 256:768], in_=bk.t[:]), reads=[b_gv.res], awrites=[GK.res])
                P.dma("sp", P.gs(10 + i), glaKV[t * 128:(t + 1) * 128, :], GK.t[:], reads=[GK.res], awrites=[R["glaKV"]])
                if upto < 8:
                    continue
                for d_ in range(2):
                    GT = ggT[d_][i]
                    P.op("act", lambda E, GT=GT, d_=d_, bk=b_g: E.copy(out=GT.t[0:16, :], in_=bk.t[0:16, d_ * 128:(d_ + 1) * 128]),
                         reads=[b_g.res], awrites=[GT.res])
                bk = pp.next()

                def lg(E, bk=bk, i=i):
                    E.matmul(bk.t[:, 0:256], lhsT=ggT[0][i].t[:], rhs=wgF.t[:], start=True, stop=True)
                    return E.matmul(bk.t[:, 256:512], lhsT=ggT[1][i].t[:], rhs=wgB.t[:], start=True, stop=True)
                P.op("pe", lg, reads=[ggT[0][i].res, ggT[1][i].res, wgF.res, wgB.res], writes=[bk.res])
                GE = ge[i]
                P.op("act", lambda E, GE=GE, bk=bk: E.activation(out=GE.t[:], in_=bk.t[:], func=AF.Exp, scale=-1.0), reads=[bk.res], writes=[GE.res])
                P.op("dve", lambda E, GE=GE: E.tensor_scalar(out=GE.t[:], in0=GE.t[:], scalar1=1.0, scalar2=None, op0=ALU.add),
                     reads=[GE.res], writes=[GE.res])
                GR = gr_[i]
                P.op("act", lambda E, GE=GE, GR=GR: E.activation(out=GR.t[:], in_=GE.t[:], func=AF.Ln), reads=[GE.res], writes=[GR.res])
                P.dma("sp", P.gs(12 + i), glaG[t * 128:(t + 1) * 128, :], GR.t[:], reads=[GR.res], awrites=[R["glaG"]])
            P.barrier()
            P.flush()


    def phaseA2(l, tiles):
        with ExitStack() as ps:
            wq = sb(ps, nc, "wq", [128, 16, 2304], BF16)
            wuq = sb(ps, nc, "wuq", [128, 4, 2048], BF16)
            qnb = sb(ps, nc, "qnb", [128, 512], F32)
            qgn_s = sb(ps, nc, "qgn_s", [128, 1], F32)
            qgpe_s = sb(ps, nc, "qgpe_s", [128, 1024], F32)
            sgun_s = sb(ps, nc, "sgun_s", [128, 512], F32)
            sgub_s = sb(ps, nc, "sgub_s", [128, 512], F32)
            wsT = sb(ps, nc, "wsT", [128, 4, 128], BF16)
            hT = sbn(ps, nc, "hTq", [128, 16, 128], BF16, 2)
            tq = sbn(ps, nc, "tq", [128, 1024], F32, 2)
            junk = sb(ps, nc, "junkq", [128, 512], BF16)
            st = sbn(ps, nc, "stq", [128, 64], F32, 2)
            cqn = sbn(ps, nc, "cqn", [128, 512], BF16, 2)
            cqnT = sbn(ps, nc, "cqnT", [128, 4, 128], BF16, 2)
            qn16 = sbn(ps, nc, "qn16", [128, 1024], BF16, 2)
            QnTt = sbn(ps, nc, "QnTt", [128, 8, 128], BF16, 2)
            rq = sbn(ps, nc, "rq", [128, 1024], F32, 2)
            qpb = sbn(ps, nc, "qpb", [128, 512], BF16, 2)
            QpeTt = sbn(ps, nc, "QpeTt", [128, 4, 128], BF16, 2)
            gqb = sbn(ps, nc, "gqb", [128, 256], BF16, 2)
            grb = sbn(ps, nc, "grb", [128, 512], BF16, 2)
            gvv = sbn(ps, nc, "gvv", [128, 512], F32, 2)
            vn = sbn(ps, nc, "vn", [128, 512], BF16, 2)
            uT = sbn(ps, nc, "uT", [128, 512], F32, 2)
            t2 = sbn(ps, nc, "t2", [128, 512], F32, 2)
            bxT = sbn(ps, nc, "bxT", [128, 4, 128], BF16, 2)
            for kq in range(4):
                P.dma("pool", P.one(), wq.t[:, kq * 4:(kq + 1) * 4, :],
                      wq_d[l, kq * 512:(kq + 1) * 512, :].rearrange("(kc p) n -> p kc n", p=128), writes=[wq.res])
            P.dma("pool", P.one(), wuq.t[:], wuq_d[l].rearrange("(kc p) n -> p kc n", p=128), writes=[wuq.res])
            P.dma("pool", P.one(), wsT.t[:], wsT_d[l], writes=[wsT.res])
            P.dma("sp", P.one(), qnb.t[:], qnorm[l].partition_broadcast(128), writes=[qnb.res])
            P.dma("sp", P.one(), qgn_s.t[:], qgn[l], writes=[qgn_s.res])
            P.dma("sp", P.one(), qgpe_s.t[:], qgpe[l].partition_broadcast(128), writes=[qgpe_s.res])
            P.dma("sp", P.one(), sgun_s.t[:], sgun[l].partition_broadcast(128), writes=[sgun_s.res])
            P.dma("sp", P.one(), sgub_s.t[:], sgub[l].partition_broadcast(128), writes=[sgub_s.res])
            hsl = [P.gs(20 + j) for j in range(2)]
            tsl = [P.gs(23 + j) for j in range(2)]
            pp = Pool8(banks[4:8])
            pz = Pool8(banks[0:4])
            for n, t in enumerate(tiles):
                i = n % 2
                HT = hT[i]
                TQ = tq[i]
                S = st[i]
                P.dma("sp", hsl[i], HT.t[:], hTbuf[t], reads=[R["hTbuf"]], writes=[HT.res])
                P.dma("sp", tsl[i], TQ.t[:], tabq[t], writes=[TQ.res])

                def zmm(c0, c1):
                    bk = pz.next()

                    def f(E, bk=bk, HT=HT):
                        for kc in range(16):
                            ins = E.matmul(bk.t[:, 0:c1 - c0], lhsT=HT.t[:, kc, :], rhs=wq.t[:, kc, c0:c1], start=(kc == 0), stop=(kc == 15))
                        return ins
                    P.op("pe", f, reads=[HT.res, wq.res], writes=[bk.res])
                    return bk
                b_cq = zmm(0, 512)
                P.op("act", lambda E, S=S, bk=b_cq: E.activation(out=junk.t[:], in_=bk.t[:], func=AF.Square, accum_out=S.t[:, 0:1]),
                     reads=[b_cq.res], writes=[junk.res, S.res])
                rstd_ops(P, S, 0, 1, 1, 1.0 / 512)
                CQ = cqn[i]
                P.op("dve", lambda E, S=S, bk=b_cq, CQ=CQ: E.scalar_tensor_tensor(out=CQ.t[:], in0=bk.t[:], scalar=S.t[:, 1:2], in1=qnb.t[:],
                                                                                   op0=ALU.mult, op1=ALU.mult),
                     reads=[b_cq.res, S.res, qnb.res], writes=[CQ.res])
                bk = pp.next()

                def tr2(E, CQ=CQ, bk=bk):
                    for j in range(4):
                        ins = E.transpose(out=bf(bk)[:, j * 128:(j + 1) * 128], in_=CQ.t[:, j * 128:(j + 1) * 128], identity=ident.t[:])
                    return ins
                P.op("pe", tr2, reads=[CQ.res, ident.res], writes=[bk.res])
                CT = cqnT[i]
                P.op("act", lambda E, CT=CT, bk=bk: E.copy(out=CT.t[:], in_=bf(bk, 512)), reads=[bk.res], writes=[CT.res])
                qb = []
                for q4 in range(4):
                    bk = pp.next()

                    def f(E, bk=bk, CT=CT, q4=q4):
                        for kc in range(4):
                            ins = E.matmul(bk.t[:], lhsT=CT.t[:, kc, :], rhs=wuq.t[:, kc, q4 * 512:(q4 + 1) * 512], start=(kc == 0), stop=(kc == 3))
                        return ins
                    P.op("pe", f, reads=[CT.res, wuq.res], writes=[bk.res])
                    qb.append(bk)
                for h in range(8):
                    P.op("act", lambda E, S=S, bk=qb[h // 4], h=h: E.activation(out=junk.t[:, 0:128], in_=bk.t[:, (h % 4) * 128:(h % 4 + 1) * 128],
                                                                               func=AF.Square, accum_out=S.t[:, 8 + h:9 + h]),
                         reads=[qb[h // 4].res], writes=[junk.res], awrites=[S.res])
                    P.op("act", lambda E, S=S, bk=qb[2], h=h: E.activation(out=junk.t[:, 0:64], in_=bk.t[:, h * 64:(h + 1) * 64],
                                                                          func=AF.Square, accum_out=S.t[:, 16 + h:17 + h]),
                         reads=[qb[2].res], writes=[junk.res], awrites=[S.res])
                P.op("dve", lambda E, S=S: E.tensor_tensor(out=S.t[:, 8:16], in0=S.t[:, 8:16], in1=S.t[:, 16:24], op=ALU.add),
                     reads=[S.res], writes=[S.res])
                rstd_ops(P, S, 8, 24, 8, 1.0 / 192)
                QN = qn16[i]
                for h in range(8):
                    P.op("dve", lambda E, S=S, QN=QN, bk=qb[h // 4], h=h: E.tensor_scalar(
                        out=QN.t[:, h * 128:(h + 1) * 128], in0=bk.t[:, (h % 4) * 128:(h % 4 + 1) * 128], scalar1=S.t[:, 24 + h:25 + h],
                        scalar2=None, op0=ALU.mult), reads=[qb[h // 4].res, S.res], awrites=[QN.res])
                RQ = rq[i]
                P.op("dve", lambda E, RQ=RQ, TQ=TQ, bk=qb[2]: E.tensor_tensor(out=RQ.t[:, 0:512], in0=bk.t[:], in1=TQ.t[:, 0:512], op=ALU.mult),
                     reads=[qb[2].res, TQ.res], awrites=[RQ.res])
                P.op("dve", lambda E, RQ=RQ, TQ=TQ, bk=qb[3]: E.tensor_tensor(out=RQ.t[:, 512:1024], in0=bk.t[:], in1=TQ.t[:, 512:1024], op=ALU.mult),
                     reads=[qb[3].res, TQ.res], awrites=[RQ.res])
                P.op("pool", lambda E, RQ=RQ: E.tensor_tensor(out=RQ.t[:], in0=RQ.t[:], in1=qgpe_s.t[:], op=ALU.mult),
                     reads=[RQ.res, qgpe_s.res], writes=[RQ.res])
                P.op("pool", lambda E, RQ=RQ: E.tensor_tensor(out=RQ.t[:, 0:512], in0=RQ.t[:, 0:512], in1=RQ.t[:, 512:1024], op=ALU.add),
                     reads=[RQ.res], writes=[RQ.res])
                QP = qpb[i]
                for h in range(8):
                    P.op("dve", lambda E, S=S, QP=QP, RQ=RQ, h=h: E.tensor_scalar(
                        out=QP.t[:, h * 64:(h + 1) * 64], in0=RQ.t[:, h * 64:(h + 1) * 64], scalar1=S.t[:, 24 + h:25 + h],
                        scalar2=None, op0=ALU.mult), reads=[RQ.res, S.res], awrites=[QP.res])
                bk = pp.next()

                def tr3(E, QN=QN, bk=bk):
                    for h in range(8):
                        ins = E.transpose(out=bf(bk)[:, h * 128:(h + 1) * 128], in_=QN.t[:, h * 128:(h + 1) * 128], identity=ident.t[:])
                    return ins
                P.op("pe", tr3, reads=[QN.res, ident.res], writes=[bk.res])
                QT = QnTt[i]
                P.op("dve", lambda E, QT=QT, bk=bk: E.tensor_scalar(out=QT.t[:].rearrange("p h t -> p (h t)"), in0=bf(bk), scalar1=qgn_s.t[:, 0:1],
                                                                     scalar2=None, op0=ALU.mult),
                     reads=[bk.res, qgn_s.res], writes=[QT.res])
                P.dma("sp", P.gs(0 + i), QnT[t], QT.t[:], reads=[QT.res], awrites=[R["QnT"]])
                bk = pp.next()

                def tr4(E, QP=QP, bk=bk):
                    for j in range(4):
                        ins = E.transpose(out=bf(bk)[:, j * 128:(j + 1) * 128], in_=QP.t[:, j * 128:(j + 1) * 128], identity=ident.t[:])
                    return ins
                P.op("pe", tr4, reads=[QP.res, ident.res], writes=[bk.res])
                QPT = QpeTt[i]
                P.op("act", lambda E, QPT=QPT, bk=bk: E.copy(out=QPT.t[:], in_=bf(bk, 512)), reads=[bk.res], writes=[QPT.res])
                P.dma("sp", P.gs(2 + i), QpeT[t], QPT.t[:], reads=[QPT.res], awrites=[R["QpeT"]])
                b_gq = zmm(512, 768)
                GQ = gqb[i]
                P.op("act", lambda E, GQ=GQ, bk=b_gq: E.activation(out=GQ.t[:], in_=bk.t[:, 0:256], func=AF.Copy, scale=0.125),
                     reads=[b_gq.res], writes=[GQ.res])
                P.dma("sp", P.gs(4 + i), glaQ[t * 128:(t + 1) * 128, :], GQ.t[:], reads=[GQ.res], awrites=[R["glaQ"]])
                b_gr = pz.next()

                def grT(E, bk=b_gr, HT=HT):
                    for g in range(4):
                        for kc in range(16):
                            ins = E.matmul(bk.t[:, g * 128:(g + 1) * 128], lhsT=wq.t[:, kc, 768 + g * 128:768 + (g + 1) * 128], rhs=HT.t[:, kc, :],
                                           start=(kc == 0), stop=(kc == 15))
                    return ins
                P.op("pe", grT, reads=[HT.res, wq.res], writes=[b_gr.res])
                GR = grb[i]
                P.op("act", lambda E, GR=GR, bk=b_gr: E.activation(out=GR.t[:], in_=bk.t[:], func=AF.Silu), reads=[b_gr.res], writes=[GR.res])
                P.dma("sp", P.gs(6 + i), glaR[t], GR.t[:].rearrange("p (g k) -> p g k", g=4), reads=[GR.res], awrites=[R["glaR"]])
                b_zv = zmm(1280, 1792)
                GV = gvv[i]
                P.op("act", lambda E, GV=GV, bk=b_zv: E.activation(out=GV.t[:], in_=bk.t[:], func=AF.Gelu_apprx_tanh), reads=[b_zv.res], writes=[GV.res])
                for g in range(4):
                    P.op("act", lambda E, S=S, GV=GV, g=g: E.activation(out=junk.t[:, 0:128], in_=GV.t[:, g * 128:(g + 1) * 128], func=AF.Square,
                                                                        accum_out=S.t[:, 32 + g:33 + g]),
                         reads=[GV.res], writes=[junk.res], awrites=[S.res])
                rstd_ops(P, S, 32, 36, 4, 1.0 / 128)
                for g in range(4):
                    P.op("dve", lambda E, S=S, GV=GV, g=g: E.tensor_scalar(out=GV.t[:, g * 128:(g + 1) * 128], in0=GV.t[:, g * 128:(g + 1) * 128],
                                                                           scalar1=S.t[:, 36 + g:37 + g], scalar2=None, op0=ALU.mult),
                         reads=[GV.res, S.res], writes=[GV.res])
                VN = vn[i]
                P.op("pool", lambda E, VN=VN, GV=GV: E.tensor_tensor(out=VN.t[:], in0=GV.t[:], in1=sgun_s.t[:], op=ALU.mult),
                     reads=[GV.res, sgun_s.res], writes=[VN.res])
                b_zu = pz.next()

                def zu(E, bk=b_zu, HT=HT):
                    for g in range(4):
                        for kc in range(16):
                            ins = E.matmul(bk.t[:, g * 128:(g + 1) * 128], lhsT=wq.t[:, kc, 1792 + g * 128:1792 + (g + 1) * 128], rhs=HT.t[:, kc, :],
                                           start=(kc == 0), stop=(kc == 15))
                    return ins
                P.op("pe", zu, reads=[HT.res, wq.res], writes=[b_zu.res])
                U = uT[i]
                P.op("act", lambda E, U=U, bk=b_zu: E.activation(out=U.t[:], in_=bk.t[:], func=AF.Gelu_apprx_tanh), reads=[b_zu.res], writes=[U.res])
                bk = pp.next()

                def sg(E, bk=bk, VN=VN):
                    for g in range(4):
                        ins = E.matmul(bk.t[:, g * 128:(g + 1) * 128], lhsT=VN.t[:, g * 128:(g + 1) * 128], rhs=wsT.t[:, g, :], start=True, stop=True)
                    return ins
                P.op("pe", sg, reads=[VN.res, wsT.res], writes=[bk.res])
                T2 = t2[i]
                P.op("dve", lambda E, T2=T2, bk=bk: E.tensor_tensor(out=T2.t[:], in0=bk.t[:], in1=sgub_s.t[:], op=ALU.add),
                     reads=[bk.res, sgub_s.res], writes=[T2.res])
                BX = bxT[i]
                P.op("pool", lambda E, T2=T2, U=U, BX=BX: E.tensor_tensor(out=BX.t[:].rearrange("p g t -> p (g t)"), in0=T2.t[:], in1=U.t[:], op=ALU.mult),
                     reads=[T2.res, U.res], writes=[BX.res])
                P.dma("sp", P.gs(8 + i), mixT[t, :, 8:12, :], BX.t[:], reads=[BX.res], awrites=[R["mixT"]])
            P.barrier()
            P.flush()

    cx.phaseA2 = phaseA2


    def phaseB(l, with_ctx, heads=range(8), qblocks=None):
        with ExitStack() as ps:
            Kn = sbn(ps, nc, "Kn", [128, NT, 128], BF16, 2)
            Vh = sbn(ps, nc, "Vh", [128, NT, 128], BF16, 2)
            Qn = sbn(ps, nc, "Qn", [128, NT, 128], BF16, 2)
            Qp = sbn(ps, nc, "Qp", [128, NT, 128], BF16, 2)
            KpeEO = sbn(ps, nc, "KpeEO", [128, NTOK], BF16, 2)
            rk = sb(ps, nc, "rk", [128, NT, 8], F32)
            pT = sbn(ps, nc, "pT", [128, 512], BF16, 3)
            rden = sbn(ps, nc, "rden", [128, 512], F32, 2)
            o16 = sbn(ps, nc, "o16", [128, 4, 128], BF16, 2)
            P.op("pool", lambda E: E.memset(KpeEO[0].t[64:128, :], 0.0), awrites=[KpeEO[0].res])
            P.op("pool", lambda E: E.memset(KpeEO[1].t[0:64, :], 0.0), awrites=[KpeEO[1].res])
            P.dma("sp", P.one(), KpeEO[0].t[0:64, :], KpeT[0:64, :], reads=[R["KpeT"]], awrites=[KpeEO[0].res])
            P.dma("sp", P.one(), KpeEO[1].t[64:128, :], KpeT[64:128, :], reads=[R["KpeT"]], awrites=[KpeEO[1].res])
            P.dma("sp", P.one(), rk.t[:], rkb.rearrange("(t p) h -> p t h", p=128), reads=[R["rkb"]], writes=[rk.res])
            KnS = KnT.rearrange("t d h k -> d t h k")
            QnS = QnT.rearrange("t d h k -> d t h k")
            QpS = QpeT.rearrange("t p j k -> p t j k")
            VS = Vb.rearrange("(t p) c -> p t c", p=128)
            heads = list(heads)

            def loads(h):
                s_ = h % 2
                for a, b in ((0, 17), (17, NT)):
                    P.dma("sp", P.gs(0 + s_), Kn[s_].t[:, a:b, :], KnS[:, a:b, h, :], reads=[R["KnT"]], awrites=[Kn[s_].res])
                    P.dma("sp", P.gs(2 + s_), Vh[s_].t[:, a:b, :], VS[:, a:b, h * 128:(h + 1) * 128], reads=[R["Vb"]], awrites=[Vh[s_].res])
                    P.dma("sp", P.gs(4 + s_), Qn[s_].t[:, a:b, :], QnS[:, a:b, h, :], reads=[R["QnT"]], awrites=[Qn[s_].res])
                    P.dma("sp", P.gs(6 + s_), Qp[s_].t[:, a:b, :], QpS[:, a:b, h // 2, :], reads=[R["QpeT"]], awrites=[Qp[s_].res])
            spool = Pool8(banks[0:4])
            cnt = [0]
            loads(heads[0])
            for hi, h in enumerate(heads):
                if hi + 1 < len(heads):
                    loads(heads[hi + 1])
                s_ = h % 2
                hp = h % 2
                KN, VH, QN, QP = Kn[s_], Vh[s_], Qn[s_], Qp[s_]
                blocks = [(q0, 4, list(range(NT))) for q0 in range(0, NXT, 4)]
                if with_ctx:
                    blocks.append((NXT, 2, [NXT, NXT + 1]))
                if qblocks is not None:
                    blocks = [blocks[j] for j in qblocks]
                for (q0, nq, keys) in blocks:
                    N = nq * 128
                    c = cnt[0]
                    cnt[0] += 1
                    ob, db = banks[4 + c % 2], banks[6 + c % 2]
                    qn_ap = QN.t[:, q0:q0 + nq, :].rearrange("p a b -> p (a b)")
                    qp_ap = QP.t[:, q0:q0 + nq, :].rearrange("p a b -> p (a b)")
                    Kpe = KpeEO[hp]
                    sb_ = {}

                    def score(j, N=N, KN=KN, QN=QN, QP=QP, Kpe=Kpe, qn_ap=qn_ap, qp_ap=qp_ap, keys=keys, sb_=sb_):
                        kt = keys[j]
                        bk = spool.next()

                        def f(E, bk=bk, kt=kt, N=N, KN=KN, Kpe=Kpe, qn_ap=qn_ap, qp_ap=qp_ap):
                            E.matmul(bk.t[:, 0:N], lhsT=KN.t[:, kt, :], rhs=qn_ap, start=True, stop=False)
                            return E.matmul(bk.t[:, 0:N], lhsT=Kpe.t[:, kt * 128:(kt + 1) * 128], rhs=qp_ap, start=False, stop=True)
                        P.op("pe", f, reads=[KN.res, QN.res, QP.res, Kpe.res], writes=[bk.res])
                        sb_[j] = bk
                    nk = len(keys)
                    score(0)
                    if nk > 1:
                        score(1)
                    for j in range(nk):
                        kt = keys[j]
                        bk = sb_.pop(j)
                        PT = pT[j % 3]
                        P.op("act", lambda E, PT=PT, bk=bk, kt=kt, N=N, h=h: E.activation(out=PT.t[:, 0:N], in_=bk.t[:, 0:N], func=AF.Exp,
                                                                                       scale=rk.t[:, kt, h:h + 1]),
                             reads=[bk.res, rk.res], writes=[PT.res])
                        if j + 2 < nk:
                            score(j + 2)

                        def acc(E, PT=PT, kt=kt, j=j, N=N, nk=nk, ob=ob, db=db, VH=VH):
                            E.matmul(ob.t[:, 0:N], lhsT=VH.t[:, kt, :], rhs=PT.t[:, 0:N], start=(j == 0), stop=(j == nk - 1))
                            return E.matmul(db.t[:, 0:N], lhsT=ones_bf.t[:], rhs=PT.t[:, 0:N], start=(j == 0), stop=(j == nk - 1))
                        P.op("pe", acc, reads=[VH.res, PT.res, ones_bf.res], writes=[ob.res, db.res])
                    RD = rden[c % 2]
                    P.op("dve", lambda E, RD=RD, db=db, N=N: E.reciprocal(out=RD.t[:, 0:N], in_=db.t[:, 0:N]), reads=[db.res], writes=[RD.res])
                    O = o16[c % 2]
                    P.op("dve", lambda E, RD=RD, O=O, ob=ob, N=N, nq=nq: E.tensor_tensor(out=O.t[:, 0:nq, :].rearrange("p a b -> p (a b)"), in0=ob.t[:, 0:N],
                                                                                      in1=RD.t[:, 0:N], op=ALU.mult),
                         reads=[ob.res, RD.res], writes=[O.res])
                    P.dma("sp", P.gs(8 + c % 2), mixT[q0:q0 + nq, :, h, :].rearrange("t p k -> p t k"), O.t[:, 0:nq, :], reads=[O.res], awrites=[R["mixT"]])
            P.barrier()
            P.flush()

    cx.phaseB = phaseB


    def phaseC(l, ctx_out, xtiles=None, cstop=99):
        with ExitStack() as ps:
            gc = sb(ps, nc, "gc", [128, 1412], F32)
            mk = sb(ps, nc, "mk", [128, 4, 256], BF16)
            gcb = sb(ps, nc, "gcb", [128, 386], BF16)
            rh = sbn(ps, nc, "rh", [128, 256], BF16, 2)
            tdg = sbn(ps, nc, "tdg", [128, 128], F32, 2)
            rl = sbn(ps, nc, "rl", [128, 256], BF16, 2)
            gon = sb(ps, nc, "gon", [128, 1], F32)
            S2 = sb(ps, nc, "S2", [128, 2, 128], F32)
            Sb = sb(ps, nc, "Sb", [128, 2, 128], BF16)
            rr = sbn(ps, nc, "rr", [128, 256], F32, 2)
            kv = sbn(ps, nc, "kvg", [128, 768], BF16, 2)
            qq = sbn(ps, nc, "qq", [128, 256], BF16, 2)
            eb = sbn(ps, nc, "eb", [128, 256], F32, 2)
            enb = sbn(ps, nc, "enb", [128, 256], F32, 2)
            ebt = sbn(ps, nc, "ebt", [128, 256], F32, 2)
            edT = sbn(ps, nc, "edT", [128, 4], F32, 2)
            qe = sbn(ps, nc, "qe", [128, 256], BF16, 2)
            ke = sbn(ps, nc, "ke", [128, 256], BF16, 2)
            kw = sbn(ps, nc, "kw", [128, 256], F32, 2)
            kwz = sbn(ps, nc, "kwz", [128, 2, 256], BF16, 2)
            qeEO = sbn(ps, nc, "qeEO", [128, 2, 2, 128], BF16, 2)
            keT = sbn(ps, nc, "keT", [128, 2, 128], BF16, 2)
            aTm = sbn(ps, nc, "aTm", [128, 256], BF16, 2)
            oF = sbn(ps, nc, "oF", [128, 4, 128], F32, 2)
            oS = sbn(ps, nc, "oS", [128, 4, 128], F32, 2)
            sq = sbn(ps, nc, "sqo", [128, 512], BF16, 2)
            rs = sbn(ps, nc, "rso", [128, 512], F32, 2)
            srT = sbn(ps, nc, "srT", [128, 4, 128], BF16, 2)
            cxT = sbn(ps, nc, "cxT", [128, 4, 128], BF16, 2)
            P.dma("sp", P.one(), gc.t[:], gcon, writes=[gc.res])
            P.dma("pool", P.one(), mk.t[:].rearrange("p a b -> p (a b)"), gcon[:, 388:1412], writes=[mk.res])
            P.dma("pool", P.one(), gcb.t[:], gcon[:, 0:386], writes=[gcb.res])
            P.dma("sp", P.one(), gon.t[:], glaon[l, 0:128].rearrange("(p o) -> p o", o=1), writes=[gon.res])
            for i in range(2):
                P.op("pool", lambda E, i=i: E.memset(qeEO[i].t[:], 0.0), writes=[qeEO[i].res])
            TRI = {"F": gcb.t[:, 0:128], "B": gcb.t[:, 128:256]}
            ONES2 = gcb.t[:, 256:384]
            CIND = gcb.t[:, 384:386]
            pA = Pool8(banks[0:3])
            pB = Pool8(banks[3:5])
            pO = Pool8(banks[5:7])
            bKV = banks[7]
            xt_ = list(range(NXT)) if xtiles is None else list(xtiles)
            n = [0]

            def tile_pass(t, d, want_out, final):
                i = n[0] % 2
                n[0] += 1
                RR, KV, QQ = rr[i], kv[i], qq[i]
                P.dma("sp", P.gs(0 + i), RR.t[:], glaG[t * 128:(t + 1) * 128, (0 if d == "F" else 256):(256 if d == "F" else 512)], reads=[R["glaG"]], writes=[RR.res])
                P.dma("sp", P.gs(2 + i), KV.t[:], glaKV[t * 128:(t + 1) * 128, :], reads=[R["glaKV"]], writes=[KV.res])
                if want_out:
                    P.dma("sp", P.gs(4 + i), QQ.t[:], glaQ[t * 128:(t + 1) * 128, :], reads=[R["glaQ"]], writes=[QQ.res])
                b1, b2, b3 = pA.next(), pA.next(), pA.next()
                tri = TRI[d]
                RH, RL = rh[i], rl[i]
                P.op("dve", lambda E, RH=RH, RR=RR: E.tensor_copy(out=RH.t[:], in_=RR.t[:]), reads=[RR.res], writes=[RH.res])
                P.op("dve", lambda E, RH=RH, RL=RL, RR=RR: E.tensor_tensor(out=RL.t[:], in0=RR.t[:], in1=RH.t[:], op=ALU.subtract), reads=[RR.res, RH.res], writes=[RL.res])

                def f1(E, b1=b1, RH=RH, RL=RL, tri=tri):
                    E.matmul(b1.t[:, 0:256], lhsT=tri, rhs=RH.t[:], start=True, stop=False)
                    return E.matmul(b1.t[:, 0:256], lhsT=tri, rhs=RL.t[:], start=False, stop=True)
                P.op("pe", f1, reads=[gcb.res, RH.res, RL.res], writes=[b1.res])

                def f2(E, b2=b2, RH=RH, RL=RL):
                    E.matmul(b2.t[:, 0:256], lhsT=ONES2, rhs=RH.t[:], start=True, stop=False)
                    return E.matmul(b2.t[:, 0:256], lhsT=ONES2, rhs=RL.t[:], start=False, stop=True)
                P.op("pe", f2, reads=[gcb.res, RH.res, RL.res], writes=[b2.res])

                def f3(E, b3=b3, RH=RH, RL=RL):
                    for pr in range(2):
                        E.matmul(b3.t[:, pr * 2:pr * 2 + 2], lhsT=RH.t[:, pr * 128:(pr + 1) * 128], rhs=CIND, start=True, stop=False)
                        ins = E.matmul(b3.t[:, pr * 2:pr * 2 + 2], lhsT=RL.t[:, pr * 128:(pr + 1) * 128], rhs=CIND, start=False, stop=True)
                    return ins
                P.op("pe", f3, reads=[gcb.res, RH.res, RL.res], writes=[b3.res])
                EB, ENB, EBT, EDT = eb[i], enb[i], ebt[i], edT[i]
                if want_out:
                    P.op("act", lambda E, EB=EB, b1=b1: E.activation(out=EB.t[:], in_=b1.t[:, 0:256], func=AF.Exp), reads=[b1.res], writes=[EB.res])
                P.op("act", lambda E, ENB=ENB, b1=b1: E.activation(out=ENB.t[:], in_=b1.t[:, 0:256], func=AF.Exp, scale=-1.0), reads=[b1.res], writes=[ENB.res])
                P.op("act", lambda E, EBT=EBT, b2=b2: E.activation(out=EBT.t[:], in_=b2.t[:, 0:256], func=AF.Exp), reads=[b2.res], writes=[EBT.res])
                P.op("act", lambda E, EDT=EDT, b3=b3: E.activation(out=EDT.t[:], in_=b3.t[:, 0:4], func=AF.Exp), reads=[b3.res], writes=[EDT.res])
                if cstop < 1:
                    return
                QE, KE, KW, KWZ = qe[i], ke[i], kw[i], kwz[i]
                if want_out:
                    P.op("dve", lambda E, QE=QE, QQ=QQ, EB=EB: E.tensor_tensor(out=QE.t[:], in0=QQ.t[:], in1=EB.t[:], op=ALU.mult), reads=[QQ.res, EB.res], writes=[QE.res])
                    P.op("dve", lambda E, KE=KE, KV=KV, ENB=ENB: E.tensor_tensor(out=KE.t[:], in0=KV.t[:, 0:256], in1=ENB.t[:], op=ALU.mult), reads=[KV.res, ENB.res], writes=[KE.res])
                P.op("pool", lambda E, KW=KW, ENB=ENB, EBT=EBT: E.tensor_tensor(out=KW.t[:], in0=ENB.t[:], in1=EBT.t[:], op=ALU.mult), reads=[ENB.res, EBT.res], writes=[KW.res])
                P.op("pool", lambda E, KW=KW, KV=KV: E.tensor_tensor(out=KW.t[:], in0=KW.t[:], in1=KV.t[:, 0:256], op=ALU.mult), reads=[KW.res, KV.res], writes=[KW.res])
                for c in range(2):
                    P.op("dve", lambda E, KW=KW, KWZ=KWZ, c=c: E.tensor_scalar(out=KWZ.t[:, c, :], in0=KW.t[:], scalar1=gc.t[:, 386 + c:387 + c], scalar2=None, op0=ALU.mult),
                         reads=[KW.res, gc.res], awrites=[KWZ.res])
                if cstop < 2:
                    return
                QEO, KET = qeEO[i], keT[i]
                if want_out:
                    bt = pA.next()

                    def tr(E, bt=bt, QE=QE, KE=KE):
                        for j in range(2):
                            E.transpose(out=bf(bt)[:, j * 128:(j + 1) * 128], in_=QE.t[:, j * 128:(j + 1) * 128], identity=ident.t[:])
                        for j in range(2):
                            ins = E.transpose(out=bf(bt)[:, (2 + j) * 128:(3 + j) * 128], in_=KE.t[:, j * 128:(j + 1) * 128], identity=ident.t[:])
                        return ins
                    P.op("pe", tr, reads=[QE.res, KE.res, ident.res], writes=[bt.res])
                    for hh in range(2):
                        P.op("dve", lambda E, bt=bt, QEO=QEO, hh=hh: E.tensor_scalar(out=QEO.t[:, hh, :, :].rearrange("p a b -> p (a b)"), in0=bf(bt)[:, 0:256],
                                                                                    scalar1=gc.t[:, 386 + hh:387 + hh], scalar2=None, op0=ALU.mult),
                             reads=[bt.res, gc.res], awrites=[QEO.res])
                    P.op("dve", lambda E, bt=bt, KET=KET: E.tensor_copy(out=KET.t[:].rearrange("p a b -> p (a b)"), in_=bf(bt)[:, 256:512]), reads=[bt.res], writes=[KET.res])
                    OS = oS[i]
                    if final:
                        OFl = oF[i]
                        P.dma("sp", P.gs(6 + i), OFl.t[:], glaO[t], reads=[R["glaO"]], writes=[OFl.res])
                if cstop < 3:
                    return
                for c in ([0, 1] if d == "F" else [1, 0]):
                    if want_out and cstop >= 4:
                        ba = pB.next()

                        def fa(E, ba=ba, KET=KET, QEO=QEO, c=c):
                            for h in range(4):
                                ins = E.matmul(ba.t[:, h * 64:(h + 1) * 64], lhsT=KET.t[:, h // 2, :], rhs=QEO.t[:, h % 2, h // 2, c * 64:(c + 1) * 64], start=True, stop=True)
                            return ins
                        P.op("pe", fa, reads=[KET.res, QEO.res], writes=[ba.res])
                        AT = aTm[c]
                        mi = (0 if d == "F" else 2) + c
                        P.op("dve", lambda E, AT=AT, ba=ba, mi=mi: E.tensor_tensor(out=AT.t[:], in0=ba.t[:, 0:256], in1=mk.t[:, mi, :], op=ALU.mult), reads=[ba.res, mk.res], writes=[AT.res])
                        bo = pO.next()

                        def fo(E, bo=bo, QEO=QEO, AT=AT, KV=KV, c=c):
                            for h in range(4):
                                E.matmul(bo.t[:, h * 64:(h + 1) * 64], lhsT=Sb.t[:, h // 2, :], rhs=QEO.t[:, h % 2, h // 2, c * 64:(c + 1) * 64], start=True, stop=False)
                                ins = E.matmul(bo.t[:, h * 64:(h + 1) * 64], lhsT=KV.t[:, 256 + h * 128:256 + (h + 1) * 128], rhs=AT.t[:, h * 64:(h + 1) * 64], start=False, stop=True)
                            return ins
                        P.op("pe", fo, reads=[Sb.res, QEO.res, AT.res, KV.res], writes=[bo.res])
                        bo_v = bo.t[:, 0:256].rearrange("p (h k) -> p h k", h=4)
                        if final:
                            P.op("dve", lambda E, OS=OS, OFl=OFl, bo_v=bo_v, c=c: E.tensor_tensor(out=OS.t[:, :, c * 64:(c + 1) * 64], in0=bo_v, in1=OFl.t[:, :, c * 64:(c + 1) * 64], op=ALU.add),
                                 reads=[bo.res, OFl.res], awrites=[OS.res])
                        else:
                            P.op("act", lambda E, OS=OS, bo_v=bo_v, c=c: E.copy(out=OS.t[:, :, c * 64:(c + 1) * 64], in_=bo_v), reads=[bo.res], awrites=[OS.res])

                    def fk(E, KWZ=KWZ, KV=KV, c=c):
                        E.matmul(bKV.t[:, 0:256], lhsT=KWZ.t[:, c, 0:128], rhs=KV.t[:, 256:512], start=True, stop=True)
                        return E.matmul(bKV.t[:, 256:512], lhsT=KWZ.t[:, c, 128:256], rhs=KV.t[:, 512:768], start=True, stop=True)
                    P.op("pe", fk, reads=[KWZ.res, KV.res], writes=[bKV.res])
                    for pr in range(2):
                        TD = tdg[pr]
                        P.op("dve", lambda E, pr=pr, TD=TD: E.tensor_scalar(out=TD.t[:], in0=bKV.t[:, pr * 256:pr * 256 + 128], scalar1=gc.t[:, 386:387], scalar2=None, op0=ALU.mult),
                             reads=[bKV.res, gc.res], writes=[TD.res])
                        P.op("dve", lambda E, pr=pr, TD=TD: E.scalar_tensor_tensor(out=TD.t[:], in0=bKV.t[:, pr * 256 + 128:pr * 256 + 256], scalar=gc.t[:, 387:388], in1=TD.t[:],
                                                                                  op0=ALU.mult, op1=ALU.add), reads=[bKV.res, gc.res, TD.res], writes=[TD.res])
                        P.op("dve", lambda E, pr=pr, TD=TD, c=c, EDT=EDT: E.scalar_tensor_tensor(out=S2.t[:, pr, :], in0=S2.t[:, pr, :], scalar=EDT.t[:, pr * 2 + c:pr * 2 + c + 1], in1=TD.t[:],
                                                                                              op0=ALU.mult, op1=ALU.add), reads=[S2.res, EDT.res, TD.res], awrites=[S2.res])
                    P.op("act", lambda E: E.copy(out=Sb.t[:], in_=S2.t[:]), reads=[S2.res], writes=[Sb.res])
                if cstop < 5:
                    return
                if want_out and not final:
                    P.dma("sp", P.gs(8 + i), glaO[t], OS.t[:], reads=[OS.res], awrites=[R["glaO"]])
                if want_out and final:
                    SQ, RS, SR, CX = sq[i], rs[i], srT[i], cxT[i]
                    P.dma("sp", P.gs(10 + i), SR.t[:], glaR[t], reads=[R["glaR"]], writes=[SR.res])
                    osf = OS.t[:].rearrange("p h k -> p (h k)")
                    P.op("pool", lambda E, SQ=SQ, osf=osf: E.tensor_tensor(out=SQ.t[:], in0=osf, in1=osf, op=ALU.mult), reads=[OS.res], writes=[SQ.res])
                    bs = pB.next()
                    P.op("pe", lambda E, bs=bs, SQ=SQ: E.matmul(bs.t[:], lhsT=ones_bf.t[:], rhs=SQ.t[:], start=True, stop=True), reads=[SQ.res, ones_bf.res], writes=[bs.res])
                    P.op("act", lambda E, bs=bs, RS=RS: E.activation(out=RS.t[:], in_=bs.t[:], func=AF.Ln, scale=1.0 / 128, bias=EPS), reads=[bs.res], writes=[RS.res])
                    P.op("act", lambda E, RS=RS: E.activation(out=RS.t[:], in_=RS.t[:], func=AF.Exp, scale=-0.5), reads=[RS.res], writes=[RS.res])
                    P.op("dve", lambda E, RS=RS, osf=osf: E.scalar_tensor_tensor(out=RS.t[:], in0=osf, scalar=gon.t[:, 0:1], in1=RS.t[:], op0=ALU.mult, op1=ALU.mult),
                         reads=[OS.res, RS.res, gon.res], writes=[RS.res])
                    P.op("pool", lambda E, RS=RS, SR=SR, CX=CX: E.tensor_tensor(out=CX.t[:].rearrange("p h k -> p (h k)"), in0=RS.t[:], in1=SR.t[:].rearrange("p h k -> p (h k)"), op=ALU.mult),
                         reads=[RS.res, SR.res], writes=[CX.res])
                    P.dma("sp", P.gs(12 + i), mixT[t, :, 12:16, :], CX.t[:], reads=[CX.res], awrites=[R["mixT"]])

            for d in ("F", "B"):
                P.op("pool", lambda E: E.memset(S2.t[:], 0.0), writes=[S2.res])
                P.op("pool", lambda E: E.memset(Sb.t[:], 0.0), writes=[Sb.res])
                ct = [NXT, NXT + 1] if d == "F" else [NXT + 1, NXT]
                for t in ct:
                    tile_pass(t, d, ctx_out, d == "B")
                for t in (xt_ if d == "F" else xt_[::-1]):
                    tile_pass(t, d, True, d == "B")
            P.barrier()
            P.flush()

    cx.phaseC = phaseC


    def phaseD(l, tiles):
        with ExitStack() as ps:
            wo = sb(ps, nc, "wo", [128, 16, D], BF16)
            wr = sb(ps, nc, "wrs", [128, 16, 20], BF16)
            brs = sb(ps, nc, "brs", [128, 20], F32)
            gt1 = sbn(ps, nc, "gt1", [128, D], F32, 2)
            G2 = sbn(ps, nc, "G2_", [128, D], F32, 2)
            S2m = sbn(ps, nc, "S2m", [128, D], F32, 2)
            mx = sbn(ps, nc, "mx", [128, 16, 128], BF16, 2)
            xt = sbn(ps, nc, "xd", [128, D], F32, 2)
            tmp = sb(ps, nc, "tmpd", [128, D], F32)
            junk = sb(ps, nc, "junkd", [128, D], BF16)
            h2 = sbn(ps, nc, "h2", [128, D], BF16, 2)
            h2T = sbn(ps, nc, "h2T", [128, 16, 128], BF16, 2)
            st = sbn(ps, nc, "std", [128, 8], F32, 2)
            rt = sbn(ps, nc, "rt", [128, 128], F32, 2)
            for kq in range(4):
                P.dma("pool", P.one(), wo.t[:, kq * 4:(kq + 1) * 4, :], wout_d[l, kq * 512:(kq + 1) * 512, :].rearrange("(kc p) n -> p kc n", p=128), writes=[wo.res])
            P.dma("pool", P.one(), wr.t[:], wr_d[l].rearrange("(kc p) n -> p kc n", p=128), writes=[wr.res])
            P.dma("sp", P.one(), brs.t[:], br_d[l].partition_broadcast(128), writes=[brs.res])
            for r in range(2):
                P.dma("sp", P.one(), gt1[r].t[:], modrow(l, r, 2), reads=[R["modbuf"]], writes=[gt1[r].res])
                P.dma("sp", P.one(), G2[r].t[:], modrow(l, r, 4), reads=[R["modbuf"]], writes=[G2[r].res])
                P.dma("sp", P.one(), S2m[r].t[:], modrow(l, r, 3), reads=[R["modbuf"]], writes=[S2m[r].res])
            pz = Pool8(banks[0:4])
            pp = Pool8(banks[4:8])
            for n, t in enumerate(tiles):
                i = n % 2
                r = 0 if t < NXT else 1
                MX, X, S, H2, HT, RT = mx[i], xt[i], st[i], h2[i], h2T[i], rt[i]
                src, sres = tile_src(l, t)
                P.dma("sp", P.gs(0 + i), MX.t[:], mixT[t], reads=[R["mixT"]], writes=[MX.res])
                P.dma("sp", P.gs(2 + i), X.t[:], src, reads=([sres] if sres else []), writes=[X.res])
                for nb in range(4):
                    bk = pz.next()

                    def f(E, bk=bk, MX=MX, nb=nb):
                        for kc in range(16):
                            ins = E.matmul(bk.t[:], lhsT=MX.t[:, kc, :], rhs=wo.t[:, kc, nb * 512:(nb + 1) * 512], start=(kc == 0), stop=(kc == 15))
                        return ins
                    P.op("pe", f, reads=[MX.res, wo.res], writes=[bk.res])
                    P.op("dve", lambda E, bk=bk, nb=nb, r=r: E.tensor_tensor(out=tmp.t[:, nb * 512:(nb + 1) * 512], in0=bk.t[:], in1=gt1[r].t[:, nb * 512:(nb + 1) * 512], op=ALU.mult),
                         reads=[bk.res, gt1[r].res], awrites=[tmp.res])
                P.op("pool", lambda E, X=X: E.tensor_tensor(out=X.t[:], in0=X.t[:], in1=tmp.t[:], op=ALU.add), reads=[X.res, tmp.res], writes=[X.res])
                P.dma("sp", P.gs(4 + i), x1buf[t * 128:(t + 1) * 128, :], X.t[:], reads=[X.res], awrites=[R["x1buf"]])
                P.op("act", lambda E, X=X, S=S: E.activation(out=junk.t[:], in_=X.t[:], func=AF.Square, accum_out=S.t[:, 0:1]), reads=[X.res], writes=[junk.res, S.res])
                rstd_ops(P, S, 0, 1, 1, 1.0 / D)
                P.op("dve", lambda E, X=X, S=S, r=r: E.scalar_tensor_tensor(out=tmp.t[:], in0=X.t[:], scalar=S.t[:, 1:2], in1=G2[r].t[:], op0=ALU.mult, op1=ALU.mult),
                     reads=[X.res, S.res, G2[r].res], writes=[tmp.res])
                P.op("pool", lambda E, H2=H2, r=r: E.tensor_tensor(out=H2.t[:], in0=tmp.t[:], in1=S2m[r].t[:], op=ALU.add), reads=[tmp.res, S2m[r].res], writes=[H2.res])
                for half in range(2):
                    bk = pp.next()

                    def tr(E, H2=H2, bk=bk, half=half):
                        for j in range(8):
                            kc = half * 8 + j
                            ins = E.transpose(out=bf(bk)[:, j * 128:(j + 1) * 128], in_=H2.t[:, kc * 128:(kc + 1) * 128], identity=ident.t[:])
                        return ins
                    P.op("pe", tr, reads=[H2.res, ident.res], writes=[bk.res])
                    if half == 0:
                        P.op("act", lambda E, HT=HT, bk=bk: E.copy(out=HT.t[:, 0:8, :], in_=bf(bk)), reads=[bk.res], awrites=[HT.res])
                    else:
                        P.op("dve", lambda E, HT=HT, bk=bk: E.tensor_copy(out=HT.t[:, 8:16, :], in_=bf(bk)), reads=[bk.res], awrites=[HT.res])
                P.dma("sp", P.gs(6 + i), h2Tbuf[t], HT.t[:], reads=[HT.res], awrites=[R["h2Tbuf"]])
                bk = pp.next()

                def rm(E, bk=bk, HT=HT):
                    for kc in range(16):
                        ins = E.matmul(bk.t[:, 0:20], lhsT=HT.t[:, kc, :], rhs=wr.t[:, kc, :], start=(kc == 0), stop=(kc == 15))
                    return ins
                P.op("pe", rm, reads=[HT.res, wr.res], writes=[bk.res])
                dv = lambda f_, rd, wr_: P.op("dve", f_, reads=rd, writes=wr_)
                dv(lambda E, RT=RT, bk=bk: E.tensor_tensor(out=RT.t[:, 0:20], in0=bk.t[:, 0:20], in1=brs.t[:], op=ALU.add), [bk.res, brs.res], [RT.res])
                dv(lambda E, RT=RT, S=S: E.tensor_reduce(out=S.t[:, 2:3], in_=RT.t[:, 0:4], axis=AX.X, op=ALU.max), [RT.res], [S.res])
                dv(lambda E, S=S: E.tensor_scalar(out=S.t[:, 3:4], in0=S.t[:, 2:3], scalar1=-1.0, scalar2=None, op0=ALU.mult), [S.res], [S.res])
                P.op("act", lambda E, RT=RT, S=S: E.activation(out=RT.t[:, 20:24], in_=RT.t[:, 0:4], func=AF.Exp, bias=S.t[:, 3:4], accum_out=S.t[:, 4:5]),
                     reads=[RT.res, S.res], writes=[RT.res, S.res])
                dv(lambda E, S=S: E.reciprocal(out=S.t[:, 4:5], in_=S.t[:, 4:5]), [S.res], [S.res])
                dv(lambda E, RT=RT, S=S: E.tensor_scalar(out=RT.t[:, 24:28], in0=RT.t[:, 0:4], scalar1=S.t[:, 2:3], scalar2=None, op0=ALU.is_ge), [RT.res, S.res], [RT.res])
                dv(lambda E, RT=RT: E.tensor_scalar(out=RT.t[:, 24:28], in0=RT.t[:, 24:28], scalar1=-1.0, scalar2=1e30, op0=ALU.add, op1=ALU.mult), [RT.res], [RT.res])
                for g in range(4):
                    dv(lambda E, RT=RT, g=g: E.tensor_scalar(out=RT.t[:, 32 + 4 * g:36 + 4 * g], in0=RT.t[:, 4 + 4 * g:8 + 4 * g], scalar1=RT.t[:, 24 + g:25 + g], scalar2=None, op0=ALU.add),
                       [RT.res], [RT.res])
                dv(lambda E, RT=RT, S=S: E.tensor_reduce(out=S.t[:, 5:6], in_=RT.t[:, 32:48], axis=AX.X, op=ALU.max), [RT.res], [S.res])
                dv(lambda E, RT=RT, S=S: E.tensor_scalar(out=RT.t[:, 48:64], in0=RT.t[:, 32:48], scalar1=S.t[:, 5:6], scalar2=None, op0=ALU.is_ge), [RT.res, S.res], [RT.res])
                dv(lambda E, RT=RT: E.scalar_tensor_tensor(out=RT.t[:, 64:80], in0=RT.t[:, 48:64], scalar=-1e30, in1=RT.t[:, 32:48], op0=ALU.mult, op1=ALU.add), [RT.res], [RT.res])
                dv(lambda E, RT=RT, S=S: E.tensor_reduce(out=S.t[:, 6:7], in_=RT.t[:, 64:80], axis=AX.X, op=ALU.max), [RT.res], [S.res])
                dv(lambda E, RT=RT, S=S: E.tensor_scalar(out=RT.t[:, 80:96], in0=RT.t[:, 64:80], scalar1=S.t[:, 6:7], scalar2=None, op0=ALU.is_ge), [RT.res, S.res], [RT.res])
                dv(lambda E, S=S: E.tensor_tensor(out=S.t[:, 7:8], in0=S.t[:, 6:7], in1=S.t[:, 5:6], op=ALU.subtract), [S.res], [S.res])
                P.op("act", lambda E, S=S: E.activation(out=S.t[:, 7:8], in_=S.t[:, 7:8], func=AF.Exp), reads=[S.res], writes=[S.res])
                dv(lambda E, S=S: E.tensor_scalar(out=S.t[:, 6:7], in0=S.t[:, 7:8], scalar1=1.0, scalar2=None, op0=ALU.add), [S.res], [S.res])
                dv(lambda E, S=S: E.reciprocal(out=S.t[:, 6:7], in_=S.t[:, 6:7]), [S.res], [S.res])
                dv(lambda E, S=S: E.tensor_tensor(out=S.t[:, 7:8], in0=S.t[:, 7:8], in1=S.t[:, 6:7], op=ALU.mult), [S.res], [S.res])
                dv(lambda E, RT=RT, S=S: E.tensor_scalar(out=RT.t[:, 96:112], in0=RT.t[:, 48:64], scalar1=S.t[:, 6:7], scalar2=None, op0=ALU.mult), [RT.res, S.res], [RT.res])
                dv(lambda E, RT=RT, S=S: E.scalar_tensor_tensor(out=RT.t[:, 96:112], in0=RT.t[:, 80:96], scalar=S.t[:, 7:8], in1=RT.t[:, 96:112], op0=ALU.mult, op1=ALU.add),
                   [RT.res, S.res], [RT.res])
                dv(lambda E, RT=RT, S=S: E.tensor_scalar(out=RT.t[:, 96:112], in0=RT.t[:, 96:112], scalar1=S.t[:, 4:5], scalar2=None, op0=ALU.mult), [RT.res, S.res], [RT.res])
                P.dma("sp", P.gs(8 + i), combb[t * 128:(t + 1) * 128, :], RT.t[:, 96:112], reads=[RT.res], awrites=[R["combb"]])
            P.barrier()
            P.flush()

    def phaseE(l, tiles, experts=range(NE)):
        groups = []
        rest = list(tiles)
        ng = (len(rest) + 8) // 9
        base, extra = divmod(len(rest), ng)
        for g in range(ng):
            k = base + (1 if g < extra else 0)
            groups.append(rest[:k])
            rest = rest[k:]
        for grp in groups:
            with ExitStack() as ps:
                G = len(grp)
                hT = sb(ps, nc, "hTe", [128, 16, 9 * 128], BF16)
                acc = sb(ps, nc, "acc", [128, 9, D], F32)
                cmb = sb(ps, nc, "cmb", [128, 9, 16], F32)
                w1s = sbn(ps, nc, "w1s", [128, 16, 256], BF16, 2)
                w3s = sbn(ps, nc, "w3s", [128, 16, 256], BF16, 2)
                w2s = sbn(ps, nc, "w2s", [128, 2, D], BF16, 2)
                sg = sbn(ps, nc, "sg", [128, 512], F32, 2)
                aT = sbn(ps, nc, "aT", [128, 2, 512], BF16, 2)
                x1 = sb(ps, nc, "x1e", [128, D], F32)
                gt2 = sbn(ps, nc, "gt2", [128, D], F32, 2)
                for r in range(2):
                    P.dma("sp", P.one(), gt2[r].t[:], modrow(l, r, 5), reads=[R["modbuf"]], writes=[gt2[r].res])
                for j, t in enumerate(grp):
                    P.dma("sp", P.one(), hT.t[:, :, j * 128:(j + 1) * 128], h2Tbuf[t], reads=[R["h2Tbuf"]], awrites=[hT.res])
                    P.dma("sp", P.one(), cmb.t[:, j, :], combb[t * 128:(t + 1) * 128, :], reads=[R["combb"]], awrites=[cmb.res])
                P.op("pool", lambda E, acc=acc: E.memset(acc.t[:], 0.0), writes=[acc.res])
                pz = Pool8(banks[0:4])
                pp = Pool8(banks[4:8])
                blocks = [(a, min(4, G - a)) for a in range(0, G, 4)]
                u = 0
                for e in experts:
                    for q in range(4):
                        i = u % 2
                        u += 1
                        W1, W3, W2 = w1s[i], w3s[i], w2s[i]
                        P.dma("pool", P.gs(0 + i), W1.t[:], w1_d[l, e, :, q * 256:(q + 1) * 256].rearrange("(kc p) n -> p kc n", p=128), writes=[W1.res])
                        P.dma("pool", P.gs(2 + i), W3.t[:], w3_d[l, e, :, q * 256:(q + 1) * 256].rearrange("(kc p) n -> p kc n", p=128), writes=[W3.res])
                        P.dma("pool", P.gs(4 + i), W2.t[:], w2_d[l, e, q * 256:(q + 1) * 256, :].rearrange("(kc p) n -> p kc n", p=128), writes=[W2.res])
                        for bi, (a, nt) in enumerate(blocks):
                            N = nt * 128
                            gb = {}
                            for wi, W in enumerate((W1, W3)):
                                for dc in range(2):
                                    bk = pz.next()

                                    def f(E, bk=bk, W=W, dc=dc, a=a, N=N, hT=hT):
                                        for kc in range(16):
                                            ins = E.matmul(bk.t[:, 0:N], lhsT=W.t[:, kc, dc * 128:(dc + 1) * 128], rhs=hT.t[:, kc, a * 128:a * 128 + N], start=(kc == 0), stop=(kc == 15))
                                        return ins
                                    P.op("pe", f, reads=[W.res, hT.res], writes=[bk.res])
                                    gb[(wi, dc)] = bk
                            AT = aT[bi % 2]
                            for dc in range(2):
                                SG = sg[dc]
                                P.op("act", lambda E, SG=SG, bk=gb[(0, dc)], N=N: E.activation(out=SG.t[:, 0:N], in_=bk.t[:, 0:N], func=AF.Silu), reads=[gb[(0, dc)].res], writes=[SG.res])
                                P.op("dve", lambda E, SG=SG, AT=AT, bk=gb[(1, dc)], N=N, dc=dc: E.tensor_tensor(out=AT.t[:, dc, 0:N], in0=bk.t[:, 0:N], in1=SG.t[:, 0:N], op=ALU.mult),
                                     reads=[gb[(1, dc)].res, SG.res], awrites=[AT.res])
                            for j in range(nt):
                                jt = a + j
                                for nb in range(4):
                                    bk = pp.next()

                                    def fd(E, bk=bk, AT=AT, W2=W2, j=j, nb=nb):
                                        E.matmul(bk.t[:], lhsT=AT.t[:, 0, j * 128:(j + 1) * 128], rhs=W2.t[:, 0, nb * 512:(nb + 1) * 512], start=True, stop=False)
                                        return E.matmul(bk.t[:], lhsT=AT.t[:, 1, j * 128:(j + 1) * 128], rhs=W2.t[:, 1, nb * 512:(nb + 1) * 512], start=False, stop=True)
                                    P.op("pe", fd, reads=[AT.res, W2.res], writes=[bk.res])
                                    P.op("dve", lambda E, bk=bk, jt=jt, nb=nb, e=e, acc=acc, cmb=cmb: E.scalar_tensor_tensor(
                                        out=acc.t[:, jt, nb * 512:(nb + 1) * 512], in0=bk.t[:], scalar=cmb.t[:, jt, e:e + 1], in1=acc.t[:, jt, nb * 512:(nb + 1) * 512],
                                        op0=ALU.mult, op1=ALU.add), reads=[bk.res, cmb.res], awrites=[acc.res])
                for j, t in enumerate(grp):
                    r = 0 if t < NXT else 1
                    P.dma("sp", P.gs(6), x1.t[:], x1buf[t * 128:(t + 1) * 128, :], reads=[R["x1buf"]], writes=[x1.res])
                    P.op("dve", lambda E, j=j, r=r, acc=acc, gt2=gt2: E.tensor_tensor(out=acc.t[:, j, :], in0=acc.t[:, j, :], in1=gt2[r].t[:], op=ALU.mult),
                         reads=[acc.res, gt2[r].res], awrites=[acc.res])
                    P.op("pool", lambda E, j=j, acc=acc, x1=x1: E.tensor_tensor(out=acc.t[:, j, :], in0=acc.t[:, j, :], in1=x1.t[:], op=ALU.add),
                         reads=[acc.res, x1.res], awrites=[acc.res])
                    if t < NXT:
                        dst, dres = (xbuf, R["xbuf"]) if l < DEPTH - 1 else (y_out, R["y"])
                        dst = dst[t * 128:(t + 1) * 128, :]
                    else:
                        dst, dres = cbuf[(t - NXT) * 128:(t - NXT + 1) * 128, :], R["cbuf"]
                    P.dma("sp", P.gs(7 + j), dst, acc.t[:, j, :], reads=[acc.res], awrites=[dres])
                P.barrier()
                P.flush()

    cx.phaseD = phaseD
    cx.phaseE = phaseE

    def phase_copy_out():
        with ExitStack() as ps:
            xt = sbn(ps, nc, "cpx", [128, D], F32, 2)
            ls = [P.gs(20), P.gs(21), P.gs(0), P.gs(1)]
            for t in range(NXT):
                X = xt[t % 2]
                P.dma("sp", ls[t % 2], X.t[:], x_in[t * 128:(t + 1) * 128, :], writes=[X.res])
                P.dma("sp", ls[2 + t % 2], y_out[t * 128:(t + 1) * 128, :], X.t[:], reads=[X.res], awrites=[R["y"]])
            P.barrier()
            P.flush()

    cx.phase_copy_out = phase_copy_out
    cx.phase0 = phase0
    cx.phaseA1 = phaseA1
    cx.P = P
    cx.nc = nc
    cx.gs = gs
    cx.R = R
    cx.y_out = y_out
    return cx


def finish(cx):
    P = cx.P
    P.barrier()
    P.flush()
    cx.gs.close()
    P.es.close()
    return cx.nc


IN_OFF = dict(cq=0, ckv=512, kpe=1024, zu=1088, zv=1600, gq=2112, gk=2368, gv=2624, gr=3136, ggf=3648, ggb=3664)


def _rope_tables():
    rows = SEQ // 64
    row = np.repeat(np.arange(rows), 64).astype(np.float32)
    col = np.tile(np.arange(64), rows).astype(np.float32)
    inv = (10000.0 ** (-np.arange(16, dtype=np.float32) / 16)).astype(np.float32)
    ar = row[:, None] * inv
    ac = col[:, None] * inv
    cos64 = np.concatenate([np.cos(ar), np.cos(ar), np.cos(ac), np.cos(ac)], axis=1)
    sin64 = np.concatenate([-np.sin(ar), np.sin(ar), -np.sin(ac), np.sin(ac)], axis=1)
    cos64 = np.concatenate([cos64, np.ones((CTX, 64), np.float32)], axis=0).astype(np.float32)
    sin64 = np.concatenate([sin64, np.zeros((CTX, 64), np.float32)], axis=0).astype(np.float32)
    tabk = np.concatenate([cos64, sin64], axis=1).reshape(NT, 128, 128)
    tabq = np.concatenate([np.tile(cos64, (1, 8)), np.tile(sin64, (1, 8))], axis=1).reshape(NT, 128, 1024)
    return np.ascontiguousarray(tabk), np.ascontiguousarray(tabq)


def _swap64():
    idx = np.arange(64)
    blk = idx // 32
    j = idx % 32
    return blk * 32 + (j + 16) % 32


def prep_shared(inp):
    sw = _swap64()
    o = IN_OFF
    w_in = inp["w_in"]
    colk = np.concatenate([np.arange(o["ckv"], o["ckv"] + 512), np.arange(o["kpe"], o["kpe"] + 64), o["kpe"] + sw,
                           np.arange(o["gk"], o["gk"] + 256), np.arange(o["ggf"], o["ggf"] + 16), np.arange(o["ggb"], o["ggb"] + 16),
                           np.zeros(96, np.int64), np.arange(o["gv"], o["gv"] + 512)])
    colq = np.concatenate([np.arange(o["cq"], o["cq"] + 512), np.arange(o["gq"], o["gq"] + 256), np.arange(o["gr"], o["gr"] + 512),
                           np.arange(o["zv"], o["zv"] + 512), np.arange(o["zu"], o["zu"] + 512)])
    sh = {}
    sh["wk"] = np.ascontiguousarray(w_in[:, :, colk])
    sh["wq"] = np.ascontiguousarray(w_in[:, :, colq])
    ukv = inp["mla_w_ukv"].reshape(DEPTH, 512, 8, 2, 128)
    sh["wukv"] = np.ascontiguousarray(np.concatenate([ukv[:, :, :, 0, :].reshape(DEPTH, 512, 1024), ukv[:, :, :, 1, :].reshape(DEPTH, 512, 1024)], axis=2))
    uq = inp["mla_w_uq"].reshape(DEPTH, 512, 8, 192)
    sh["wuq"] = np.ascontiguousarray(np.concatenate([uq[..., :128].reshape(DEPTH, 512, 1024), uq[..., 128:].reshape(DEPTH, 512, 512),
                                                     uq[..., 128:][..., sw].reshape(DEPTH, 512, 512)], axis=2))
    sh["qnorm"] = inp["mla_q_norm"]
    sh["kvnorm"] = inp["mla_kv_norm"]
    qg, kg = inp["mla_q_gain"], inp["mla_k_gain"]
    sh["qgn"] = np.ascontiguousarray(qg[:, :128, None])
    sh["kgn"] = np.ascontiguousarray(kg[:, :128, None])
    sh["qgpe"] = np.ascontiguousarray(np.concatenate([np.tile(qg[:, 128:], (1, 8)), np.tile(qg[:, 128:][:, sw], (1, 8))], axis=1))
    sh["kgpe"] = np.ascontiguousarray(np.concatenate([kg[:, 128:], kg[:, 128:][:, sw]], axis=1))
    sh["sgun"] = np.ascontiguousarray(inp["sgu_norm"].reshape(DEPTH, 512))
    sh["wsT"] = np.ascontiguousarray(inp["sgu_w"].transpose(0, 3, 1, 2))
    sh["sgub"] = np.ascontiguousarray(inp["sgu_b"].reshape(DEPTH, 512))
    sh["wgF"] = np.ascontiguousarray(np.concatenate([inp["gla_wg_f"], inp["gla_bg_f"][:, None, :]], axis=1))
    sh["wgB"] = np.ascontiguousarray(np.concatenate([inp["gla_wg_b"], inp["gla_bg_b"][:, None, :]], axis=1))
    sh["glaon"] = np.ascontiguousarray(np.tile(inp["gla_out_norm"], (1, 4)))
    sh["w_out"] = inp["w_out"]
    sh["wr"] = np.ascontiguousarray(np.concatenate([inp["moe_w_group"], inp["moe_w_expert"]], axis=2))
    sh["br"] = np.ascontiguousarray(np.concatenate([inp["moe_b_group"], inp["moe_b_expert"]], axis=1))
    sh["w1"], sh["w3"], sh["w2"] = inp["moe_w1"], inp["moe_w3"], inp["moe_w2"]
    sh["ada_w"], sh["ada_b"] = inp["ada_w"], inp["ada_b"]
    sh["g1"], sh["g2"] = inp["norm1_g"], inp["norm2_g"]
    sh["tabk"], sh["tabq"] = _rope_tables()
    sh["ident"] = np.eye(128, dtype=np.float32)
    s_, t_ = np.meshgrid(np.arange(128), np.arange(128), indexing="ij")
    same = (s_ // 64) == (t_ // 64)
    triF = np.where(same & (s_ <= t_), -1.0 / 16, 0.0)
    triB = np.where(same & (s_ >= t_), -1.0 / 16, 0.0)
    ones2 = np.where(same, -1.0 / 16, 0.0)
    cind = np.stack([np.where(np.arange(128) // 64 == c, -1.0 / 16, 0.0) for c in range(2)], axis=1)
    cm = np.stack([(np.arange(128) // 64 == c).astype(np.float32) for c in range(2)], axis=1)
    sl_ = np.arange(128)[:, None]
    tl_ = np.arange(64)[None, :]
    masks = []
    for dF in (True, False):
        for c in range(2):
            inc = (sl_ // 64 == c) & (((sl_ % 64) <= tl_) if dF else ((sl_ % 64) >= tl_))
            masks.append(np.tile(inc.astype(np.float32), (1, 4)))
    sh["gcon"] = np.ascontiguousarray(np.concatenate([triF, triB, ones2, cind, cm] + masks, axis=1).astype(np.float32))
    return sh


def core_inputs(inp, sh, b):
    m = dict(sh)
    m["x"] = np.ascontiguousarray(inp["x"][b])
    m["ctx"] = np.ascontiguousarray(inp["ctx"][b])
    cv = np.stack([inp["c"][b], inp["c_ctx"]], axis=0)
    m["cT"] = np.ascontiguousarray(cv.reshape(2, 16, 128).transpose(2, 1, 0))
    return m


def build_full():
    cx = build()
    xt = list(range(NXT))
    allt = list(range(NT))
    for l in range(DEPTH):
        ctx_out = l < DEPTH - 1
        full = allt if ctx_out else xt
        cx.phase0(l)
        cx.phaseA1(l, allt)
        cx.phaseA2(l, full)
        cx.phaseB(l, ctx_out)
        cx.phaseC(l, ctx_out)
        cx.phaseD(l, full)
        cx.phaseE(l, full)
    return finish(cx)


def kernel(**inputs):
    inp = {k: np.asarray(v) for k, v in inputs.items()}
    sh = prep_shared(inp)
    nc = build_full()
    in_maps = [core_inputs(inp, sh, b) for b in range(NCORES)]
    res = run_bass_kernel_spmd(nc, in_maps, core_ids=list(range(NCORES)))
    return np.stack([np.asarray(r["y"], dtype=np.float32) for r in res.results], axis=0)
```

```python
from contextlib import ExitStack
import numpy as np
import concourse.bass as bass
import concourse.mybir as mybir
from concourse.bass_utils import run_bass_kernel_spmd

F32, BF16 = mybir.dt.float32, mybir.dt.bfloat16
AF = mybir.ActivationFunctionType
ALU = mybir.AluOpType
AX = mybir.AxisListType

D = 2048
SEQ = 4096
CTX = 256
NXT = SEQ // 128
NCT = CTX // 128
NT = NXT + NCT
NTOK = NT * 128
DEPTH = 2
EPS = 1e-6
MLA_SCALE = 192 ** -0.5
NE = 16
DE = 1024
NCORES = 8


class Sem:
    def __init__(self, h, k):
        self.h, self.k, self.n = h, k, 0


class Res:
    __slots__ = ("w", "r")

    def __init__(self):
        self.w = {}
        self.r = {}


def _merge(dst, src):
    for k, (s, v) in src.items():
        if k not in dst or dst[k][1] < v:
            dst[k] = (s, v)


class Buf:
    def __init__(self, t):
        self.t = t
        self.res = Res()

    def __getitem__(self, key):
        return self.t[key]


class Prog:
    ENG = ("pe", "act", "dve", "pool", "sp")

    def __init__(self, nc):
        self.nc = nc
        self.es = ExitStack()
        self.nsem = 0
        self.streams = {e: [] for e in self.ENG}
        self.esem = {e: self.newsem() for e in self.ENG}
        self.waited = {e: {} for e in self.ENG}
        self.dsems = []
        self.ninstr = 0

    def newsem(self):
        h = self.es.enter_context(self.nc.semaphore(f"sm{self.nsem}"))
        s = Sem(h, self.nsem)
        self.nsem += 1
        return s

    def slot(self):
        s = self.newsem()
        self.dsems.append(s)
        return s

    def gs(self, j):
        if not hasattr(self, "_gs"):
            self._gs = {}
        if j not in self._gs:
            self._gs[j] = self.slot()
        return self._gs[j]

    def one(self):
        if not hasattr(self, "_ones"):
            self._ones = [self.slot() for _ in range(20)]
            self._onei = 0
        s = self._ones[self._onei % 20]
        self._onei += 1
        return s

    def _emit(self, eng, fn, sem, inc, reads, writes, awrites):
        deps = {}
        for r in reads:
            _merge(deps, r.w)
        for w in writes:
            _merge(deps, w.w)
            _merge(deps, w.r)
        for w in awrites:
            _merge(deps, w.r)
        st = self.streams[eng]
        wd = self.waited[eng]
        for k, (s, v) in deps.items():
            if eng == "pe" and s is self.esem["pe"]:
                continue
            if wd.get(k, 0) >= v:
                continue
            wd[k] = v
            st.append(("w", s, v))
        sem.n += inc
        tok = {sem.k: (sem, sem.n)}
        st.append(("i", fn, sem, inc))
        self.ninstr += 1
        for r in reads:
            _merge(r.r, tok)
        for w in writes:
            w.w = dict(tok)
            w.r = {}
        for w in awrites:
            _merge(w.w, tok)
        return tok

    def op(self, eng, fn, reads=(), writes=(), awrites=()):
        if self.esem[eng].n > 12000:
            self.dsems.append(self.esem[eng])
            self.esem[eng] = self.newsem()
        return self._emit(eng, fn, self.esem[eng], 1, reads, writes, awrites)

    def dma(self, eng, slot, out, in_, reads=(), writes=(), awrites=(), **kw):
        return self._emit(eng, lambda E: E.dma_start(out=out, in_=in_, **kw), slot, 16, reads, writes, awrites)

    def barrier(self):
        toks = {}
        for e in self.ENG:
            s = self.esem[e]
            if s.n:
                toks[s.k] = (s, s.n)
        for s in self.dsems:
            if s.n:
                toks[s.k] = (s, s.n)
        for e in self.ENG:
            wd = self.waited[e]
            for k, (s, v) in toks.items():
                if wd.get(k, 0) >= v:
                    continue
                wd[k] = v
                self.streams[e].append(("w", s, v))

    def flush(self):
        nc = self.nc
        streams = self.streams
        self.streams = {e: [] for e in self.ENG}

        def replay(E, st):
            for it in st:
                if it[0] == "w":
                    E.wait_ge(it[1].h, it[2])
                else:
                    it[1](E).then_inc(it[2].h, it[3])

        with nc.Block() as block:
            @block.tensor
            def _(E):
                replay(E, streams["pe"])

            @block.scalar
            def _(E):
                replay(E, streams["act"])

            @block.vector
            def _(E):
                replay(E, streams["dve"])

            @block.gpsimd
            def _(E):
                replay(E, streams["pool"])

            @block.sync
            def _(E):
                replay(E, streams["sp"])


class Pool8:
    def __init__(self, banks):
        self.b = banks
        self.i = 0

    def next(self):
        b = self.b[self.i % len(self.b)]
        self.i += 1
        return b


_UID = [0]


def sb(ps, nc, name, shape, dt):
    _UID[0] += 1
    return Buf(ps.enter_context(nc.sbuf_tensor(f"s{_UID[0]}_{name}", shape, dt)))


def sbn(ps, nc, name, shape, dt, n):
    return [sb(ps, nc, f"{name}{i}", shape, dt) for i in range(n)]


class Ctx:
    pass


def rstd_ops(P, st, ssres_cols, out_cols, n, scale):
    a, b = ssres_cols, out_cols
    P.op("act", lambda E: E.activation(out=st.t[:, b:b + n], in_=st.t[:, a:a + n], func=AF.Ln, scale=scale, bias=EPS),
         reads=[st.res], writes=[st.res])
    P.op("act", lambda E: E.activation(out=st.t[:, b:b + n], in_=st.t[:, b:b + n], func=AF.Exp, scale=-0.5),
         reads=[st.res], writes=[st.res])


def build(nlayers=DEPTH, dbg=(), lite=False, upto=99):
    nc = bass.Bass("TRN2", target_bir_lowering=False)
    P = Prog(nc)
    cx = Ctx()
    L = DEPTH

    def din(name, shape, dt=F32):
        return nc.dram_tensor(name, list(shape), dt, kind="ExternalInput").ap()

    def dscr(name, shape, dt):
        kind = "ExternalOutput" if name in dbg else "Internal"
        return nc.dram_tensor(name, list(shape), dt, kind=kind).ap()

    x_in = din("x", [SEQ, D])
    c_in = din("ctx", [CTX, D])
    cT = din("cT", [128, 16, 2])
    ada_w = din("ada_w", [L, D, 6 * D])
    ada_b = din("ada_b", [L, 6 * D])
    g1 = din("g1", [L, D])
    g2 = din("g2", [L, D])
    wk_d = din("wk", [L, D, 1536])
    wq_d = din("wq", [L, D, 2304])
    wukv_d = din("wukv", [L, 512, 2048])
    wuq_d = din("wuq", [L, 512, 2048])
    qnorm = din("qnorm", [L, 512])
    kvnorm = din("kvnorm", [L, 512])
    qgn = din("qgn", [L, 128, 1])
    kgn = din("kgn", [L, 128, 1])
    qgpe = din("qgpe", [L, 1024])
    kgpe = din("kgpe", [L, 128])
    sgun = din("sgun", [L, 512])
    wsT_d = din("wsT", [L, 128, 4, 128])
    sgub = din("sgub", [L, 512])
    wgF_d = din("wgF", [L, 17, 256])
    wgB_d = din("wgB", [L, 17, 256])
    glaon = din("glaon", [L, 512])
    wout_d = din("w_out", [L, D, D])
    wr_d = din("wr", [L, D, 20])
    br_d = din("br", [L, 20])
    nE = 1 if lite else NE
    w1_d = din("w1", [L, nE, D, DE])
    w3_d = din("w3", [L, nE, D, DE])
    w2_d = din("w2", [L, nE, DE, D])
    tabk = din("tabk", [NT, 128, 128])
    tabq = din("tabq", [NT, 128, 1024])
    ident_d = din("ident", [128, 128])
    gcon = din("gcon", [128, 1412])
    hfm = din("hfm", [128, 2])
    y_out = nc.dram_tensor("y", [SEQ // 2, D], F32, kind="ExternalOutput").ap()

    modbuf = dscr("modbuf", [L, 2, 6 * D], F32)
    xbuf = dscr("xbuf", [SEQ, D], F32)
    cbuf = dscr("cbuf", [CTX, D], F32)
    x1buf = dscr("x1buf", [NTOK, D], F32)
    hTbuf = dscr("hTbuf", [NT, 128, 16, 128], BF16)
    h2Tbuf = dscr("h2Tbuf", [NT, 128, 16, 128], BF16)
    Vb = dscr("Vb", [NTOK, 1024], BF16)
    KnT = dscr("KnT", [NT, 128, 8, 128], BF16)
    KpeT = dscr("KpeT", [128, NTOK], BF16)
    rkb = dscr("rkb", [NTOK, 8], F32)
    QnT = dscr("QnT", [NT, 128, 8, 128], BF16)
    QpeT = dscr("QpeT", [NT, 128, 4, 128], BF16)
    glaKV = dscr("glaKV", [NTOK, 768], BF16)
    glaG = dscr("glaG", [NTOK, 512], F32)
    glaQ = dscr("glaQ", [NTOK, 256], BF16)
    glaR = dscr("glaR", [NT, 128, 4, 128], BF16)
    glaO = dscr("glaO", [NT, 128, 4, 128], F32)
    mixT = dscr("mixT", [NT, 128, 16, 128], BF16)
    combb = dscr("combb", [NTOK, 16], F32)
    mixS = dscr("mixS", [NXT // 2, 128, 16, 128], BF16)
    xS = dscr("xS", [SEQ // 2, D], F32)
    R = {n: Res() for n in ("modbuf", "xbuf", "cbuf", "x1buf", "hTbuf", "h2Tbuf", "Vb", "KnT", "KpeT", "rkb", "QnT", "QpeT",
                            "glaKV", "glaG", "glaQ", "glaR", "glaO", "mixT", "combb", "y", "mixS", "xS")}

    gs = ExitStack()
    banks = []
    for i in range(8):
        banks.append(Buf(gs.enter_context(nc.psum_tensor(f"bank{i}", [128, 512], F32))))
    ident = sb(gs, nc, "ident", [128, 128], BF16)
    ones_bf = sb(gs, nc, "ones_bf", [128, 128], BF16)
    P.dma("pool", P.one(), ident.t[:], ident_d, writes=[ident.res])
    P.op("pool", lambda E: E.memset(ones_bf.t[:], 1.0), writes=[ones_bf.res])

    def bf(bank, n=1024):
        return bank.t[:].bitcast(BF16)[:, 0:n]

    def phase0(l):
        with ExitStack() as ps:
            sil = sb(ps, nc, "sil", [128, 16, 2], F32)
            mod = sb(ps, nc, "mod", [2, 6 * D], F32)
            adab = sb(ps, nc, "adab", [2, 6 * D], F32)
            gg = sb(ps, nc, "gg", [2, 2 * D], F32)
            blk = sbn(ps, nc, "adablk", [128, 16, 512], F32, 2)
            bslot = [P.gs(20), P.gs(21)]
            P.dma("sp", P.one(), sil.t[:], cT, writes=[sil.res])
            P.dma("sp", P.one(), adab.t[:], ada_b[l].partition_broadcast(2), writes=[adab.res])
            P.dma("sp", P.one(), gg.t[:, 0:D], g1[l].partition_broadcast(2), writes=[gg.res])
            P.dma("sp", P.one(), gg.t[:, D:2 * D], g2[l].partition_broadcast(2), writes=[gg.res])
            P.op("act", lambda E: E.activation(out=sil.t[:], in_=sil.t[:], func=AF.Silu), reads=[sil.res], writes=[sil.res])
            pp = Pool8(banks)
            src = ada_w[l].rearrange("(kc p) n -> p kc n", p=128)
            for j in range(24):
                b = blk[j % 2]
                P.dma("sp", bslot[j % 2], b.t[:], src[:, :, j * 512:(j + 1) * 512], writes=[b.res])
                bk = pp.next()

                def mm(E, b=b, bk=bk):
                    for kc in range(16):
                        ins = E.matmul(bk.t[0:2, :], lhsT=sil.t[:, kc, :], rhs=b.t[:, kc, :], start=(kc == 0), stop=(kc == 15))
                    return ins
                P.op("pe", mm, reads=[sil.res, b.res], writes=[bk.res])
                P.op("dve", lambda E, bk=bk, j=j: E.tensor_tensor(out=mod.t[:, j * 512:(j + 1) * 512], in0=bk.t[0:2, :],
                                                                  in1=adab.t[:, j * 512:(j + 1) * 512], op=ALU.add),
                     reads=[bk.res, adab.res], awrites=[mod.res])
            P.op("dve", lambda E: E.scalar_tensor_tensor(out=mod.t[:, D:2 * D], in0=mod.t[:, D:2 * D], scalar=1.0, in1=gg.t[:, 0:D],
                                                          op0=ALU.add, op1=ALU.mult), reads=[mod.res, gg.res], writes=[mod.res])
            P.op("dve", lambda E: E.scalar_tensor_tensor(out=mod.t[:, 4 * D:5 * D], in0=mod.t[:, 4 * D:5 * D], scalar=1.0, in1=gg.t[:, D:2 * D],
                                                          op0=ALU.add, op1=ALU.mult), reads=[mod.res, gg.res], writes=[mod.res])
            P.dma("sp", P.one(), modbuf[l], mod.t[:], reads=[mod.res], awrites=[R["modbuf"]])
            P.barrier()
            P.flush()

    def modrow(l, r, i):
        return modbuf[l, r, i * D:(i + 1) * D].partition_broadcast(128)

    def tile_src(l, t):
        xs = x_in if l == 0 else xbuf
        cs = c_in if l == 0 else cbuf
        if t < NXT:
            return xs[t * 128:(t + 1) * 128, :], (R["xbuf"] if l else None)
        return cs[(t - NXT) * 128:(t - NXT + 1) * 128, :], (R["cbuf"] if l else None)

    def phaseA1(l, tiles):
        with ExitStack() as ps:
            wk = sb(ps, nc, "wk", [128, 16, 1536], BF16)
            wukv = sb(ps, nc, "wukv", [128, 4, 2048], BF16)
            G1 = sbn(ps, nc, "G1_", [128, D], F32, 2)
            S1 = sbn(ps, nc, "S1_", [128, D], F32, 2)
            kvn = sb(ps, nc, "kvn", [128, 512], F32)
            kgn_s = sb(ps, nc, "kgn_s", [128, 1], F32)
            kgpe_s = sb(ps, nc, "kgpe_s", [128, 128], F32)
            wgF = sb(ps, nc, "wgF", [17, 256], BF16)
            wgB = sb(ps, nc, "wgB", [17, 256], BF16)
            xt = sbn(ps, nc, "xt", [128, D], F32, 3)
            tab = sbn(ps, nc, "tab", [128, 128], F32, 2)
            junk = sb(ps, nc, "junk", [128, D], BF16)
            tmp = sbn(ps, nc, "tmp", [128, D], F32, 2)
            hb = sbn(ps, nc, "hb", [128, D], BF16, 2)
            hT = sbn(ps, nc, "hT", [128, 16, 128], BF16, 2)
            st = sbn(ps, nc, "st", [128, 64], F32, 2)
            ckvn = sbn(ps, nc, "ckvn", [128, 512], BF16, 2)
            ckvnT = sbn(ps, nc, "ckvnT", [128, 4, 128], BF16, 2)
            Vt = sbn(ps, nc, "Vt", [128, 1024], BF16, 2)
            knb = sbn(ps, nc, "knb", [128, 1024], BF16, 2)
            KnTt = sbn(ps, nc, "KnTt", [128, 8, 128], BF16, 2)
            rkt = sbn(ps, nc, "rkt", [128, 8], F32, 2)
            rp = sbn(ps, nc, "rp", [128, 192], F32, 2)
            krb = sbn(ps, nc, "krb", [128, 128], BF16, 2)
            KpeTt = sbn(ps, nc, "KpeTt", [128, 128], BF16, 2)
            gkv = sbn(ps, nc, "gkv", [128, 768], BF16, 2)
            ggT = [sbn(ps, nc, "ggTF", [17, 128], BF16, 2), sbn(ps, nc, "ggTB", [17, 128], BF16, 2)]
            ge = sbn(ps, nc, "ge", [128, 512], F32, 2)
            gr_ = sbn(ps, nc, "grr", [128, 512], F32, 2)
            for kq in range(4):
                P.dma("pool", P.one(), wk.t[:, kq * 4:(kq + 1) * 4, :],
                      wk_d[l, kq * 512:(kq + 1) * 512, :].rearrange("(kc p) n -> p kc n", p=128), writes=[wk.res])
            P.dma("pool", P.one(), wukv.t[:], wukv_d[l].rearrange("(kc p) n -> p kc n", p=128), writes=[wukv.res])
            P.dma("pool", P.one(), wgF.t[:], wgF_d[l], writes=[wgF.res])
            P.dma("pool", P.one(), wgB.t[:], wgB_d[l], writes=[wgB.res])
            for r in range(2):
                P.dma("sp", P.one(), G1[r].t[:], modrow(l, r, 1), reads=[R["modbuf"]], writes=[G1[r].res])
                P.dma("sp", P.one(), S1[r].t[:], modrow(l, r, 0), reads=[R["modbuf"]], writes=[S1[r].res])
            P.dma("sp", P.one(), kvn.t[:], kvnorm[l].partition_broadcast(128), writes=[kvn.res])
            P.dma("sp", P.one(), kgn_s.t[:], kgn[l], writes=[kgn_s.res])
            P.dma("sp", P.one(), kgpe_s.t[:], kgpe[l].partition_broadcast(128), writes=[kgpe_s.res])
            for i in range(2):
                for d_ in range(2):
                    P.op("pool", lambda E, t_=ggT[d_][i]: E.memset(t_.t[:], 1.0), writes=[ggT[d_][i].res])
            xsl = [P.gs(20 + j) for j in range(3)]
            tsl = [P.gs(23 + j) for j in range(2)]
            pp = Pool8(banks[4:8])
            pz = Pool8(banks[0:4])
            for n, t in enumerate(tiles):
                i = n % 2
                r = 0 if t < NXT else 1
                X = xt[n % 3]
                src, sres = tile_src(l, t)
                P.dma("sp", xsl[n % 3], X.t[:], src, reads=([sres] if sres else []), writes=[X.res])
                P.dma("sp", tsl[i], tab[i].t[:], tabk[t], writes=[tab[i].res])
                S = st[i]
                P.op("act", lambda E, X=X, S=S: E.activation(out=junk.t[:], in_=X.t[:], func=AF.Square, accum_out=S.t[:, 0:1]),
                     reads=[X.res], writes=[junk.res, S.res])
                rstd_ops(P, S, 0, 1, 1, 1.0 / D)
                T_ = tmp[i]
                P.op("dve", lambda E, X=X, S=S, T_=T_, r=r: E.scalar_tensor_tensor(out=T_.t[:], in0=X.t[:], scalar=S.t[:, 1:2], in1=G1[r].t[:],
                                                                                    op0=ALU.mult, op1=ALU.mult),
                     reads=[X.res, S.res, G1[r].res], writes=[T_.res])
                H = hb[i]
                P.op("pool", lambda E, T_=T_, H=H, r=r: E.tensor_tensor(out=H.t[:], in0=T_.t[:], in1=S1[r].t[:], op=ALU.add),
                     reads=[T_.res, S1[r].res], writes=[H.res])
                HT = hT[i]
                for half in range(2):
                    bk = pp.next()

                    def tr(E, H=H, bk=bk, half=half):
                        for j in range(8):
                            kc = half * 8 + j
                            ins = E.transpose(out=bf(bk)[:, j * 128:(j + 1) * 128], in_=H.t[:, kc * 128:(kc + 1) * 128], identity=ident.t[:])
                        return ins
                    P.op("pe", tr, reads=[H.res, ident.res], writes=[bk.res])
                    eng = "act" if half == 0 else "dve"
                    if eng == "act":
                        P.op("act", lambda E, HT=HT, bk=bk, half=half: E.copy(out=HT.t[:, half * 8:(half + 1) * 8, :], in_=bf(bk)),
                             reads=[bk.res], awrites=[HT.res])
                    else:
                        P.op("dve", lambda E, HT=HT, bk=bk, half=half: E.tensor_copy(out=HT.t[:, half * 8:(half + 1) * 8, :], in_=bf(bk)),
                             reads=[bk.res], awrites=[HT.res])
                P.dma("sp", P.gs(0 + i), hTbuf[t], HT.t[:], reads=[HT.res], awrites=[R["hTbuf"]])
                if upto < 1:
                    continue

                def zmm(c0, c1, M=None):
                    bk = pz.next()

                    def f(E, bk=bk, HT=HT):
                        for kc in range(16):
                            ins = E.matmul(bk.t[:, 0:c1 - c0], lhsT=HT.t[:, kc, :], rhs=wk.t[:, kc, c0:c1], start=(kc == 0), stop=(kc == 15))
                        return ins
                    P.op("pe", f, reads=[HT.res, wk.res], writes=[bk.res])
                    return bk
                b_ckv = zmm(0, 512)
                b_mid = zmm(512, 896)
                b_gv = zmm(1024, 1536)
                b_g = pz.next()

                def gmm(E, bk=b_g, HT=HT):
                    for d_ in range(2):
                        for kc in range(16):
                            ins = E.matmul(bk.t[0:16, d_ * 128:(d_ + 1) * 128], lhsT=wk.t[:, kc, 896 + 16 * d_:912 + 16 * d_], rhs=HT.t[:, kc, :],
                                           start=(kc == 0), stop=(kc == 15))
                    return ins
                P.op("pe", gmm, reads=[HT.res, wk.res], writes=[b_g.res])
                if upto < 2:
                    continue
                P.op("act", lambda E, S=S, bk=b_ckv: E.activation(out=junk.t[:, 0:512], in_=bk.t[:], func=AF.Square, accum_out=S.t[:, 2:3]),
                     reads=[bk.res if False else b_ckv.res], writes=[junk.res, S.res])
                rstd_ops(P, S, 2, 3, 1, 1.0 / 512)
                CK = ckvn[i]
                P.op("dve", lambda E, S=S, bk=b_ckv, CK=CK: E.scalar_tensor_tensor(out=CK.t[:], in0=bk.t[:], scalar=S.t[:, 3:4], in1=kvn.t[:],
                                                                                   op0=ALU.mult, op1=ALU.mult),
                     reads=[b_ckv.res, S.res, kvn.res], writes=[CK.res])
                bk = pp.next()

                def tr2(E, CK=CK, bk=bk):
                    for j in range(4):
                        ins = E.transpose(out=bf(bk)[:, j * 128:(j + 1) * 128], in_=CK.t[:, j * 128:(j + 1) * 128], identity=ident.t[:])
                    return ins
                P.op("pe", tr2, reads=[CK.res, ident.res], writes=[bk.res])
                CT = ckvnT[i]
                P.op("act", lambda E, CT=CT, bk=bk: E.copy(out=CT.t[:], in_=bf(bk, 512)), reads=[bk.res], writes=[CT.res])
                if upto < 3:
                    continue
                kvb = []
                for q4 in range(4):
                    bk = pp.next()

                    def f(E, bk=bk, CT=CT, q4=q4):
                        for kc in range(4):
                            ins = E.matmul(bk.t[:], lhsT=CT.t[:, kc, :], rhs=wukv.t[:, kc, q4 * 512:(q4 + 1) * 512], start=(kc == 0), stop=(kc == 3))
                        return ins
                    P.op("pe", f, reads=[CT.res, wukv.res], writes=[bk.res])
                    kvb.append(bk)
                V = Vt[i]
                P.op("act", lambda E, V=V, bk=kvb[2]: E.copy(out=V.t[:, 0:512], in_=bk.t[:]), reads=[kvb[2].res], awrites=[V.res])
                P.op("dve", lambda E, V=V, bk=kvb[3]: E.tensor_copy(out=V.t[:, 512:1024], in_=bk.t[:]), reads=[kvb[3].res], awrites=[V.res])
                P.dma("sp", P.gs(2 + i), Vb[t * 128:(t + 1) * 128, :], V.t[:], reads=[V.res], awrites=[R["Vb"]])
                if upto < 4:
                    continue
                for h in range(8):
                    P.op("act", lambda E, S=S, bk=kvb[h // 4], h=h: E.activation(out=junk.t[:, 0:128], in_=bk.t[:, (h % 4) * 128:(h % 4 + 1) * 128],
                                                                                func=AF.Square, accum_out=S.t[:, 8 + h:9 + h]),
                         reads=[kvb[h // 4].res], writes=[junk.res], awrites=[S.res])
                P.op("act", lambda E, S=S, bk=b_mid: E.activation(out=junk.t[:, 0:64], in_=bk.t[:, 0:64], func=AF.Square, accum_out=S.t[:, 6:7]),
                     reads=[b_mid.res], writes=[junk.res, S.res])
                P.op("dve", lambda E, S=S: E.tensor_scalar(out=S.t[:, 8:16], in0=S.t[:, 8:16], scalar1=S.t[:, 6:7], scalar2=None, op0=ALU.add),
                     reads=[S.res], writes=[S.res])
                rstd_ops(P, S, 8, 16, 8, 1.0 / 192)
                RK = rkt[i]
                P.op("dve", lambda E, S=S, RK=RK: E.tensor_scalar(out=RK.t[:], in0=S.t[:, 16:24], scalar1=MLA_SCALE, scalar2=None, op0=ALU.mult),
                     reads=[S.res], writes=[RK.res])
                P.dma("sp", P.gs(4 + i), rkb[t * 128:(t + 1) * 128, :], RK.t[:], reads=[RK.res], awrites=[R["rkb"]])
                if upto < 5:
                    continue
                KB = knb[i]
                P.op("dve", lambda E, KB=KB, bk=kvb[0]: E.tensor_copy(out=KB.t[:, 0:512], in_=bk.t[:]), reads=[kvb[0].res], awrites=[KB.res])
                P.op("act", lambda E, KB=KB, bk=kvb[1]: E.copy(out=KB.t[:, 512:1024], in_=bk.t[:]), reads=[kvb[1].res], awrites=[KB.res])
                bk = pp.next()

                def tr3(E, KB=KB, bk=bk):
                    for h in range(8):
                        ins = E.transpose(out=bf(bk)[:, h * 128:(h + 1) * 128], in_=KB.t[:, h * 128:(h + 1) * 128], identity=ident.t[:])
                    return ins
                P.op("pe", tr3, reads=[KB.res, ident.res], writes=[bk.res])
                if upto < 5.1:
                    continue
                KT = KnTt[i]
                P.op("dve", lambda E, KT=KT, bk=bk: E.tensor_scalar(out=KT.t[:].rearrange("p h t -> p (h t)"), in0=bf(bk), scalar1=kgn_s.t[:, 0:1],
                                                                     scalar2=None, op0=ALU.mult),
                     reads=[bk.res, kgn_s.res], writes=[KT.res])
                if upto < 5.2:
                    continue
                P.dma("sp", P.gs(6 + i), KnT[t], KT.t[:], reads=[KT.res], awrites=[R["KnT"]])
                if upto < 6:
                    continue
                RP = rp[i]
                TB = tab[i]
                P.op("dve", lambda E, RP=RP, TB=TB, bk=b_mid: E.tensor_tensor(out=RP.t[:, 0:128], in0=bk.t[:, 0:128], in1=TB.t[:], op=ALU.mult),
                     reads=[b_mid.res, TB.res], writes=[RP.res])
                P.op("dve", lambda E, RP=RP: E.tensor_tensor(out=RP.t[:, 0:128], in0=RP.t[:, 0:128], in1=kgpe_s.t[:], op=ALU.mult),
                     reads=[RP.res, kgpe_s.res], writes=[RP.res])
                KR = krb[i]
                for hh in range(2):
                    P.op("dve", lambda E, RP=RP, KR=KR, hh=hh: E.tensor_tensor(out=KR.t[:, hh * 64:(hh + 1) * 64], in0=RP.t[:, 0:64], in1=RP.t[:, 64:128], op=ALU.add),
                         reads=[RP.res], awrites=[KR.res])
                bk = pp.next()
                P.op("pe", lambda E, KR=KR, bk=bk: E.transpose(out=bf(bk)[:, 0:128], in_=KR.t[:], identity=ident.t[:]),
                     reads=[KR.res, ident.res], writes=[bk.res])
                KP = KpeTt[i]
                P.op("act", lambda E, KP=KP, bk=bk: E.copy(out=KP.t[:], in_=bf(bk)[:, 0:128]), reads=[bk.res], writes=[KP.res])
                P.dma("sp", P.gs(8 + i), KpeT[:, t * 128:(t + 1) * 128], KP.t[:], reads=[KP.res], awrites=[R["KpeT"]])
                if upto < 7:
                    continue
                GK = gkv[i]
                P.op("dve", lambda E, GK=GK, bk=b_mid: E.tensor_copy(out=GK.t[:, 0:256], in_=bk.t[:, 128:384]), reads=[b_mid.res], awrites=[GK.res])
                P.op("act", lambda E, GK=GK, bk=b_gv: E.copy(out=GK.t[:, 256:768], in_=bk.t[:]), reads=[b_gv.res], awrites=[GK.res])
                P.dma("sp", P.gs(10 + i), glaKV[t * 128:(t + 1) * 128, :], GK.t[:], reads=[GK.res], awrites=[R["glaKV"]])
                if upto < 8:
                    continue
                for d_ in range(2):
                    GT = ggT[d_][i]
                    P.op("act", lambda E, GT=GT, d_=d_, bk=b_g: E.copy(out=GT.t[0:16, :], in_=bk.t[0:16, d_ * 128:(d_ + 1) * 128]),
                         reads=[b_g.res], awrites=[GT.res])
                bk = pp.next()

                def lg(E, bk=bk, i=i):
                    E.matmul(bk.t[:, 0:256], lhsT=ggT[0][i].t[:], rhs=wgF.t[:], start=True, stop=True)
                    return E.matmul(bk.t[:, 256:512], lhsT=ggT[1][i].t[:], rhs=wgB.t[:], start=True, stop=True)
                P.op("pe", lg, reads=[ggT[0][i].res, ggT[1][i].res, wgF.res, wgB.res], writes=[bk.res])
                GE = ge[i]
                P.op("act", lambda E, GE=GE, bk=bk: E.activation(out=GE.t[:], in_=bk.t[:], func=AF.Exp, scale=-1.0), reads=[bk.res], writes=[GE.res])
                P.op("dve", lambda E, GE=GE: E.tensor_scalar(out=GE.t[:], in0=GE.t[:], scalar1=1.0, scalar2=None, op0=ALU.add),
                     reads=[GE.res], writes=[GE.res])
                GR = gr_[i]
                P.op("act", lambda E, GE=GE, GR=GR: E.activation(out=GR.t[:], in_=GE.t[:], func=AF.Ln), reads=[GE.res], writes=[GR.res])
                P.dma("sp", P.gs(12 + i), glaG[t * 128:(t + 1) * 128, :], GR.t[:], reads=[GR.res], awrites=[R["glaG"]])
            P.barrier()
            P.flush()


    def phaseA2(l, tiles):
        with ExitStack() as ps:
            wq = sb(ps, nc, "wq", [128, 16, 2304], BF16)
            wuq = sb(ps, nc, "wuq", [128, 4, 2048], BF16)
            qnb = sb(ps, nc, "qnb", [128, 512], F32)
            qgn_s = sb(ps, nc, "qgn_s", [128, 1], F32)
            qgpe_s = sb(ps, nc, "qgpe_s", [128, 1024], F32)
            sgun_s = sb(ps, nc, "sgun_s", [128, 512], F32)
            sgub_s = sb(ps, nc, "sgub_s", [128, 512], F32)
            wsT = sb(ps, nc, "wsT", [128, 4, 128], BF16)
            hT = sbn(ps, nc, "hTq", [128, 16, 128], BF16, 2)
            tq = sbn(ps, nc, "tq", [128, 1024], F32, 2)
            junk = sb(ps, nc, "junkq", [128, 512], BF16)
            st = sbn(ps, nc, "stq", [128, 64], F32, 2)
            cqn = sbn(ps, nc, "cqn", [128, 512], BF16, 2)
            cqnT = sbn(ps, nc, "cqnT", [128, 4, 128], BF16, 2)
            qn16 = sbn(ps, nc, "qn16", [128, 1024], BF16, 2)
            QnTt = sbn(ps, nc, "QnTt", [128, 8, 128], BF16, 2)
            rq = sbn(ps, nc, "rq", [128, 1024], F32, 2)
            qpb = sbn(ps, nc, "qpb", [128, 512], BF16, 2)
            QpeTt = sbn(ps, nc, "QpeTt", [128, 4, 128], BF16, 2)
            gqb = sbn(ps, nc, "gqb", [128, 256], BF16, 2)
            grb = sbn(ps, nc, "grb", [128, 512], BF16, 2)
            gvv = sbn(ps, nc, "gvv", [128, 512], F32, 2)
            vn = sbn(ps, nc, "vn", [128, 512], BF16, 2)
            uT = sbn(ps, nc, "uT", [128, 512], F32, 2)
            t2 = sbn(ps, nc, "t2", [128, 512], F32, 2)
            bxT = sbn(ps, nc, "bxT", [128, 4, 128], BF16, 2)
            for kq in range(4):
                P.dma("pool", P.one(), wq.t[:, kq * 4:(kq + 1) * 4, :],
                      wq_d[l, kq * 512:(kq + 1) * 512, :].rearrange("(kc p) n -> p kc n", p=128), writes=[wq.res])
            P.dma("pool", P.one(), wuq.t[:], wuq_d[l].rearrange("(kc p) n -> p kc n", p=128), writes=[wuq.res])
            P.dma("pool", P.one(), wsT.t[:], wsT_d[l], writes=[wsT.res])
            P.dma("sp", P.one(), qnb.t[:], qnorm[l].partition_broadcast(128), writes=[qnb.res])
            P.dma("sp", P.one(), qgn_s.t[:], qgn[l], writes=[qgn_s.res])
            P.dma("sp", P.one(), qgpe_s.t[:], qgpe[l].partition_broadcast(128), writes=[qgpe_s.res])
            P.dma("sp", P.one(), sgun_s.t[:], sgun[l].partition_broadcast(128), writes=[sgun_s.res])
            P.dma("sp", P.one(), sgub_s.t[:], sgub[l].partition_broadcast(128), writes=[sgub_s.res])
            hsl = [P.gs(20 + j) for j in range(2)]
            tsl = [P.gs(23 + j) for j in range(2)]
            pp = Pool8(banks[4:8])
            pz = Pool8(banks[0:4])
            for n, t in enumerate(tiles):
                i = n % 2
                HT = hT[i]
                TQ = tq[i]
                S = st[i]
                P.dma("sp", hsl[i], HT.t[:], hTbuf[t], reads=[R["hTbuf"]], writes=[HT.res])
                P.dma("sp", tsl[i], TQ.t[:], tabq[t], writes=[TQ.res])

                def zmm(c0, c1):
                    bk = pz.next()

                    def f(E, bk=bk, HT=HT):
                        for kc in range(16):
                            ins = E.matmul(bk.t[:, 0:c1 - c0], lhsT=HT.t[:, kc, :], rhs=wq.t[:, kc, c0:c1], start=(kc == 0), stop=(kc == 15))
                        return ins
                    P.op("pe", f, reads=[HT.res, wq.res], writes=[bk.res])
                    return bk
                b_cq = zmm(0, 512)
                P.op("act", lambda E, S=S, bk=b_cq: E.activation(out=junk.t[:], in_=bk.t[:], func=AF.Square, accum_out=S.t[:, 0:1]),
                     reads=[b_cq.res], writes=[junk.res, S.res])
                rstd_ops(P, S, 0, 1, 1, 1.0 / 512)
                CQ = cqn[i]
                P.op("dve", lambda E, S=S, bk=b_cq, CQ=CQ: E.scalar_tensor_tensor(out=CQ.t[:], in0=bk.t[:], scalar=S.t[:, 1:2], in1=qnb.t[:],
                                                                                   op0=ALU.mult, op1=ALU.mult),
                     reads=[b_cq.res, S.res, qnb.res], writes=[CQ.res])
                bk = pp.next()

                def tr2(E, CQ=CQ, bk=bk):
                    for j in range(4):
                        ins = E.transpose(out=bf(bk)[:, j * 128:(j + 1) * 128], in_=CQ.t[:, j * 128:(j + 1) * 128], identity=ident.t[:])
                    return ins
                P.op("pe", tr2, reads=[CQ.res, ident.res], writes=[bk.res])
                CT = cqnT[i]
                P.op("act", lambda E, CT=CT, bk=bk: E.copy(out=CT.t[:], in_=bf(bk, 512)), reads=[bk.res], writes=[CT.res])
                qb = []
                for q4 in range(4):
                    bk = pp.next()

                    def f(E, bk=bk, CT=CT, q4=q4):
                        for kc in range(4):
                            ins = E.matmul(bk.t[:], lhsT=CT.t[:, kc, :], rhs=wuq.t[:, kc, q4 * 512:(q4 + 1) * 512], start=(kc == 0), stop=(kc == 3))
                        return ins
                    P.op("pe", f, reads=[CT.res, wuq.res], writes=[bk.res])
                    qb.append(bk)
                for h in range(8):
                    P.op("act", lambda E, S=S, bk=qb[h // 4], h=h: E.activation(out=junk.t[:, 0:128], in_=bk.t[:, (h % 4) * 128:(h % 4 + 1) * 128],
                                                                               func=AF.Square, accum_out=S.t[:, 8 + h:9 + h]),
                         reads=[qb[h // 4].res], writes=[junk.res], awrites=[S.res])
                    P.op("act", lambda E, S=S, bk=qb[2], h=h: E.activation(out=junk.t[:, 0:64], in_=bk.t[:, h * 64:(h + 1) * 64],
                                                                          func=AF.Square, accum_out=S.t[:, 16 + h:17 + h]),
                         reads=[qb[2].res], writes=[junk.res], awrites=[S.res])
                P.op("dve", lambda E, S=S: E.tensor_tensor(out=S.t[:, 8:16], in0=S.t[:, 8:16], in1=S.t[:, 16:24], op=ALU.add),
                     reads=[S.res], writes=[S.res])
                rstd_ops(P, S, 8, 24, 8, 1.0 / 192)
                QN = qn16[i]
                for h in range(8):
                    P.op("dve", lambda E, S=S, QN=QN, bk=qb[h // 4], h=h: E.tensor_scalar(
                        out=QN.t[:, h * 128:(h + 1) * 128], in0=bk.t[:, (h % 4) * 128:(h % 4 + 1) * 128], scalar1=S.t[:, 24 + h:25 + h],
                        scalar2=None, op0=ALU.mult), reads=[qb[h // 4].res, S.res], awrites=[QN.res])
                RQ = rq[i]
                P.op("dve", lambda E, RQ=RQ, TQ=TQ, bk=qb[2]: E.tensor_tensor(out=RQ.t[:, 0:512], in0=bk.t[:], in1=TQ.t[:, 0:512], op=ALU.mult),
                     reads=[qb[2].res, TQ.res], awrites=[RQ.res])
                P.op("dve", lambda E, RQ=RQ, TQ=TQ, bk=qb[3]: E.tensor_tensor(out=RQ.t[:, 512:1024], in0=bk.t[:], in1=TQ.t[:, 512:1024], op=ALU.mult),
                     reads=[qb[3].res, TQ.res], awrites=[RQ.res])
                P.op("pool", lambda E, RQ=RQ: E.tensor_tensor(out=RQ.t[:], in0=RQ.t[:], in1=qgpe_s.t[:], op=ALU.mult),
                     reads=[RQ.res, qgpe_s.res], writes=[RQ.res])
                P.op("pool", lambda E, RQ=RQ: E.tensor_tensor(out=RQ.t[:, 0:512], in0=RQ.t[:, 0:512], in1=RQ.t[:, 512:1024], op=ALU.add),
                     reads=[RQ.res], writes=[RQ.res])
                QP = qpb[i]
                for h in range(8):
                    P.op("dve", lambda E, S=S, QP=QP, RQ=RQ, h=h: E.tensor_scalar(
                        out=QP.t[:, h * 64:(h + 1) * 64], in0=RQ.t[:, h * 64:(h + 1) * 64], scalar1=S.t[:, 24 + h:25 + h],
                        scalar2=None, op0=ALU.mult), reads=[RQ.res, S.res], awrites=[QP.res])
                bk = pp.next()

                def tr3(E, QN=QN, bk=bk):
                    for h in range(8):
                        ins = E.transpose(out=bf(bk)[:, h * 128:(h + 1) * 128], in_=QN.t[:, h * 128:(h + 1) * 128], identity=ident.t[:])
                    return ins
                P.op("pe", tr3, reads=[QN.res, ident.res], writes=[bk.res])
                QT = QnTt[i]
                P.op("dve", lambda E, QT=QT, bk=bk: E.tensor_scalar(out=QT.t[:].rearrange("p h t -> p (h t)"), in0=bf(bk), scalar1=qgn_s.t[:, 0:1],
                                                                     scalar2=None, op0=ALU.mult),
                     reads=[bk.res, qgn_s.res], writes=[QT.res])
                P.dma("sp", P.gs(0 + i), QnT[t], QT.t[:], reads=[QT.res], awrites=[R["QnT"]])
                bk = pp.next()

                def tr4(E, QP=QP, bk=bk):
                    for j in range(4):
                        ins = E.transpose(out=bf(bk)[:, j * 128:(j + 1) * 128], in_=QP.t[:, j * 128:(j + 1) * 128], identity=ident.t[:])
                    return ins
                P.op("pe", tr4, reads=[QP.res, ident.res], writes=[bk.res])
                QPT = QpeTt[i]
                P.op("act", lambda E, QPT=QPT, bk=bk: E.copy(out=QPT.t[:], in_=bf(bk, 512)), reads=[bk.res], writes=[QPT.res])
                P.dma("sp", P.gs(2 + i), QpeT[t], QPT.t[:], reads=[QPT.res], awrites=[R["QpeT"]])
                b_gq = zmm(512, 768)
                GQ = gqb[i]
                P.op("act", lambda E, GQ=GQ, bk=b_gq: E.activation(out=GQ.t[:], in_=bk.t[:, 0:256], func=AF.Copy, scale=0.125),
                     reads=[b_gq.res], writes=[GQ.res])
                P.dma("sp", P.gs(4 + i), glaQ[t * 128:(t + 1) * 128, :], GQ.t[:], reads=[GQ.res], awrites=[R["glaQ"]])
                b_gr = pz.next()

                def grT(E, bk=b_gr, HT=HT):
                    for g in range(4):
                        for kc in range(16):
                            ins = E.matmul(bk.t[:, g * 128:(g + 1) * 128], lhsT=wq.t[:, kc, 768 + g * 128:768 + (g + 1) * 128], rhs=HT.t[:, kc, :],
                                           start=(kc == 0), stop=(kc == 15))
                    return ins
                P.op("pe", grT, reads=[HT.res, wq.res], writes=[b_gr.res])
                GR = grb[i]
                P.op("act", lambda E, GR=GR, bk=b_gr: E.activation(out=GR.t[:], in_=bk.t[:], func=AF.Silu), reads=[b_gr.res], writes=[GR.res])
                P.dma("sp", P.gs(6 + i), glaR[t], GR.t[:].rearrange("p (g k) -> p g k", g=4), reads=[GR.res], awrites=[R["glaR"]])
                b_zv = zmm(1280, 1792)
                GV = gvv[i]
                P.op("act", lambda E, GV=GV, bk=b_zv: E.activation(out=GV.t[:], in_=bk.t[:], func=AF.Gelu_apprx_tanh), reads=[b_zv.res], writes=[GV.res])
                for g in range(4):
                    P.op("act", lambda E, S=S, GV=GV, g=g: E.activation(out=junk.t[:, 0:128], in_=GV.t[:, g * 128:(g + 1) * 128], func=AF.Square,
                                                                        accum_out=S.t[:, 32 + g:33 + g]),
                         reads=[GV.res], writes=[junk.res], awrites=[S.res])
                rstd_ops(P, S, 32, 36, 4, 1.0 / 128)
                for g in range(4):
                    P.op("dve", lambda E, S=S, GV=GV, g=g: E.tensor_scalar(out=GV.t[:, g * 128:(g + 1) * 128], in0=GV.t[:, g * 128:(g + 1) * 128],
                                                                           scalar1=S.t[:, 36 + g:37 + g], scalar2=None, op0=ALU.mult),
                         reads=[GV.res, S.res], writes=[GV.res])
                VN = vn[i]
                P.op("pool", lambda E, VN=VN, GV=GV: E.tensor_tensor(out=VN.t[:], in0=GV.t[:], in1=sgun_s.t[:], op=ALU.mult),
                     reads=[GV.res, sgun_s.res], writes=[VN.res])
                b_zu = pz.next()

                def zu(E, bk=b_zu, HT=HT):
                    for g in range(4):
                        for kc in range(16):
                            ins = E.matmul(bk.t[:, g * 128:(g + 1) * 128], lhsT=wq.t[:, kc, 1792 + g * 128:1792 + (g + 1) * 128], rhs=HT.t[:, kc, :],
                                           start=(kc == 0), stop=(kc == 15))
                    return ins
                P.op("pe", zu, reads=[HT.res, wq.res], writes=[b_zu.res])
                U = uT[i]
                P.op("act", lambda E, U=U, bk=b_zu: E.activation(out=U.t[:], in_=bk.t[:], func=AF.Gelu_apprx_tanh), reads=[b_zu.res], writes=[U.res])
                bk = pp.next()

                def sg(E, bk=bk, VN=VN):
                    for g in range(4):
                        ins = E.matmul(bk.t[:, g * 128:(g + 1) * 128], lhsT=VN.t[:, g * 128:(g + 1) * 128], rhs=wsT.t[:, g, :], start=True, stop=True)
                    return ins
                P.op("pe", sg, reads=[VN.res, wsT.res], writes=[bk.res])
                T2 = t2[i]
                P.op("dve", lambda E, T2=T2, bk=bk: E.tensor_tensor(out=T2.t[:], in0=bk.t[:], in1=sgub_s.t[:], op=ALU.add),
                     reads=[bk.res, sgub_s.res], writes=[T2.res])
                BX = bxT[i]
                P.op("pool", lambda E, T2=T2, U=U, BX=BX: E.tensor_tensor(out=BX.t[:].rearrange("p g t -> p (g t)"), in0=T2.t[:], in1=U.t[:], op=ALU.mult),
                     reads=[T2.res, U.res], writes=[BX.res])
                P.dma("sp", P.gs(8 + i), mixT[t, :, 8:12, :], BX.t[:], reads=[BX.res], awrites=[R["mixT"]])
            P.barrier()
            P.flush()

    cx.phaseA2 = phaseA2


    def phaseB(l, with_ctx, heads=range(8), qblocks=None):
        with ExitStack() as ps:
            Kn = sbn(ps, nc, "Kn", [128, NT, 128], BF16, 2)
            Vh = sbn(ps, nc, "Vh", [128, NT, 128], BF16, 2)
            Qn = sbn(ps, nc, "Qn", [128, NT, 128], BF16, 2)
            Qp = sbn(ps, nc, "Qp", [128, NT, 128], BF16, 2)
            KpeEO = sbn(ps, nc, "KpeEO", [128, NTOK], BF16, 2)
            rk = sb(ps, nc, "rk", [128, NT, 8], F32)
            pT = sbn(ps, nc, "pT", [128, 512], BF16, 3)
            rden = sbn(ps, nc, "rden", [128, 512], F32, 2)
            o16 = sbn(ps, nc, "o16", [128, 4, 128], BF16, 2)
            P.op("pool", lambda E: E.memset(KpeEO[0].t[64:128, :], 0.0), awrites=[KpeEO[0].res])
            P.op("pool", lambda E: E.memset(KpeEO[1].t[0:64, :], 0.0), awrites=[KpeEO[1].res])
            P.dma("sp", P.one(), KpeEO[0].t[0:64, :], KpeT[0:64, :], reads=[R["KpeT"]], awrites=[KpeEO[0].res])
            P.dma("sp", P.one(), KpeEO[1].t[64:128, :], KpeT[64:128, :], reads=[R["KpeT"]], awrites=[KpeEO[1].res])
            P.dma("sp", P.one(), rk.t[:], rkb.rearrange("(t p) h -> p t h", p=128), reads=[R["rkb"]], writes=[rk.res])
            KnS = KnT.rearrange("t d h k -> d t h k")
            QnS = QnT.rearrange("t d h k -> d t h k")
            QpS = QpeT.rearrange("t p j k -> p t j k")
            VS = Vb.rearrange("(t p) c -> p t c", p=128)
            heads = list(heads)

            def loads(h):
                s_ = h % 2
                for a, b in ((0, 17), (17, NT)):
                    P.dma("sp", P.gs(0 + s_), Kn[s_].t[:, a:b, :], KnS[:, a:b, h, :], reads=[R["KnT"]], awrites=[Kn[s_].res])
                    P.dma("sp", P.gs(2 + s_), Vh[s_].t[:, a:b, :], VS[:, a:b, h * 128:(h + 1) * 128], reads=[R["Vb"]], awrites=[Vh[s_].res])
                    P.dma("sp", P.gs(4 + s_), Qn[s_].t[:, a:b, :], QnS[:, a:b, h, :], reads=[R["QnT"]], awrites=[Qn[s_].res])
                    P.dma("sp", P.gs(6 + s_), Qp[s_].t[:, a:b, :], QpS[:, a:b, h // 2, :], reads=[R["QpeT"]], awrites=[Qp[s_].res])
            spool = Pool8(banks[0:4])
            cnt = [0]
            loads(heads[0])
            for hi, h in enumerate(heads):
                if hi + 1 < len(heads):
                    loads(heads[hi + 1])
                s_ = h % 2
                hp = h % 2
                KN, VH, QN, QP = Kn[s_], Vh[s_], Qn[s_], Qp[s_]
                blocks = [(q0, 4, list(range(NT))) for q0 in range(0, NXT, 4)]
                if with_ctx:
                    blocks.append((NXT, 2, [NXT, NXT + 1]))
                if qblocks is not None:
                    blocks = [blocks[j] for j in qblocks]
                for (q0, nq, keys) in blocks:
                    N = nq * 128
                    c = cnt[0]
                    cnt[0] += 2
                    ob, db = banks[4 + c % 2], banks[6 + c % 2]
                    qn_ap = QN.t[:, q0:q0 + nq, :].rearrange("p a b -> p (a b)")
                    qp_ap = QP.t[:, q0:q0 + nq, :].rearrange("p a b -> p (a b)")
                    Kpe = KpeEO[hp]
                    sb_ = {}

                    def score(j, N=N, KN=KN, QN=QN, QP=QP, Kpe=Kpe, qn_ap=qn_ap, qp_ap=qp_ap, keys=keys, sb_=sb_):
                        kt = keys[j]
                        bk = spool.next()

                        def f(E, bk=bk, kt=kt, N=N, KN=KN, Kpe=Kpe, qn_ap=qn_ap, qp_ap=qp_ap):
                            E.matmul(bk.t[:, 0:N], lhsT=KN.t[:, kt, :], rhs=qn_ap, start=True, stop=False)
                            return E.matmul(bk.t[:, 0:N], lhsT=Kpe.t[:, kt * 128:(kt + 1) * 128], rhs=qp_ap, start=False, stop=True)
                        P.op("pe", f, reads=[KN.res, QN.res, QP.res, Kpe.res], writes=[bk.res])
                        sb_[j] = bk
                    nk = len(keys)
                    score(0)
                    if nk > 1:
                        score(1)
                    for j in range(nk):
                        kt = keys[j]
                        bk = sb_.pop(j)
                        PT = pT[j % 3]
                        P.op("act", lambda E, PT=PT, bk=bk, kt=kt, N=N, h=h: E.activation(out=PT.t[:, 0:N], in_=bk.t[:, 0:N], func=AF.Exp,
                                                                                       scale=rk.t[:, kt, h:h + 1]),
                             reads=[bk.res, rk.res], writes=[PT.res])
                        if j + 2 < nk:
                            score(j + 2)

                        def acc(E, PT=PT, kt=kt, j=j, N=N, nk=nk, ob=ob, db=db, VH=VH):
                            E.matmul(ob.t[:, 0:N], lhsT=VH.t[:, kt, :], rhs=PT.t[:, 0:N], start=(j == 0), stop=(j == nk - 1))
                            return E.matmul(db.t[:, 0:N], lhsT=ones_bf.t[:], rhs=PT.t[:, 0:N], start=(j == 0), stop=(j == nk - 1))
                        P.op("pe", acc, reads=[VH.res, PT.res, ones_bf.res], writes=[ob.res, db.res])
                    RD = rden[c % 2]
                    P.op("dve", lambda E, RD=RD, db=db, N=N: E.reciprocal(out=RD.t[:, 0:N], in_=db.t[:, 0:N]), reads=[db.res], writes=[RD.res])
                    O = o16[c % 2]
                    P.op("dve", lambda E, RD=RD, O=O, ob=ob, N=N, nq=nq: E.tensor_tensor(out=O.t[:, 0:nq, :].rearrange("p a b -> p (a b)"), in0=ob.t[:, 0:N],
                                                                                      in1=RD.t[:, 0:N], op=ALU.mult),
                         reads=[ob.res, RD.res], writes=[O.res])
                    P.dma("sp", P.gs(8 + c % 2), mixT[q0:q0 + nq, :, h, :].rearrange("t p k -> p t k"), O.t[:, 0:nq, :], reads=[O.res], awrites=[R["mixT"]])
            P.barrier()
            P.flush()

    cx.phaseB = phaseB


    def phaseC(l, ctx_out, xtiles=None, cstop=99):
        with ExitStack() as ps:
            gc = sb(ps, nc, "gc", [128, 1412], F32)
            mk = sb(ps, nc, "mk", [128, 4, 256], BF16)
            gcb = sb(ps, nc, "gcb", [128, 386], BF16)
            rh = sbn(ps, nc, "rh", [128, 256], BF16, 2)
            tdg = sbn(ps, nc, "tdg", [128, 128], F32, 2)
            rl = sbn(ps, nc, "rl", [128, 256], BF16, 2)
            gon = sb(ps, nc, "gon", [128, 1], F32)
            S2 = sb(ps, nc, "S2", [128, 2, 128], F32)
            Sb = sb(ps, nc, "Sb", [128, 2, 128], BF16)
            rr = sbn(ps, nc, "rr", [128, 256], F32, 2)
            kv = sbn(ps, nc, "kvg", [128, 768], BF16, 2)
            qq = sbn(ps, nc, "qq", [128, 256], BF16, 2)
            eb = sbn(ps, nc, "eb", [128, 256], F32, 2)
            enb = sbn(ps, nc, "enb", [128, 256], F32, 2)
            ebt = sbn(ps, nc, "ebt", [128, 256], F32, 2)
            edT = sbn(ps, nc, "edT", [128, 4], F32, 2)
            qe = sbn(ps, nc, "qe", [128, 256], BF16, 2)
            ke = sbn(ps, nc, "ke", [128, 256], BF16, 2)
            kw = sbn(ps, nc, "kw", [128, 256], F32, 2)
            kwz = sbn(ps, nc, "kwz", [128, 2, 256], BF16, 2)
            qeEO = sbn(ps, nc, "qeEO", [128, 2, 2, 128], BF16, 2)
            keT = sbn(ps, nc, "keT", [128, 2, 128], BF16, 2)
            aTm = sbn(ps, nc, "aTm", [128, 256], BF16, 2)
            oF = sbn(ps, nc, "oF", [128, 4, 128], F32, 2)
            oS = sbn(ps, nc, "oS", [128, 4, 128], F32, 2)
            sq = sbn(ps, nc, "sqo", [128, 512], BF16, 2)
            rs = sbn(ps, nc, "rso", [128, 512], F32, 2)
            srT = sbn(ps, nc, "srT", [128, 4, 128], BF16, 2)
            cxT = sbn(ps, nc, "cxT", [128, 4, 128], BF16, 2)
            P.dma("sp", P.one(), gc.t[:], gcon, writes=[gc.res])
            P.dma("pool", P.one(), mk.t[:].rearrange("p a b -> p (a b)"), gcon[:, 388:1412], writes=[mk.res])
            P.dma("pool", P.one(), gcb.t[:], gcon[:, 0:386], writes=[gcb.res])
            P.dma("sp", P.one(), gon.t[:], glaon[l, 0:128].rearrange("(p o) -> p o", o=1), writes=[gon.res])
            for i in range(2):
                P.op("pool", lambda E, i=i: E.memset(qeEO[i].t[:], 0.0), writes=[qeEO[i].res])
            TRI = {"F": gcb.t[:, 0:128], "B": gcb.t[:, 128:256]}
            ONES2 = gcb.t[:, 256:384]
            CIND = gcb.t[:, 384:386]
            pA = Pool8(banks[0:3])
            pB = Pool8(banks[3:5])
            pO = Pool8(banks[5:7])
            bKV = banks[7]
            xt_ = list(range(NXT)) if xtiles is None else list(xtiles)
            n = [0]

            def tile_pass(t, d, want_out, final):
                i = n[0] % 2
                n[0] += 1
                RR, KV, QQ = rr[i], kv[i], qq[i]
                P.dma("sp", P.gs(0 + i), RR.t[:], glaG[t * 128:(t + 1) * 128, (0 if d == "F" else 256):(256 if d == "F" else 512)], reads=[R["glaG"]], writes=[RR.res])
                P.dma("sp", P.gs(2 + i), KV.t[:], glaKV[t * 128:(t + 1) * 128, :], reads=[R["glaKV"]], writes=[KV.res])
                if want_out:
                    P.dma("sp", P.gs(4 + i), QQ.t[:], glaQ[t * 128:(t + 1) * 128, :], reads=[R["glaQ"]], writes=[QQ.res])
                b1, b2, b3 = pA.next(), pA.next(), pA.next()
                tri = TRI[d]
                RH, RL = rh[i], rl[i]
                P.op("dve", lambda E, RH=RH, RR=RR: E.tensor_copy(out=RH.t[:], in_=RR.t[:]), reads=[RR.res], writes=[RH.res])
                P.op("dve", lambda E, RH=RH, RL=RL, RR=RR: E.tensor_tensor(out=RL.t[:], in0=RR.t[:], in1=RH.t[:], op=ALU.subtract), reads=[RR.res, RH.res], writes=[RL.res])

                def f1(E, b1=b1, RH=RH, RL=RL, tri=tri):
                    E.matmul(b1.t[:, 0:256], lhsT=tri, rhs=RH.t[:], start=True, stop=False)
                    return E.matmul(b1.t[:, 0:256], lhsT=tri, rhs=RL.t[:], start=False, stop=True)
                P.op("pe", f1, reads=[gcb.res, RH.res, RL.res], writes=[b1.res])

                def f2(E, b2=b2, RH=RH, RL=RL):
                    E.matmul(b2.t[:, 0:256], lhsT=ONES2, rhs=RH.t[:], start=True, stop=False)
                    return E.matmul(b2.t[:, 0:256], lhsT=ONES2, rhs=RL.t[:], start=False, stop=True)
                P.op("pe", f2, reads=[gcb.res, RH.res, RL.res], writes=[b2.res])

                def f3(E, b3=b3, RH=RH, RL=RL):
                    for pr in range(2):
                        E.matmul(b3.t[:, pr * 2:pr * 2 + 2], lhsT=RH.t[:, pr * 128:(pr + 1) * 128], rhs=CIND, start=True, stop=False)
                        ins = E.matmul(b3.t[:, pr * 2:pr * 2 + 2], lhsT=RL.t[:, pr * 128:(pr + 1) * 128], rhs=CIND, start=False, stop=True)
                    return ins
                P.op("pe", f3, reads=[gcb.res, RH.res, RL.res], writes=[b3.res])
                EB, ENB, EBT, EDT = eb[i], enb[i], ebt[i], edT[i]
                if want_out:
                    P.op("act", lambda E, EB=EB, b1=b1: E.activation(out=EB.t[:], in_=b1.t[:, 0:256], func=AF.Exp), reads=[b1.res], writes=[EB.res])
                P.op("act", lambda E, ENB=ENB, b1=b1: E.activation(out=ENB.t[:], in_=b1.t[:, 0:256], func=AF.Exp, scale=-1.0), reads=[b1.res], writes=[ENB.res])
                P.op("act", lambda E, EBT=EBT, b2=b2: E.activation(out=EBT.t[:], in_=b2.t[:, 0:256], func=AF.Exp), reads=[b2.res], writes=[EBT.res])
                P.op("act", lambda E, EDT=EDT, b3=b3: E.activation(out=EDT.t[:], in_=b3.t[:, 0:4], func=AF.Exp), reads=[b3.res], writes=[EDT.res])
                if cstop < 1:
                    return
                QE, KE, KW, KWZ = qe[i], ke[i], kw[i], kwz[i]
                if want_out:
                    P.op("dve", lambda E, QE=QE, QQ=QQ, EB=EB: E.tensor_tensor(out=QE.t[:], in0=QQ.t[:], in1=EB.t[:], op=ALU.mult), reads=[QQ.res, EB.res], writes=[QE.res])
                    P.op("dve", lambda E, KE=KE, KV=KV, ENB=ENB: E.tensor_tensor(out=KE.t[:], in0=KV.t[:, 0:256], in1=ENB.t[:], op=ALU.mult), reads=[KV.res, ENB.res], writes=[KE.res])
                P.op("pool", lambda E, KW=KW, ENB=ENB, EBT=EBT: E.tensor_tensor(out=KW.t[:], in0=ENB.t[:], in1=EBT.t[:], op=ALU.mult), reads=[ENB.res, EBT.res], writes=[KW.res])
                P.op("pool", lambda E, KW=KW, KV=KV: E.tensor_tensor(out=KW.t[:], in0=KW.t[:], in1=KV.t[:, 0:256], op=ALU.mult), reads=[KW.res, KV.res], writes=[KW.res])
                for c in range(2):
                    P.op("dve", lambda E, KW=KW, KWZ=KWZ, c=c: E.tensor_scalar(out=KWZ.t[:, c, :], in0=KW.t[:], scalar1=gc.t[:, 386 + c:387 + c], scalar2=None, op0=ALU.mult),
                         reads=[KW.res, gc.res], awrites=[KWZ.res])
                if cstop < 2:
                    return
                QEO, KET = qeEO[i], keT[i]
                if want_out:
                    bt = pA.next()

                    def tr(E, bt=bt, QE=QE, KE=KE):
                        for j in range(2):
                            E.transpose(out=bf(bt)[:, j * 128:(j + 1) * 128], in_=QE.t[:, j * 128:(j + 1) * 128], identity=ident.t[:])
                        for j in range(2):
                            ins = E.transpose(out=bf(bt)[:, (2 + j) * 128:(3 + j) * 128], in_=KE.t[:, j * 128:(j + 1) * 128], identity=ident.t[:])
                        return ins
                    P.op("pe", tr, reads=[QE.res, KE.res, ident.res], writes=[bt.res])
                    for hh in range(2):
                        P.op("dve", lambda E, bt=bt, QEO=QEO, hh=hh: E.tensor_scalar(out=QEO.t[:, hh, :, :].rearrange("p a b -> p (a b)"), in0=bf(bt)[:, 0:256],
                                                                                    scalar1=gc.t[:, 386 + hh:387 + hh], scalar2=None, op0=ALU.mult),
                             reads=[bt.res, gc.res], awrites=[QEO.res])
                    P.op("dve", lambda E, bt=bt, KET=KET: E.tensor_copy(out=KET.t[:].rearrange("p a b -> p (a b)"), in_=bf(bt)[:, 256:512]), reads=[bt.res], writes=[KET.res])
                    OS = oS[i]
                    if final:
                        OFl = oF[i]
                        P.dma("sp", P.gs(6 + i), OFl.t[:], glaO[t], reads=[R["glaO"]], writes=[OFl.res])
                if cstop < 3:
                    return
                for c in ([0, 1] if d == "F" else [1, 0]):
                    if want_out and cstop >= 4:
                        ba = pB.next()

                        def fa(E, ba=ba, KET=KET, QEO=QEO, c=c):
                            for h in range(4):
                                ins = E.matmul(ba.t[:, h * 64:(h + 1) * 64], lhsT=KET.t[:, h // 2, :], rhs=QEO.t[:, h % 2, h // 2, c * 64:(c + 1) * 64], start=True, stop=True)
                            return ins
                        P.op("pe", fa, reads=[KET.res, QEO.res], writes=[ba.res])
                        AT = aTm[c]
                        mi = (0 if d == "F" else 2) + c
                        P.op("dve", lambda E, AT=AT, ba=ba, mi=mi: E.tensor_tensor(out=AT.t[:], in0=ba.t[:, 0:256], in1=mk.t[:, mi, :], op=ALU.mult), reads=[ba.res, mk.res], writes=[AT.res])
                        bo = pO.next()

                        def fo(E, bo=bo, QEO=QEO, AT=AT, KV=KV, c=c):
                            for h in range(4):
                                E.matmul(bo.t[:, h * 64:(h + 1) * 64], lhsT=Sb.t[:, h // 2, :], rhs=QEO.t[:, h % 2, h // 2, c * 64:(c + 1) * 64], start=True, stop=False)
                                ins = E.matmul(bo.t[:, h * 64:(h + 1) * 64], lhsT=KV.t[:, 256 + h * 128:256 + (h + 1) * 128], rhs=AT.t[:, h * 64:(h + 1) * 64], start=False, stop=True)
                            return ins
                        P.op("pe", fo, reads=[Sb.res, QEO.res, AT.res, KV.res], writes=[bo.res])
                        bo_v = bo.t[:, 0:256].rearrange("p (h k) -> p h k", h=4)
                        if final:
                            P.op("dve", lambda E, OS=OS, OFl=OFl, bo_v=bo_v, c=c: E.tensor_tensor(out=OS.t[:, :, c * 64:(c + 1) * 64], in0=bo_v, in1=OFl.t[:, :, c * 64:(c + 1) * 64], op=ALU.add),
                                 reads=[bo.res, OFl.res], awrites=[OS.res])
                        else:
                            P.op("act", lambda E, OS=OS, bo_v=bo_v, c=c: E.copy(out=OS.t[:, :, c * 64:(c + 1) * 64], in_=bo_v), reads=[bo.res], awrites=[OS.res])

                    def fk(E, KWZ=KWZ, KV=KV, c=c):
                        E.matmul(bKV.t[:, 0:256], lhsT=KWZ.t[:, c, 0:128], rhs=KV.t[:, 256:512], start=True, stop=True)
                        return E.matmul(bKV.t[:, 256:512], lhsT=KWZ.t[:, c, 128:256], rhs=KV.t[:, 512:768], start=True, stop=True)
                    P.op("pe", fk, reads=[KWZ.res, KV.res], writes=[bKV.res])
                    for pr in range(2):
                        TD = tdg[pr]
                        P.op("dve", lambda E, pr=pr, TD=TD: E.tensor_scalar(out=TD.t[:], in0=bKV.t[:, pr * 256:pr * 256 + 128], scalar1=gc.t[:, 386:387], scalar2=None, op0=ALU.mult),
                             reads=[bKV.res, gc.res], writes=[TD.res])
                        P.op("dve", lambda E, pr=pr, TD=TD: E.scalar_tensor_tensor(out=TD.t[:], in0=bKV.t[:, pr * 256 + 128:pr * 256 + 256], scalar=gc.t[:, 387:388], in1=TD.t[:],
                                                                                  op0=ALU.mult, op1=ALU.add), reads=[bKV.res, gc.res, TD.res], writes=[TD.res])
                        P.op("dve", lambda E, pr=pr, TD=TD, c=c, EDT=EDT: E.scalar_tensor_tensor(out=S2.t[:, pr, :], in0=S2.t[:, pr, :], scalar=EDT.t[:, pr * 2 + c:pr * 2 + c + 1], in1=TD.t[:],
                                                                                              op0=ALU.mult, op1=ALU.add), reads=[S2.res, EDT.res, TD.res], awrites=[S2.res])
                    P.op("act", lambda E: E.copy(out=Sb.t[:], in_=S2.t[:]), reads=[S2.res], writes=[Sb.res])
                if cstop < 5:
                    return
                if want_out and not final:
                    P.dma("sp", P.gs(8 + i), glaO[t], OS.t[:], reads=[OS.res], awrites=[R["glaO"]])
                if want_out and final:
                    SQ, RS, SR, CX = sq[i], rs[i], srT[i], cxT[i]
                    P.dma("sp", P.gs(10 + i), SR.t[:], glaR[t], reads=[R["glaR"]], writes=[SR.res])
                    osf = OS.t[:].rearrange("p h k -> p (h k)")
                    P.op("pool", lambda E, SQ=SQ, osf=osf: E.tensor_tensor(out=SQ.t[:], in0=osf, in1=osf, op=ALU.mult), reads=[OS.res], writes=[SQ.res])
                    bs = pB.next()
                    P.op("pe", lambda E, bs=bs, SQ=SQ: E.matmul(bs.t[:], lhsT=ones_bf.t[:], rhs=SQ.t[:], start=True, stop=True), reads=[SQ.res, ones_bf.res], writes=[bs.res])
                    P.op("act", lambda E, bs=bs, RS=RS: E.activation(out=RS.t[:], in_=bs.t[:], func=AF.Ln, scale=1.0 / 128, bias=EPS), reads=[bs.res], writes=[RS.res])
                    P.op("act", lambda E, RS=RS: E.activation(out=RS.t[:], in_=RS.t[:], func=AF.Exp, scale=-0.5), reads=[RS.res], writes=[RS.res])
                    P.op("dve", lambda E, RS=RS, osf=osf: E.scalar_tensor_tensor(out=RS.t[:], in0=osf, scalar=gon.t[:, 0:1], in1=RS.t[:], op0=ALU.mult, op1=ALU.mult),
                         reads=[OS.res, RS.res, gon.res], writes=[RS.res])
                    P.op("pool", lambda E, RS=RS, SR=SR, CX=CX: E.tensor_tensor(out=CX.t[:].rearrange("p h k -> p (h k)"), in0=RS.t[:], in1=SR.t[:].rearrange("p h k -> p (h k)"), op=ALU.mult),
                         reads=[RS.res, SR.res], writes=[CX.res])
                    P.dma("sp", P.gs(12 + i), mixT[t, :, 12:16, :], CX.t[:], reads=[CX.res], awrites=[R["mixT"]])

            for d in ("F", "B"):
                P.op("pool", lambda E: E.memset(S2.t[:], 0.0), writes=[S2.res])
                P.op("pool", lambda E: E.memset(Sb.t[:], 0.0), writes=[Sb.res])
                ct = [NXT, NXT + 1] if d == "F" else [NXT + 1, NXT]
                for t in ct:
                    tile_pass(t, d, ctx_out, d == "B")
                for t in (xt_ if d == "F" else xt_[::-1]):
                    tile_pass(t, d, True, d == "B")
            P.barrier()
            P.flush()

    cx.phaseC = phaseC


    def phaseD(l, tiles, sel=False):
        with ExitStack() as ps:
            wo = sb(ps, nc, "wo", [128, 16, D], BF16)
            wr = sb(ps, nc, "wrs", [128, 16, 20], BF16)
            brs = sb(ps, nc, "brs", [128, 20], F32)
            gt1 = sbn(ps, nc, "gt1", [128, D], F32, 2)
            G2 = sbn(ps, nc, "G2_", [128, D], F32, 2)
            S2m = sbn(ps, nc, "S2m", [128, D], F32, 2)
            mx = sbn(ps, nc, "mx", [128, 16, 128], BF16, 2)
            xt = sbn(ps, nc, "xd", [128, D], F32, 2)
            tmp = sb(ps, nc, "tmpd", [128, D], F32)
            junk = sb(ps, nc, "junkd", [128, D], BF16)
            h2 = sbn(ps, nc, "h2", [128, D], BF16, 2)
            h2T = sbn(ps, nc, "h2T", [128, 16, 128], BF16, 2)
            st = sbn(ps, nc, "std", [128, 8], F32, 2)
            rt = sbn(ps, nc, "rt", [128, 128], F32, 2)
            for kq in range(4):
                P.dma("pool", P.one(), wo.t[:, kq * 4:(kq + 1) * 4, :], wout_d[l, kq * 512:(kq + 1) * 512, :].rearrange("(kc p) n -> p kc n", p=128), writes=[wo.res])
            P.dma("pool", P.one(), wr.t[:], wr_d[l].rearrange("(kc p) n -> p kc n", p=128), writes=[wr.res])
            P.dma("sp", P.one(), brs.t[:], br_d[l].partition_broadcast(128), writes=[brs.res])
            for r in range(2):
                P.dma("sp", P.one(), gt1[r].t[:], modrow(l, r, 2), reads=[R["modbuf"]], writes=[gt1[r].res])
                P.dma("sp", P.one(), G2[r].t[:], modrow(l, r, 4), reads=[R["modbuf"]], writes=[G2[r].res])
                P.dma("sp", P.one(), S2m[r].t[:], modrow(l, r, 3), reads=[R["modbuf"]], writes=[S2m[r].res])
            pz = Pool8(banks[0:4])
            pp = Pool8(banks[4:8])
            for n, t in enumerate(tiles):
                i = n % 2
                r = 0 if t < NXT else 1
                MX, X, S, H2, HT, RT = mx[i], xt[i], st[i], h2[i], h2T[i], rt[i]
                if sel:
                    src, sres, msrc, mres = xS[t * 128:(t + 1) * 128, :], R["xS"], mixS[t], R["mixS"]
                else:
                    src, sres = tile_src(l, t)
                    msrc, mres = mixT[t], R["mixT"]
                P.dma("sp", P.gs(0 + i), MX.t[:], msrc, reads=[mres], writes=[MX.res])
                P.dma("sp", P.gs(2 + i), X.t[:], src, reads=([sres] if sres else []), writes=[X.res])
                for nb in range(4):
                    bk = pz.next()

                    def f(E, bk=bk, MX=MX, nb=nb):
                        for kc in range(16):
                            ins = E.matmul(bk.t[:], lhsT=MX.t[:, kc, :], rhs=wo.t[:, kc, nb * 512:(nb + 1) * 512], start=(kc == 0), stop=(kc == 15))
                        return ins
                    P.op("pe", f, reads=[MX.res, wo.res], writes=[bk.res])
                    P.op("dve", lambda E, bk=bk, nb=nb, r=r: E.tensor_tensor(out=tmp.t[:, nb * 512:(nb + 1) * 512], in0=bk.t[:], in1=gt1[r].t[:, nb * 512:(nb + 1) * 512], op=ALU.mult),
                         reads=[bk.res, gt1[r].res], awrites=[tmp.res])
                P.op("pool", lambda E, X=X: E.tensor_tensor(out=X.t[:], in0=X.t[:], in1=tmp.t[:], op=ALU.add), reads=[X.res, tmp.res], writes=[X.res])
                P.dma("sp", P.gs(4 + i), x1buf[t * 128:(t + 1) * 128, :], X.t[:], reads=[X.res], awrites=[R["x1buf"]])
                P.op("act", lambda E, X=X, S=S: E.activation(out=junk.t[:], in_=X.t[:], func=AF.Square, accum_out=S.t[:, 0:1]), reads=[X.res], writes=[junk.res, S.res])
                rstd_ops(P, S, 0, 1, 1, 1.0 / D)
                P.op("dve", lambda E, X=X, S=S, r=r: E.scalar_tensor_tensor(out=tmp.t[:], in0=X.t[:], scalar=S.t[:, 1:2], in1=G2[r].t[:], op0=ALU.mult, op1=ALU.mult),
                     reads=[X.res, S.res, G2[r].res], writes=[tmp.res])
                P.op("pool", lambda E, H2=H2, r=r: E.tensor_tensor(out=H2.t[:], in0=tmp.t[:], in1=S2m[r].t[:], op=ALU.add), reads=[tmp.res, S2m[r].res], writes=[H2.res])
                for half in range(2):
                    bk = pp.next()

                    def tr(E, H2=H2, bk=bk, half=half):
                        for j in range(8):
                            kc = half * 8 + j
                            ins = E.transpose(out=bf(bk)[:, j * 128:(j + 1) * 128], in_=H2.t[:, kc * 128:(kc + 1) * 128], identity=ident.t[:])
                        return ins
                    P.op("pe", tr, reads=[H2.res, ident.res], writes=[bk.res])
                    if half == 0:
                        P.op("act", lambda E, HT=HT, bk=bk: E.copy(out=HT.t[:, 0:8, :], in_=bf(bk)), reads=[bk.res], awrites=[HT.res])
                    else:
                        P.op("dve", lambda E, HT=HT, bk=bk: E.tensor_copy(out=HT.t[:, 8:16, :], in_=bf(bk)), reads=[bk.res], awrites=[HT.res])
                P.dma("sp", P.gs(6 + i), h2Tbuf[t], HT.t[:], reads=[HT.res], awrites=[R["h2Tbuf"]])
                bk = pp.next()

                def rm(E, bk=bk, HT=HT):
                    for kc in range(16):
                        ins = E.matmul(bk.t[:, 0:20], lhsT=HT.t[:, kc, :], rhs=wr.t[:, kc, :], start=(kc == 0), stop=(kc == 15))
                    return ins
                P.op("pe", rm, reads=[HT.res, wr.res], writes=[bk.res])
                dv = lambda f_, rd, wr_: P.op("dve", f_, reads=rd, writes=wr_)
                dv(lambda E, RT=RT, bk=bk: E.tensor_tensor(out=RT.t[:, 0:20], in0=bk.t[:, 0:20], in1=brs.t[:], op=ALU.add), [bk.res, brs.res], [RT.res])
                dv(lambda E, RT=RT, S=S: E.tensor_reduce(out=S.t[:, 2:3], in_=RT.t[:, 0:4], axis=AX.X, op=ALU.max), [RT.res], [S.res])
                dv(lambda E, S=S: E.tensor_scalar(out=S.t[:, 3:4], in0=S.t[:, 2:3], scalar1=-1.0, scalar2=None, op0=ALU.mult), [S.res], [S.res])
                P.op("act", lambda E, RT=RT, S=S: E.activation(out=RT.t[:, 20:24], in_=RT.t[:, 0:4], func=AF.Exp, bias=S.t[:, 3:4], accum_out=S.t[:, 4:5]),
                     reads=[RT.res, S.res], writes=[RT.res, S.res])
                dv(lambda E, S=S: E.reciprocal(out=S.t[:, 4:5], in_=S.t[:, 4:5]), [S.res], [S.res])
                dv(lambda E, RT=RT, S=S: E.tensor_scalar(out=RT.t[:, 24:28], in0=RT.t[:, 0:4], scalar1=S.t[:, 2:3], scalar2=None, op0=ALU.is_ge), [RT.res, S.res], [RT.res])
                dv(lambda E, RT=RT: E.tensor_scalar(out=RT.t[:, 24:28], in0=RT.t[:, 24:28], scalar1=-1.0, scalar2=1e30, op0=ALU.add, op1=ALU.mult), [RT.res], [RT.res])
                for g in range(4):
                    dv(lambda E, RT=RT, g=g: E.tensor_scalar(out=RT.t[:, 32 + 4 * g:36 + 4 * g], in0=RT.t[:, 4 + 4 * g:8 + 4 * g], scalar1=RT.t[:, 24 + g:25 + g], scalar2=None, op0=ALU.add),
                       [RT.res], [RT.res])
                dv(lambda E, RT=RT, S=S: E.tensor_reduce(out=S.t[:, 5:6], in_=RT.t[:, 32:48], axis=AX.X, op=ALU.max), [RT.res], [S.res])
                dv(lambda E, RT=RT, S=S: E.tensor_scalar(out=RT.t[:, 48:64], in0=RT.t[:, 32:48], scalar1=S.t[:, 5:6], scalar2=None, op0=ALU.is_ge), [RT.res, S.res], [RT.res])
                dv(lambda E, RT=RT: E.scalar_tensor_tensor(out=RT.t[:, 64:80], in0=RT.t[:, 48:64], scalar=-1e30, in1=RT.t[:, 32:48], op0=ALU.mult, op1=ALU.add), [RT.res], [RT.res])
                dv(lambda E, RT=RT, S=S: E.tensor_reduce(out=S.t[:, 6:7], in_=RT.t[:, 64:80], axis=AX.X, op=ALU.max), [RT.res], [S.res])
                dv(lambda E, RT=RT, S=S: E.tensor_scalar(out=RT.t[:, 80:96], in0=RT.t[:, 64:80], scalar1=S.t[:, 6:7], scalar2=None, op0=ALU.is_ge), [RT.res, S.res], [RT.res])
                dv(lambda E, S=S: E.tensor_tensor(out=S.t[:, 7:8], in0=S.t[:, 6:7], in1=S.t[:, 5:6], op=ALU.subtract), [S.res], [S.res])
                P.op("act", lambda E, S=S: E.activation(out=S.t[:, 7:8], in_=S.t[:, 7:8], func=AF.Exp), reads=[S.res], writes=[S.res])
                dv(lambda E, S=S: E.tensor_scalar(out=S.t[:, 6:7], in0=S.t[:, 7:8], scalar1=1.0, scalar2=None, op0=ALU.add), [S.res], [S.res])
                dv(lambda E, S=S: E.reciprocal(out=S.t[:, 6:7], in_=S.t[:, 6:7]), [S.res], [S.res])
                dv(lambda E, S=S: E.tensor_tensor(out=S.t[:, 7:8], in0=S.t[:, 7:8], in1=S.t[:, 6:7], op=ALU.mult), [S.res], [S.res])
                dv(lambda E, RT=RT, S=S: E.tensor_scalar(out=RT.t[:, 96:112], in0=RT.t[:, 48:64], scalar1=S.t[:, 6:7], scalar2=None, op0=ALU.mult), [RT.res, S.res], [RT.res])
                dv(lambda E, RT=RT, S=S: E.scalar_tensor_tensor(out=RT.t[:, 96:112], in0=RT.t[:, 80:96], scalar=S.t[:, 7:8], in1=RT.t[:, 96:112], op0=ALU.mult, op1=ALU.add),
                   [RT.res, S.res], [RT.res])
                dv(lambda E, RT=RT, S=S: E.tensor_scalar(out=RT.t[:, 96:112], in0=RT.t[:, 96:112], scalar1=S.t[:, 4:5], scalar2=None, op0=ALU.mult), [RT.res, S.res], [RT.res])
                P.dma("sp", P.gs(8 + i), combb[t * 128:(t + 1) * 128, :], RT.t[:, 96:112], reads=[RT.res], awrites=[R["combb"]])
            P.barrier()
            P.flush()

    def phaseE(l, tiles, experts=range(NE)):
        groups = []
        rest = list(tiles)
        ng = (len(rest) + 8) // 9
        base, extra = divmod(len(rest), ng)
        for g in range(ng):
            k = base + (1 if g < extra else 0)
            groups.append(rest[:k])
            rest = rest[k:]
        for grp in groups:
            with ExitStack() as ps:
                G = len(grp)
                hT = sb(ps, nc, "hTe", [128, 16, 9 * 128], BF16)
                acc = sb(ps, nc, "acc", [128, 9, D], F32)
                cmb = sb(ps, nc, "cmb", [128, 9, 16], F32)
                w1s = sbn(ps, nc, "w1s", [128, 16, 256], BF16, 2)
                w3s = sbn(ps, nc, "w3s", [128, 16, 256], BF16, 2)
                w2s = sbn(ps, nc, "w2s", [128, 2, D], BF16, 2)
                sg = sbn(ps, nc, "sg", [128, 512], F32, 2)
                aT = sbn(ps, nc, "aT", [128, 2, 512], BF16, 2)
                x1 = sb(ps, nc, "x1e", [128, D], F32)
                gt2 = sbn(ps, nc, "gt2", [128, D], F32, 2)
                for r in range(2):
                    P.dma("sp", P.one(), gt2[r].t[:], modrow(l, r, 5), reads=[R["modbuf"]], writes=[gt2[r].res])
                for j, t in enumerate(grp):
                    P.dma("sp", P.one(), hT.t[:, :, j * 128:(j + 1) * 128], h2Tbuf[t], reads=[R["h2Tbuf"]], awrites=[hT.res])
                    P.dma("sp", P.one(), cmb.t[:, j, :], combb[t * 128:(t + 1) * 128, :], reads=[R["combb"]], awrites=[cmb.res])
                P.op("pool", lambda E, acc=acc: E.memset(acc.t[:], 0.0), writes=[acc.res])
                pz = Pool8(banks[0:4])
                pp = Pool8(banks[4:8])
                blocks = [(a, min(4, G - a)) for a in range(0, G, 4)]
                u = 0
                for e in experts:
                    for q in range(4):
                        i = u % 2
                        u += 1
                        W1, W3, W2 = w1s[i], w3s[i], w2s[i]
                        P.dma("pool", P.gs(0 + i), W1.t[:], w1_d[l, e, :, q * 256:(q + 1) * 256].rearrange("(kc p) n -> p kc n", p=128), writes=[W1.res])
                        P.dma("pool", P.gs(2 + i), W3.t[:], w3_d[l, e, :, q * 256:(q + 1) * 256].rearrange("(kc p) n -> p kc n", p=128), writes=[W3.res])
                        P.dma("pool", P.gs(4 + i), W2.t[:], w2_d[l, e, q * 256:(q + 1) * 256, :].rearrange("(kc p) n -> p kc n", p=128), writes=[W2.res])
                        for bi, (a, nt) in enumerate(blocks):
                            N = nt * 128
                            gb = {}
                            for wi, W in enumerate((W1, W3)):
                                for dc in range(2):
                                    bk = pz.next()

                                    def f(E, bk=bk, W=W, dc=dc, a=a, N=N, hT=hT):
                                        for kc in range(16):
                                            ins = E.matmul(bk.t[:, 0:N], lhsT=W.t[:, kc, dc * 128:(dc + 1) * 128], rhs=hT.t[:, kc, a * 128:a * 128 + N], start=(kc == 0), stop=(kc == 15))
                                        return ins
                                    P.op("pe", f, reads=[W.res, hT.res], writes=[bk.res])
                                    gb[(wi, dc)] = bk
                            AT = aT[bi % 2]
                            for dc in range(2):
                                SG = sg[dc]
                                P.op("act", lambda E, SG=SG, bk=gb[(0, dc)], N=N: E.activation(out=SG.t[:, 0:N], in_=bk.t[:, 0:N], func=AF.Silu), reads=[gb[(0, dc)].res], writes=[SG.res])
                                P.op("dve", lambda E, SG=SG, AT=AT, bk=gb[(1, dc)], N=N, dc=dc: E.tensor_tensor(out=AT.t[:, dc, 0:N], in0=bk.t[:, 0:N], in1=SG.t[:, 0:N], op=ALU.mult),
                                     reads=[gb[(1, dc)].res, SG.res], awrites=[AT.res])
                            for j in range(nt):
                                jt = a + j
                                for nb in range(4):
                                    bk = pp.next()

                                    def fd(E, bk=bk, AT=AT, W2=W2, j=j, nb=nb):
                                        E.matmul(bk.t[:], lhsT=AT.t[:, 0, j * 128:(j + 1) * 128], rhs=W2.t[:, 0, nb * 512:(nb + 1) * 512], start=True, stop=False)
                                        return E.matmul(bk.t[:], lhsT=AT.t[:, 1, j * 128:(j + 1) * 128], rhs=W2.t[:, 1, nb * 512:(nb + 1) * 512], start=False, stop=True)
                                    P.op("pe", fd, reads=[AT.res, W2.res], writes=[bk.res])
                                    P.op("dve", lambda E, bk=bk, jt=jt, nb=nb, e=e, acc=acc, cmb=cmb: E.scalar_tensor_tensor(
                                        out=acc.t[:, jt, nb * 512:(nb + 1) * 512], in0=bk.t[:], scalar=cmb.t[:, jt, e:e + 1], in1=acc.t[:, jt, nb * 512:(nb + 1) * 512],
                                        op0=ALU.mult, op1=ALU.add), reads=[bk.res, cmb.res], awrites=[acc.res])
                for j, t in enumerate(grp):
                    r = 0 if t < NXT else 1
                    P.dma("sp", P.gs(6), x1.t[:], x1buf[t * 128:(t + 1) * 128, :], reads=[R["x1buf"]], writes=[x1.res])
                    P.op("dve", lambda E, j=j, r=r, acc=acc, gt2=gt2: E.tensor_tensor(out=acc.t[:, j, :], in0=acc.t[:, j, :], in1=gt2[r].t[:], op=ALU.mult),
                         reads=[acc.res, gt2[r].res], awrites=[acc.res])
                    P.op("pool", lambda E, j=j, acc=acc, x1=x1: E.tensor_tensor(out=acc.t[:, j, :], in0=acc.t[:, j, :], in1=x1.t[:], op=ALU.add),
                         reads=[acc.res, x1.res], awrites=[acc.res])
                    if t < NXT:
                        dst, dres = (xbuf, R["xbuf"]) if l < DEPTH - 1 else (y_out, R["y"])
                        dst = dst[t * 128:(t + 1) * 128, :]
                    else:
                        dst, dres = cbuf[(t - NXT) * 128:(t - NXT + 1) * 128, :], R["cbuf"]
                    P.dma("sp", P.gs(7 + j), dst, acc.t[:, j, :], reads=[acc.res], awrites=[dres])
                P.barrier()
                P.flush()

    cx.phaseD = phaseD
    cx.phaseE = phaseE


    def phaseSel(l):
        with ExitStack() as ps:
            hm = sb(ps, nc, "hm", [128, 2], F32)
            ma = sbn(ps, nc, "selma", [128, D], BF16, 2)
            mb = sbn(ps, nc, "selmb", [128, D], BF16, 2)
            xa = sbn(ps, nc, "selxa", [128, D], F32, 2)
            xb = sbn(ps, nc, "selxb", [128, D], F32, 2)
            P.dma("sp", P.one(), hm.t[:], hfm, writes=[hm.res])
            H = NXT // 2
            xs_ = x_in if l == 0 else xbuf
            xres = [R["xbuf"]] if l else []
            for j in range(H):
                i = j % 2
                MA, MB, XA, XB = ma[i], mb[i], xa[i], xb[i]
                P.dma("sp", P.gs(0 + i), MA.t[:], mixT[j].rearrange("p a b -> p (a b)"), reads=[R["mixT"]], writes=[MA.res])
                P.dma("sp", P.gs(2 + i), MB.t[:], mixT[H + j].rearrange("p a b -> p (a b)"), reads=[R["mixT"]], writes=[MB.res])
                P.dma("sp", P.gs(4 + i), XA.t[:], xs_[j * 128:(j + 1) * 128, :], reads=xres, writes=[XA.res])
                P.dma("sp", P.gs(6 + i), XB.t[:], xs_[(H + j) * 128:(H + j + 1) * 128, :], reads=xres, writes=[XB.res])
                P.op("dve", lambda E, MA=MA: E.tensor_scalar(out=MA.t[:], in0=MA.t[:], scalar1=hm.t[:, 0:1], scalar2=None, op0=ALU.mult), reads=[MA.res, hm.res], writes=[MA.res])
                P.op("dve", lambda E, MA=MA, MB=MB: E.scalar_tensor_tensor(out=MA.t[:], in0=MB.t[:], scalar=hm.t[:, 1:2], in1=MA.t[:], op0=ALU.mult, op1=ALU.add),
                     reads=[MA.res, MB.res, hm.res], writes=[MA.res])
                P.dma("sp", P.gs(8 + i), mixS[j].rearrange("p a b -> p (a b)"), MA.t[:], reads=[MA.res], awrites=[R["mixS"]])
                P.op("dve", lambda E, XA=XA: E.tensor_scalar(out=XA.t[:], in0=XA.t[:], scalar1=hm.t[:, 0:1], scalar2=None, op0=ALU.mult), reads=[XA.res, hm.res], writes=[XA.res])
                P.op("dve", lambda E, XA=XA, XB=XB: E.scalar_tensor_tensor(out=XA.t[:], in0=XB.t[:], scalar=hm.t[:, 1:2], in1=XA.t[:], op0=ALU.mult, op1=ALU.add),
                     reads=[XA.res, XB.res, hm.res], writes=[XA.res])
                P.dma("sp", P.gs(10 + i), xS[j * 128:(j + 1) * 128, :], XA.t[:], reads=[XA.res], awrites=[R["xS"]])
            P.barrier()
            P.flush()

    cx.phaseSel = phaseSel

    def phase_copy_out():
        with ExitStack() as ps:
            xt = sbn(ps, nc, "cpx", [128, D], F32, 2)
            ls = [P.gs(20), P.gs(21), P.gs(0), P.gs(1)]
            for t in range(NXT // 2):
                X = xt[t % 2]
                P.dma("sp", ls[t % 2], X.t[:], x_in[t * 128:(t + 1) * 128, :], writes=[X.res])
                P.dma("sp", ls[2 + t % 2], y_out[t * 128:(t + 1) * 128, :], X.t[:], reads=[X.res], awrites=[R["y"]])
            P.barrier()
            P.flush()

    cx.phase_copy_out = phase_copy_out
    cx.phase0 = phase0
    cx.phaseA1 = phaseA1
    cx.P = P
    cx.nc = nc
    cx.gs = gs
    cx.R = R
    cx.y_out = y_out
    return cx


def finish(cx):
    P = cx.P
    P.barrier()
    P.flush()
    cx.gs.close()
    P.es.close()
    return cx.nc


IN_OFF = dict(cq=0, ckv=512, kpe=1024, zu=1088, zv=1600, gq=2112, gk=2368, gv=2624, gr=3136, ggf=3648, ggb=3664)


def _rope_tables():
    rows = SEQ // 64
    row = np.repeat(np.arange(rows), 64).astype(np.float32)
    col = np.tile(np.arange(64), rows).astype(np.float32)
    inv = (10000.0 ** (-np.arange(16, dtype=np.float32) / 16)).astype(np.float32)
    ar = row[:, None] * inv
    ac = col[:, None] * inv
    cos64 = np.concatenate([np.cos(ar), np.cos(ar), np.cos(ac), np.cos(ac)], axis=1)
    sin64 = np.concatenate([-np.sin(ar), np.sin(ar), -np.sin(ac), np.sin(ac)], axis=1)
    cos64 = np.concatenate([cos64, np.ones((CTX, 64), np.float32)], axis=0).astype(np.float32)
    sin64 = np.concatenate([sin64, np.zeros((CTX, 64), np.float32)], axis=0).astype(np.float32)
    tabk = np.concatenate([cos64, sin64], axis=1).reshape(NT, 128, 128)
    tabq = np.concatenate([np.tile(cos64, (1, 8)), np.tile(sin64, (1, 8))], axis=1).reshape(NT, 128, 1024)
    return np.ascontiguousarray(tabk), np.ascontiguousarray(tabq)


def _swap64():
    idx = np.arange(64)
    blk = idx // 32
    j = idx % 32
    return blk * 32 + (j + 16) % 32


def prep_shared(inp):
    sw = _swap64()
    o = IN_OFF
    w_in = inp["w_in"]
    colk = np.concatenate([np.arange(o["ckv"], o["ckv"] + 512), np.arange(o["kpe"], o["kpe"] + 64), o["kpe"] + sw,
                           np.arange(o["gk"], o["gk"] + 256), np.arange(o["ggf"], o["ggf"] + 16), np.arange(o["ggb"], o["ggb"] + 16),
                           np.zeros(96, np.int64), np.arange(o["gv"], o["gv"] + 512)])
    colq = np.concatenate([np.arange(o["cq"], o["cq"] + 512), np.arange(o["gq"], o["gq"] + 256), np.arange(o["gr"], o["gr"] + 512),
                           np.arange(o["zv"], o["zv"] + 512), np.arange(o["zu"], o["zu"] + 512)])
    sh = {}
    sh["wk"] = np.ascontiguousarray(w_in[:, :, colk])
    sh["wq"] = np.ascontiguousarray(w_in[:, :, colq])
    ukv = inp["mla_w_ukv"].reshape(DEPTH, 512, 8, 2, 128)
    sh["wukv"] = np.ascontiguousarray(np.concatenate([ukv[:, :, :, 0, :].reshape(DEPTH, 512, 1024), ukv[:, :, :, 1, :].reshape(DEPTH, 512, 1024)], axis=2))
    uq = inp["mla_w_uq"].reshape(DEPTH, 512, 8, 192)
    sh["wuq"] = np.ascontiguousarray(np.concatenate([uq[..., :128].reshape(DEPTH, 512, 1024), uq[..., 128:].reshape(DEPTH, 512, 512),
                                                     uq[..., 128:][..., sw].reshape(DEPTH, 512, 512)], axis=2))
    sh["qnorm"] = inp["mla_q_norm"]
    sh["kvnorm"] = inp["mla_kv_norm"]
    qg, kg = inp["mla_q_gain"], inp["mla_k_gain"]
    sh["qgn"] = np.ascontiguousarray(qg[:, :128, None])
    sh["kgn"] = np.ascontiguousarray(kg[:, :128, None])
    sh["qgpe"] = np.ascontiguousarray(np.concatenate([np.tile(qg[:, 128:], (1, 8)), np.tile(qg[:, 128:][:, sw], (1, 8))], axis=1))
    sh["kgpe"] = np.ascontiguousarray(np.concatenate([kg[:, 128:], kg[:, 128:][:, sw]], axis=1))
    sh["sgun"] = np.ascontiguousarray(inp["sgu_norm"].reshape(DEPTH, 512))
    sh["wsT"] = np.ascontiguousarray(inp["sgu_w"].transpose(0, 3, 1, 2))
    sh["sgub"] = np.ascontiguousarray(inp["sgu_b"].reshape(DEPTH, 512))
    sh["wgF"] = np.ascontiguousarray(np.concatenate([inp["gla_wg_f"], inp["gla_bg_f"][:, None, :]], axis=1))
    sh["wgB"] = np.ascontiguousarray(np.concatenate([inp["gla_wg_b"], inp["gla_bg_b"][:, None, :]], axis=1))
    sh["glaon"] = np.ascontiguousarray(np.tile(inp["gla_out_norm"], (1, 4)))
    sh["w_out"] = inp["w_out"]
    sh["wr"] = np.ascontiguousarray(np.concatenate([inp["moe_w_group"], inp["moe_w_expert"]], axis=2))
    sh["br"] = np.ascontiguousarray(np.concatenate([inp["moe_b_group"], inp["moe_b_expert"]], axis=1))
    sh["w1"], sh["w3"], sh["w2"] = inp["moe_w1"], inp["moe_w3"], inp["moe_w2"]
    sh["ada_w"], sh["ada_b"] = inp["ada_w"], inp["ada_b"]
    sh["g1"], sh["g2"] = inp["norm1_g"], inp["norm2_g"]
    sh["tabk"], sh["tabq"] = _rope_tables()
    sh["ident"] = np.eye(128, dtype=np.float32)
    s_, t_ = np.meshgrid(np.arange(128), np.arange(128), indexing="ij")
    same = (s_ // 64) == (t_ // 64)
    triF = np.where(same & (s_ <= t_), -1.0 / 16, 0.0)
    triB = np.where(same & (s_ >= t_), -1.0 / 16, 0.0)
    ones2 = np.where(same, -1.0 / 16, 0.0)
    cind = np.stack([np.where(np.arange(128) // 64 == c, -1.0 / 16, 0.0) for c in range(2)], axis=1)
    cm = np.stack([(np.arange(128) // 64 == c).astype(np.float32) for c in range(2)], axis=1)
    sl_ = np.arange(128)[:, None]
    tl_ = np.arange(64)[None, :]
    masks = []
    for dF in (True, False):
        for c in range(2):
            inc = (sl_ // 64 == c) & (((sl_ % 64) <= tl_) if dF else ((sl_ % 64) >= tl_))
            masks.append(np.tile(inc.astype(np.float32), (1, 4)))
    sh["gcon"] = np.ascontiguousarray(np.concatenate([triF, triB, ones2, cind, cm] + masks, axis=1).astype(np.float32))
    return sh


def core_inputs(inp, sh, b):
    m = dict(sh)
    m["x"] = np.ascontiguousarray(inp["x"][b])
    m["ctx"] = np.ascontiguousarray(inp["ctx"][b])
    cv = np.stack([inp["c"][b], inp["c_ctx"]], axis=0)
    m["cT"] = np.ascontiguousarray(cv.reshape(2, 16, 128).transpose(2, 1, 0))
    return m


def build_full():
    cx = build()
    xt = list(range(NXT))
    allt = list(range(NT))
    for l in range(DEPTH):
        last = l == DEPTH - 1
        full = xt if last else allt
        cx.phase0(l)
        cx.phaseA1(l, allt)
        cx.phaseA2(l, full)
        cx.phaseB(l, not last)
        cx.phaseC(l, not last)
        if last:
            half = list(range(NXT // 2))
            cx.phaseSel(l)
            cx.phaseD(l, half, sel=True)
            cx.phaseE(l, half)
        else:
            cx.phaseD(l, full)
            cx.phaseE(l, full)
    return finish(cx)


def kernel(**inputs):
    inp = {k: np.asarray(v) for k, v in inputs.items()}
    sh = prep_shared(inp)
    nc = build_full()
    in_maps = []
    for c in range(NCORES):
        m = core_inputs(inp, sh, c // 2)
        hf = float(c % 2)
        m["hfm"] = np.ascontiguousarray(np.tile(np.array([[1.0 - hf, hf]], np.float32), (128, 1)))
        in_maps.append(m)
    res = run_bass_kernel_spmd(nc, in_maps, core_ids=list(range(NCORES)))
    B = NCORES // 2
    out = np.empty((B, SEQ, D), np.float32)
    for c in range(NCORES):
        hf = c % 2
        out[c // 2, hf * (SEQ // 2):(hf + 1) * (SEQ // 2)] = np.asarray(res.results[c]["y"], dtype=np.float32)
    return out
```

```python
from contextlib import ExitStack
import numpy as np
import concourse.bass as bass
import concourse.mybir as mybir
from concourse.bass_utils import run_bass_kernel_spmd

F32, BF16 = mybir.dt.float32, mybir.dt.bfloat16
AF = mybir.ActivationFunctionType
ALU = mybir.AluOpType
AX = mybir.AxisListType

D = 2048
SEQ = 4096
CTX = 256
NXT = SEQ // 128
NCT = CTX // 128
NT = NXT + NCT
NTOK = NT * 128
DEPTH = 2
EPS = 1e-6
MLA_SCALE = 192 ** -0.5
NE = 16
DE = 1024
NCORES = 8


class Sem:
    def __init__(self, h, k):
        self.h, self.k, self.n = h, k, 0


class Res:
    __slots__ = ("w", "r")

    def __init__(self):
        self.w = {}
        self.r = {}


def _merge(dst, src):
    for k, (s, v) in src.items():
        if k not in dst or dst[k][1] < v:
            dst[k] = (s, v)


class Buf:
    def __init__(self, t):
        self.t = t
        self.res = Res()

    def __getitem__(self, key):
        return self.t[key]


class Prog:
    ENG = ("pe", "act", "dve", "pool", "sp")

    def __init__(self, nc):
        self.nc = nc
        self.es = ExitStack()
        self.nsem = 0
        self.streams = {e: [] for e in self.ENG}
        self.esem = {e: self.newsem() for e in self.ENG}
        self.waited = {e: {} for e in self.ENG}
        self.dsems = []
        self.ninstr = 0

    def newsem(self):
        h = self.es.enter_context(self.nc.semaphore(f"sm{self.nsem}"))
        s = Sem(h, self.nsem)
        self.nsem += 1
        return s

    def slot(self):
        s = self.newsem()
        self.dsems.append(s)
        return s

    def gs(self, j):
        if not hasattr(self, "_gs"):
            self._gs = {}
        if j not in self._gs:
            self._gs[j] = self.slot()
        return self._gs[j]

    def one(self):
        if not hasattr(self, "_ones"):
            self._ones = [self.slot() for _ in range(20)]
            self._onei = 0
        s = self._ones[self._onei % 20]
        self._onei += 1
        return s

    def _emit(self, eng, fn, sem, inc, reads, writes, awrites):
        deps = {}
        for r in reads:
            _merge(deps, r.w)
        for w in writes:
            _merge(deps, w.w)
            _merge(deps, w.r)
        for w in awrites:
            _merge(deps, w.r)
        st = self.streams[eng]
        wd = self.waited[eng]
        for k, (s, v) in deps.items():
            if eng == "pe" and s is self.esem["pe"]:
                continue
            if wd.get(k, 0) >= v:
                continue
            wd[k] = v
            st.append(("w", s, v))
        sem.n += inc
        tok = {sem.k: (sem, sem.n)}
        st.append(("i", fn, sem, inc))
        self.ninstr += 1
        for r in reads:
            _merge(r.r, tok)
        for w in writes:
            w.w = dict(tok)
            w.r = {}
        for w in awrites:
            _merge(w.w, tok)
        return tok

    def op(self, eng, fn, reads=(), writes=(), awrites=()):
        if self.esem[eng].n > 12000:
            self.dsems.append(self.esem[eng])
            self.esem[eng] = self.newsem()
        return self._emit(eng, fn, self.esem[eng], 1, reads, writes, awrites)

    def dma(self, eng, slot, out, in_, reads=(), writes=(), awrites=(), **kw):
        return self._emit(eng, lambda E: E.dma_start(out=out, in_=in_, **kw), slot, 16, reads, writes, awrites)

    def barrier(self):
        toks = {}
        for e in self.ENG:
            s = self.esem[e]
            if s.n:
                toks[s.k] = (s, s.n)
        for s in self.dsems:
            if s.n:
                toks[s.k] = (s, s.n)
        for e in self.ENG:
            wd = self.waited[e]
            for k, (s, v) in toks.items():
                if wd.get(k, 0) >= v:
                    continue
                wd[k] = v
                self.streams[e].append(("w", s, v))

    def flush(self):
        nc = self.nc
        streams = self.streams
        self.streams = {e: [] for e in self.ENG}

        def replay(E, st):
            for it in st:
                if it[0] == "w":
                    E.wait_ge(it[1].h, it[2])
                else:
                    it[1](E).then_inc(it[2].h, it[3])

        with nc.Block() as block:
            @block.tensor
            def _(E):
                replay(E, streams["pe"])

            @block.scalar
            def _(E):
                replay(E, streams["act"])

            @block.vector
            def _(E):
                replay(E, streams["dve"])

            @block.gpsimd
            def _(E):
                replay(E, streams["pool"])

            @block.sync
            def _(E):
                replay(E, streams["sp"])


class Pool8:
    def __init__(self, banks):
        self.b = banks
        self.i = 0

    def next(self):
        b = self.b[self.i % len(self.b)]
        self.i += 1
        return b


_UID = [0]


def sb(ps, nc, name, shape, dt):
    _UID[0] += 1
    return Buf(ps.enter_context(nc.sbuf_tensor(f"s{_UID[0]}_{name}", shape, dt)))


def sbn(ps, nc, name, shape, dt, n):
    return [sb(ps, nc, f"{name}{i}", shape, dt) for i in range(n)]


class Ctx:
    pass


def rstd_ops(P, st, ssres_cols, out_cols, n, scale):
    a, b = ssres_cols, out_cols
    P.op("act", lambda E: E.activation(out=st.t[:, b:b + n], in_=st.t[:, a:a + n], func=AF.Ln, scale=scale, bias=EPS),
         reads=[st.res], writes=[st.res])
    P.op("act", lambda E: E.activation(out=st.t[:, b:b + n], in_=st.t[:, b:b + n], func=AF.Exp, scale=-0.5),
         reads=[st.res], writes=[st.res])


def build(nlayers=DEPTH, dbg=(), lite=False, upto=99):
    nc = bass.Bass("TRN2", target_bir_lowering=False)
    P = Prog(nc)
    cx = Ctx()
    L = DEPTH

    def din(name, shape, dt=F32):
        return nc.dram_tensor(name, list(shape), dt, kind="ExternalInput").ap()

    def dscr(name, shape, dt):
        kind = "ExternalOutput" if name in dbg else "Internal"
        return nc.dram_tensor(name, list(shape), dt, kind=kind).ap()

    x_in = din("x", [SEQ, D])
    c_in = din("ctx", [CTX, D])
    cT = din("cT", [128, 16, 2])
    ada_w = din("ada_w", [L, D, 6 * D])
    ada_b = din("ada_b", [L, 6 * D])
    g1 = din("g1", [L, D])
    g2 = din("g2", [L, D])
    wk_d = din("wk", [L, D, 1536])
    wq_d = din("wq", [L, D, 2304])
    wukv_d = din("wukv", [L, 512, 2048])
    wuq_d = din("wuq", [L, 512, 2048])
    qnorm = din("qnorm", [L, 512])
    kvnorm = din("kvnorm", [L, 512])
    qgn = din("qgn", [L, 128, 1])
    kgn = din("kgn", [L, 128, 1])
    qgpe = din("qgpe", [L, 1024])
    kgpe = din("kgpe", [L, 128])
    sgun = din("sgun", [L, 512])
    wsT_d = din("wsT", [L, 128, 4, 128])
    sgub = din("sgub", [L, 512])
    wgF_d = din("wgF", [L, 17, 256])
    wgB_d = din("wgB", [L, 17, 256])
    glaon = din("glaon", [L, 512])
    wout_d = din("w_out", [L, D, D])
    wr_d = din("wr", [L, D, 20])
    br_d = din("br", [L, 20])
    nE = 1 if lite else NE
    w1_d = din("w1", [L, nE, D, DE])
    w3_d = din("w3", [L, nE, D, DE])
    w2_d = din("w2", [L, nE, DE, D])
    tabk = din("tabk", [NT, 128, 128])
    tabq = din("tabq", [NT, 128, 1024])
    ident_d = din("ident", [128, 128])
    gcon = din("gcon", [128, 1412])
    hfm = din("hfm", [128, 2])
    y_out = nc.dram_tensor("y", [SEQ // 2, D], F32, kind="ExternalOutput").ap()

    modbuf = dscr("modbuf", [L, 2, 6 * D], F32)
    xbuf = dscr("xbuf", [SEQ, D], F32)
    cbuf = dscr("cbuf", [CTX, D], F32)
    x1buf = dscr("x1buf", [NTOK, D], F32)
    hTbuf = dscr("hTbuf", [NT, 128, 16, 128], BF16)
    h2Tbuf = dscr("h2Tbuf", [NT, 128, 16, 128], BF16)
    Vb = dscr("Vb", [NTOK, 1024], BF16)
    KnT = dscr("KnT", [NT, 128, 8, 128], BF16)
    KpeT = dscr("KpeT", [128, NTOK], BF16)
    rkb = dscr("rkb", [NTOK, 8], F32)
    QnT = dscr("QnT", [NT, 128, 8, 128], BF16)
    QpeT = dscr("QpeT", [NT, 128, 4, 128], BF16)
    glaKV = dscr("glaKV", [NTOK, 768], BF16)
    glaG = dscr("glaG", [NTOK, 512], F32)
    glaQ = dscr("glaQ", [NTOK, 256], BF16)
    glaR = dscr("glaR", [NT, 128, 4, 128], BF16)
    glaO = dscr("glaO", [NT, 128, 4, 128], F32)
    mixT = dscr("mixT", [NT, 128, 16, 128], BF16)
    combb = dscr("combb", [NTOK, 16], F32)
    mixS = dscr("mixS", [NXT // 2, 128, 16, 128], BF16)
    xS = dscr("xS", [SEQ // 2, D], F32)
    QnS = dscr("QnS", [NXT // 2, 128, 8, 128], BF16)
    QpS = dscr("QpS", [NXT // 2, 128, 4, 128], BF16)
    R = {n: Res() for n in ("modbuf", "xbuf", "cbuf", "x1buf", "hTbuf", "h2Tbuf", "Vb", "KnT", "KpeT", "rkb", "QnT", "QpeT",
                            "glaKV", "glaG", "glaQ", "glaR", "glaO", "mixT", "combb", "y", "mixS", "xS", "QnS", "QpS")}

    gs = ExitStack()
    banks = []
    for i in range(8):
        banks.append(Buf(gs.enter_context(nc.psum_tensor(f"bank{i}", [128, 512], F32))))
    ident = sb(gs, nc, "ident", [128, 128], BF16)
    ones_bf = sb(gs, nc, "ones_bf", [128, 128], BF16)
    P.dma("pool", P.one(), ident.t[:], ident_d, writes=[ident.res])
    P.op("pool", lambda E: E.memset(ones_bf.t[:], 1.0), writes=[ones_bf.res])

    def bf(bank, n=1024):
        return bank.t[:].bitcast(BF16)[:, 0:n]

    def phase0(l):
        with ExitStack() as ps:
            sil = sb(ps, nc, "sil", [128, 16, 2], F32)
            mod = sb(ps, nc, "mod", [2, 6 * D], F32)
            adab = sb(ps, nc, "adab", [2, 6 * D], F32)
            gg = sb(ps, nc, "gg", [2, 2 * D], F32)
            blk = sbn(ps, nc, "adablk", [128, 16, 512], F32, 2)
            bslot = [P.gs(20), P.gs(21)]
            P.dma("sp", P.one(), sil.t[:], cT, writes=[sil.res])
            P.dma("sp", P.one(), adab.t[:], ada_b[l].partition_broadcast(2), writes=[adab.res])
            P.dma("sp", P.one(), gg.t[:, 0:D], g1[l].partition_broadcast(2), writes=[gg.res])
            P.dma("sp", P.one(), gg.t[:, D:2 * D], g2[l].partition_broadcast(2), writes=[gg.res])
            P.op("act", lambda E: E.activation(out=sil.t[:], in_=sil.t[:], func=AF.Silu), reads=[sil.res], writes=[sil.res])
            pp = Pool8(banks)
            src = ada_w[l].rearrange("(kc p) n -> p kc n", p=128)
            for j in range(24):
                b = blk[j % 2]
                P.dma("sp", bslot[j % 2], b.t[:], src[:, :, j * 512:(j + 1) * 512], writes=[b.res])
                bk = pp.next()

                def mm(E, b=b, bk=bk):
                    for kc in range(16):
                        ins = E.matmul(bk.t[0:2, :], lhsT=sil.t[:, kc, :], rhs=b.t[:, kc, :], start=(kc == 0), stop=(kc == 15))
                    return ins
                P.op("pe", mm, reads=[sil.res, b.res], writes=[bk.res])
                P.op("dve", lambda E, bk=bk, j=j: E.tensor_tensor(out=mod.t[:, j * 512:(j + 1) * 512], in0=bk.t[0:2, :],
                                                                  in1=adab.t[:, j * 512:(j + 1) * 512], op=ALU.add),
                     reads=[bk.res, adab.res], awrites=[mod.res])
            P.op("dve", lambda E: E.scalar_tensor_tensor(out=mod.t[:, D:2 * D], in0=mod.t[:, D:2 * D], scalar=1.0, in1=gg.t[:, 0:D],
                                                          op0=ALU.add, op1=ALU.mult), reads=[mod.res, gg.res], writes=[mod.res])
            P.op("dve", lambda E: E.scalar_tensor_tensor(out=mod.t[:, 4 * D:5 * D], in0=mod.t[:, 4 * D:5 * D], scalar=1.0, in1=gg.t[:, D:2 * D],
                                                          op0=ALU.add, op1=ALU.mult), reads=[mod.res, gg.res], writes=[mod.res])
            P.dma("sp", P.one(), modbuf[l], mod.t[:], reads=[mod.res], awrites=[R["modbuf"]])
            P.barrier()
            P.flush()

    def modrow(l, r, i):
        return modbuf[l, r, i * D:(i + 1) * D].partition_broadcast(128)

    def tile_src(l, t):
        xs = x_in if l == 0 else xbuf
        cs = c_in if l == 0 else cbuf
        if t < NXT:
            return xs[t * 128:(t + 1) * 128, :], (R["xbuf"] if l else None)
        return cs[(t - NXT) * 128:(t - NXT + 1) * 128, :], (R["cbuf"] if l else None)

    def phaseA1(l, tiles):
        with ExitStack() as ps:
            wk = sb(ps, nc, "wk", [128, 16, 1536], BF16)
            wukv = sb(ps, nc, "wukv", [128, 4, 2048], BF16)
            G1 = sbn(ps, nc, "G1_", [128, D], F32, 2)
            S1 = sbn(ps, nc, "S1_", [128, D], F32, 2)
            kvn = sb(ps, nc, "kvn", [128, 512], F32)
            kgn_s = sb(ps, nc, "kgn_s", [128, 1], F32)
            kgpe_s = sb(ps, nc, "kgpe_s", [128, 128], F32)
            wgF = sb(ps, nc, "wgF", [17, 256], BF16)
            wgB = sb(ps, nc, "wgB", [17, 256], BF16)
            xt = sbn(ps, nc, "xt", [128, D], F32, 3)
            tab = sbn(ps, nc, "tab", [128, 128], F32, 2)
            junk = sb(ps, nc, "junk", [128, D], BF16)
            tmp = sbn(ps, nc, "tmp", [128, D], F32, 2)
            hb = sbn(ps, nc, "hb", [128, D], BF16, 2)
            hT = sbn(ps, nc, "hT", [128, 16, 128], BF16, 2)
            st = sbn(ps, nc, "st", [128, 64], F32, 2)
            ckvn = sbn(ps, nc, "ckvn", [128, 512], BF16, 2)
            ckvnT = sbn(ps, nc, "ckvnT", [128, 4, 128], BF16, 2)
            Vt = sbn(ps, nc, "Vt", [128, 1024], BF16, 2)
            knb = sbn(ps, nc, "knb", [128, 1024], BF16, 2)
            KnTt = sbn(ps, nc, "KnTt", [128, 8, 128], BF16, 2)
            rkt = sbn(ps, nc, "rkt", [128, 8], F32, 2)
            rp = sbn(ps, nc, "rp", [128, 192], F32, 2)
            krb = sbn(ps, nc, "krb", [128, 128], BF16, 2)
            KpeTt = sbn(ps, nc, "KpeTt", [128, 128], BF16, 2)
            gkv = sbn(ps, nc, "gkv", [128, 768], BF16, 2)
            ggT = [sbn(ps, nc, "ggTF", [17, 128], BF16, 2), sbn(ps, nc, "ggTB", [17, 128], BF16, 2)]
            ge = sbn(ps, nc, "ge", [128, 512], F32, 2)
            gr_ = sbn(ps, nc, "grr", [128, 512], F32, 2)
            for kq in range(4):
                P.dma("pool", P.one(), wk.t[:, kq * 4:(kq + 1) * 4, :],
                      wk_d[l, kq * 512:(kq + 1) * 512, :].rearrange("(kc p) n -> p kc n", p=128), writes=[wk.res])
            P.dma("pool", P.one(), wukv.t[:], wukv_d[l].rearrange("(kc p) n -> p kc n", p=128), writes=[wukv.res])
            P.dma("pool", P.one(), wgF.t[:], wgF_d[l], writes=[wgF.res])
            P.dma("pool", P.one(), wgB.t[:], wgB_d[l], writes=[wgB.res])
            for r in range(2):
                P.dma("sp", P.one(), G1[r].t[:], modrow(l, r, 1), reads=[R["modbuf"]], writes=[G1[r].res])
                P.dma("sp", P.one(), S1[r].t[:], modrow(l, r, 0), reads=[R["modbuf"]], writes=[S1[r].res])
            P.dma("sp", P.one(), kvn.t[:], kvnorm[l].partition_broadcast(128), writes=[kvn.res])
            P.dma("sp", P.one(), kgn_s.t[:], kgn[l], writes=[kgn_s.res])
            P.dma("sp", P.one(), kgpe_s.t[:], kgpe[l].partition_broadcast(128), writes=[kgpe_s.res])
            for i in range(2):
                for d_ in range(2):
                    P.op("pool", lambda E, t_=ggT[d_][i]: E.memset(t_.t[:], 1.0), writes=[ggT[d_][i].res])
            xsl = [P.gs(20 + j) for j in range(3)]
            tsl = [P.gs(23 + j) for j in range(2)]
            pp = Pool8(banks[4:8])
            pz = Pool8(banks[0:4])
            for n, t in enumerate(tiles):
                i = n % 2
                r = 0 if t < NXT else 1
                X = xt[n % 3]
                src, sres = tile_src(l, t)
                P.dma("sp", xsl[n % 3], X.t[:], src, reads=([sres] if sres else []), writes=[X.res])
                P.dma("sp", tsl[i], tab[i].t[:], tabk[t], writes=[tab[i].res])
                S = st[i]
                P.op("act", lambda E, X=X, S=S: E.activation(out=junk.t[:], in_=X.t[:], func=AF.Square, accum_out=S.t[:, 0:1]),
                     reads=[X.res], writes=[junk.res, S.res])
                rstd_ops(P, S, 0, 1, 1, 1.0 / D)
                T_ = tmp[i]
                P.op("dve", lambda E, X=X, S=S, T_=T_, r=r: E.scalar_tensor_tensor(out=T_.t[:], in0=X.t[:], scalar=S.t[:, 1:2], in1=G1[r].t[:],
                                                                                    op0=ALU.mult, op1=ALU.mult),
                     reads=[X.res, S.res, G1[r].res], writes=[T_.res])
                H = hb[i]
                P.op("pool", lambda E, T_=T_, H=H, r=r: E.tensor_tensor(out=H.t[:], in0=T_.t[:], in1=S1[r].t[:], op=ALU.add),
                     reads=[T_.res, S1[r].res], writes=[H.res])
                HT = hT[i]
                for half in range(2):
                    bk = pp.next()

                    def tr(E, H=H, bk=bk, half=half):
                        for j in range(8):
                            kc = half * 8 + j
                            ins = E.transpose(out=bf(bk)[:, j * 128:(j + 1) * 128], in_=H.t[:, kc * 128:(kc + 1) * 128], identity=ident.t[:])
                        return ins
                    P.op("pe", tr, reads=[H.res, ident.res], writes=[bk.res])
                    eng = "act" if half == 0 else "dve"
                    if eng == "act":
                        P.op("act", lambda E, HT=HT, bk=bk, half=half: E.copy(out=HT.t[:, half * 8:(half + 1) * 8, :], in_=bf(bk)),
                             reads=[bk.res], awrites=[HT.res])
                    else:
                        P.op("dve", lambda E, HT=HT, bk=bk, half=half: E.tensor_copy(out=HT.t[:, half * 8:(half + 1) * 8, :], in_=bf(bk)),
                             reads=[bk.res], awrites=[HT.res])
                P.dma("sp", P.gs(0 + i), hTbuf[t], HT.t[:], reads=[HT.res], awrites=[R["hTbuf"]])
                if upto < 1:
                    continue

                def zmm(c0, c1, M=None):
                    bk = pz.next()

                    def f(E, bk=bk, HT=HT):
                        for kc in range(16):
                            ins = E.matmul(bk.t[:, 0:c1 - c0], lhsT=HT.t[:, kc, :], rhs=wk.t[:, kc, c0:c1], start=(kc == 0), stop=(kc == 15))
                        return ins
                    P.op("pe", f, reads=[HT.res, wk.res], writes=[bk.res])
                    return bk
                b_ckv = zmm(0, 512)
                b_mid = zmm(512, 896)
                b_gv = zmm(1024, 1536)
                b_g = pz.next()

                def gmm(E, bk=b_g, HT=HT):
                    for d_ in range(2):
                        for kc in range(16):
                            ins = E.matmul(bk.t[0:16, d_ * 128:(d_ + 1) * 128], lhsT=wk.t[:, kc, 896 + 16 * d_:912 + 16 * d_], rhs=HT.t[:, kc, :],
                                           start=(kc == 0), stop=(kc == 15))
                    return ins
                P.op("pe", gmm, reads=[HT.res, wk.res], writes=[b_g.res])
                if upto < 2:
                    continue
                P.op("act", lambda E, S=S, bk=b_ckv: E.activation(out=junk.t[:, 0:512], in_=bk.t[:], func=AF.Square, accum_out=S.t[:, 2:3]),
                     reads=[bk.res if False else b_ckv.res], writes=[junk.res, S.res])
                rstd_ops(P, S, 2, 3, 1, 1.0 / 512)
                CK = ckvn[i]
                P.op("dve", lambda E, S=S, bk=b_ckv, CK=CK: E.scalar_tensor_tensor(out=CK.t[:], in0=bk.t[:], scalar=S.t[:, 3:4], in1=kvn.t[:],
                                                                                   op0=ALU.mult, op1=ALU.mult),
                     reads=[b_ckv.res, S.res, kvn.res], writes=[CK.res])
                bk = pp.next()

                def tr2(E, CK=CK, bk=bk):
                    for j in range(4):
                        ins = E.transpose(out=bf(bk)[:, j * 128:(j + 1) * 128], in_=CK.t[:, j * 128:(j + 1) * 128], identity=ident.t[:])
                    return ins
                P.op("pe", tr2, reads=[CK.res, ident.res], writes=[bk.res])
                CT = ckvnT[i]
                P.op("act", lambda E, CT=CT, bk=bk: E.copy(out=CT.t[:], in_=bf(bk, 512)), reads=[bk.res], writes=[CT.res])
                if upto < 3:
                    continue
                kvb = []
                for q4 in range(4):
                    bk = pp.next()

                    def f(E, bk=bk, CT=CT, q4=q4):
                        for kc in range(4):
                            ins = E.matmul(bk.t[:], lhsT=CT.t[:, kc, :], rhs=wukv.t[:, kc, q4 * 512:(q4 + 1) * 512], start=(kc == 0), stop=(kc == 3))
                        return ins
                    P.op("pe", f, reads=[CT.res, wukv.res], writes=[bk.res])
                    kvb.append(bk)
                V = Vt[i]
                P.op("act", lambda E, V=V, bk=kvb[2]: E.copy(out=V.t[:, 0:512], in_=bk.t[:]), reads=[kvb[2].res], awrites=[V.res])
                P.op("dve", lambda E, V=V, bk=kvb[3]: E.tensor_copy(out=V.t[:, 512:1024], in_=bk.t[:]), reads=[kvb[3].res], awrites=[V.res])
                P.dma("sp", P.gs(2 + i), Vb[t * 128:(t + 1) * 128, :], V.t[:], reads=[V.res], awrites=[R["Vb"]])
                if upto < 4:
                    continue
                for h in range(8):
                    P.op("act", lambda E, S=S, bk=kvb[h // 4], h=h: E.activation(out=junk.t[:, 0:128], in_=bk.t[:, (h % 4) * 128:(h % 4 + 1) * 128],
                                                                                func=AF.Square, accum_out=S.t[:, 8 + h:9 + h]),
                         reads=[kvb[h // 4].res], writes=[junk.res], awrites=[S.res])
                P.op("act", lambda E, S=S, bk=b_mid: E.activation(out=junk.t[:, 0:64], in_=bk.t[:, 0:64], func=AF.Square, accum_out=S.t[:, 6:7]),
                     reads=[b_mid.res], writes=[junk.res, S.res])
                P.op("dve", lambda E, S=S: E.tensor_scalar(out=S.t[:, 8:16], in0=S.t[:, 8:16], scalar1=S.t[:, 6:7], scalar2=None, op0=ALU.add),
                     reads=[S.res], writes=[S.res])
                rstd_ops(P, S, 8, 16, 8, 1.0 / 192)
                RK = rkt[i]
                P.op("dve", lambda E, S=S, RK=RK: E.tensor_scalar(out=RK.t[:], in0=S.t[:, 16:24], scalar1=MLA_SCALE, scalar2=None, op0=ALU.mult),
                     reads=[S.res], writes=[RK.res])
                P.dma("sp", P.gs(4 + i), rkb[t * 128:(t + 1) * 128, :], RK.t[:], reads=[RK.res], awrites=[R["rkb"]])
                if upto < 5:
                    continue
                KB = knb[i]
                P.op("dve", lambda E, KB=KB, bk=kvb[0]: E.tensor_copy(out=KB.t[:, 0:512], in_=bk.t[:]), reads=[kvb[0].res], awrites=[KB.res])
                P.op("act", lambda E, KB=KB, bk=kvb[1]: E.copy(out=KB.t[:, 512:1024], in_=bk.t[:]), reads=[kvb[1].res], awrites=[KB.res])
                bk = pp.next()

                def tr3(E, KB=KB, bk=bk):
                    for h in range(8):
                        ins = E.transpose(out=bf(bk)[:, h * 128:(h + 1) * 128], in_=KB.t[:, h * 128:(h + 1) * 128], identity=ident.t[:])
                    return ins
                P.op("pe", tr3, reads=[KB.res, ident.res], writes=[bk.res])
                if upto < 5.1:
                    continue
                KT = KnTt[i]
                P.op("dve", lambda E, KT=KT, bk=bk: E.tensor_scalar(out=KT.t[:].rearrange("p h t -> p (h t)"), in0=bf(bk), scalar1=kgn_s.t[:, 0:1],
                                                                     scalar2=None, op0=ALU.mult),
                     reads=[bk.res, kgn_s.res], writes=[KT.res])
                if upto < 5.2:
                    continue
                P.dma("sp", P.gs(6 + i), KnT[t], KT.t[:], reads=[KT.res], awrites=[R["KnT"]])
                if upto < 6:
                    continue
                RP = rp[i]
                TB = tab[i]
                P.op("dve", lambda E, RP=RP, TB=TB, bk=b_mid: E.tensor_tensor(out=RP.t[:, 0:128], in0=bk.t[:, 0:128], in1=TB.t[:], op=ALU.mult),
                     reads=[b_mid.res, TB.res], writes=[RP.res])
                P.op("dve", lambda E, RP=RP: E.tensor_tensor(out=RP.t[:, 0:128], in0=RP.t[:, 0:128], in1=kgpe_s.t[:], op=ALU.mult),
                     reads=[RP.res, kgpe_s.res], writes=[RP.res])
                KR = krb[i]
                for hh in range(2):
                    P.op("dve", lambda E, RP=RP, KR=KR, hh=hh: E.tensor_tensor(out=KR.t[:, hh * 64:(hh + 1) * 64], in0=RP.t[:, 0:64], in1=RP.t[:, 64:128], op=ALU.add),
                         reads=[RP.res], awrites=[KR.res])
                bk = pp.next()
                P.op("pe", lambda E, KR=KR, bk=bk: E.transpose(out=bf(bk)[:, 0:128], in_=KR.t[:], identity=ident.t[:]),
                     reads=[KR.res, ident.res], writes=[bk.res])
                KP = KpeTt[i]
                P.op("act", lambda E, KP=KP, bk=bk: E.copy(out=KP.t[:], in_=bf(bk)[:, 0:128]), reads=[bk.res], writes=[KP.res])
                P.dma("sp", P.gs(8 + i), KpeT[:, t * 128:(t + 1) * 128], KP.t[:], reads=[KP.res], awrites=[R["KpeT"]])
                if upto < 7:
                    continue
                GK = gkv[i]
                P.op("dve", lambda E, GK=GK, bk=b_mid: E.tensor_copy(out=GK.t[:, 0:256], in_=bk.t[:, 128:384]), reads=[b_mid.res], awrites=[GK.res])
                P.op("act", lambda E, GK=GK, bk=b_gv: E.copy(out=GK.t[:, 256:768], in_=bk.t[:]), reads=[b_gv.res], awrites=[GK.res])
                P.dma("sp", P.gs(10 + i), glaKV[t * 128:(t + 1) * 128, :], GK.t[:], reads=[GK.res], awrites=[R["glaKV"]])
                if upto < 8:
                    continue
                for d_ in range(2):
                    GT = ggT[d_][i]
                    P.op("act", lambda E, GT=GT, d_=d_, bk=b_g: E.copy(out=GT.t[0:16, :], in_=bk.t[0:16, d_ * 128:(d_ + 1) * 128]),
                         reads=[b_g.res], awrites=[GT.res])
                bk = pp.next()

                def lg(E, bk=bk, i=i):
                    E.matmul(bk.t[:, 0:256], lhsT=ggT[0][i].t[:], rhs=wgF.t[:], start=True, stop=True)
                    return E.matmul(bk.t[:, 256:512], lhsT=ggT[1][i].t[:], rhs=wgB.t[:], start=True, stop=True)
                P.op("pe", lg, reads=[ggT[0][i].res, ggT[1][i].res, wgF.res, wgB.res], writes=[bk.res])
                GE = ge[i]
                P.op("act", lambda E, GE=GE, bk=bk: E.activation(out=GE.t[:], in_=bk.t[:], func=AF.Exp, scale=-1.0), reads=[bk.res], writes=[GE.res])
                P.op("dve", lambda E, GE=GE: E.tensor_scalar(out=GE.t[:], in0=GE.t[:], scalar1=1.0, scalar2=None, op0=ALU.add),
                     reads=[GE.res], writes=[GE.res])
                GR = gr_[i]
                P.op("act", lambda E, GE=GE, GR=GR: E.activation(out=GR.t[:], in_=GE.t[:], func=AF.Ln), reads=[GE.res], writes=[GR.res])
                P.dma("sp", P.gs(12 + i), glaG[t * 128:(t + 1) * 128, :], GR.t[:], reads=[GR.res], awrites=[R["glaG"]])
            P.barrier()
            P.flush()


    def phaseA2(l, tiles):
        with ExitStack() as ps:
            wq = sb(ps, nc, "wq", [128, 16, 2304], BF16)
            wuq = sb(ps, nc, "wuq", [128, 4, 2048], BF16)
            qnb = sb(ps, nc, "qnb", [128, 512], F32)
            qgn_s = sb(ps, nc, "qgn_s", [128, 1], F32)
            qgpe_s = sb(ps, nc, "qgpe_s", [128, 1024], F32)
            sgun_s = sb(ps, nc, "sgun_s", [128, 512], F32)
            sgub_s = sb(ps, nc, "sgub_s", [128, 512], F32)
            wsT = sb(ps, nc, "wsT", [128, 4, 128], BF16)
            hT = sbn(ps, nc, "hTq", [128, 16, 128], BF16, 2)
            tq = sbn(ps, nc, "tq", [128, 1024], F32, 2)
            junk = sb(ps, nc, "junkq", [128, 512], BF16)
            st = sbn(ps, nc, "stq", [128, 64], F32, 2)
            cqn = sbn(ps, nc, "cqn", [128, 512], BF16, 2)
            cqnT = sbn(ps, nc, "cqnT", [128, 4, 128], BF16, 2)
            qn16 = sbn(ps, nc, "qn16", [128, 1024], BF16, 2)
            QnTt = sbn(ps, nc, "QnTt", [128, 8, 128], BF16, 2)
            rq = sbn(ps, nc, "rq", [128, 1024], F32, 2)
            qpb = sbn(ps, nc, "qpb", [128, 512], BF16, 2)
            QpeTt = sbn(ps, nc, "QpeTt", [128, 4, 128], BF16, 2)
            gqb = sbn(ps, nc, "gqb", [128, 256], BF16, 2)
            grb = sbn(ps, nc, "grb", [128, 512], BF16, 2)
            gvv = sbn(ps, nc, "gvv", [128, 512], F32, 2)
            vn = sbn(ps, nc, "vn", [128, 512], BF16, 2)
            uT = sbn(ps, nc, "uT", [128, 512], F32, 2)
            t2 = sbn(ps, nc, "t2", [128, 512], F32, 2)
            bxT = sbn(ps, nc, "bxT", [128, 4, 128], BF16, 2)
            for kq in range(4):
                P.dma("pool", P.one(), wq.t[:, kq * 4:(kq + 1) * 4, :],
                      wq_d[l, kq * 512:(kq + 1) * 512, :].rearrange("(kc p) n -> p kc n", p=128), writes=[wq.res])
            P.dma("pool", P.one(), wuq.t[:], wuq_d[l].rearrange("(kc p) n -> p kc n", p=128), writes=[wuq.res])
            P.dma("pool", P.one(), wsT.t[:], wsT_d[l], writes=[wsT.res])
            P.dma("sp", P.one(), qnb.t[:], qnorm[l].partition_broadcast(128), writes=[qnb.res])
            P.dma("sp", P.one(), qgn_s.t[:], qgn[l], writes=[qgn_s.res])
            P.dma("sp", P.one(), qgpe_s.t[:], qgpe[l].partition_broadcast(128), writes=[qgpe_s.res])
            P.dma("sp", P.one(), sgun_s.t[:], sgun[l].partition_broadcast(128), writes=[sgun_s.res])
            P.dma("sp", P.one(), sgub_s.t[:], sgub[l].partition_broadcast(128), writes=[sgub_s.res])
            hsl = [P.gs(20 + j) for j in range(2)]
            tsl = [P.gs(23 + j) for j in range(2)]
            pp = Pool8(banks[4:8])
            pz = Pool8(banks[0:4])
            for n, t in enumerate(tiles):
                i = n % 2
                HT = hT[i]
                TQ = tq[i]
                S = st[i]
                P.dma("sp", hsl[i], HT.t[:], hTbuf[t], reads=[R["hTbuf"]], writes=[HT.res])
                P.dma("sp", tsl[i], TQ.t[:], tabq[t], writes=[TQ.res])

                def zmm(c0, c1):
                    bk = pz.next()

                    def f(E, bk=bk, HT=HT):
                        for kc in range(16):
                            ins = E.matmul(bk.t[:, 0:c1 - c0], lhsT=HT.t[:, kc, :], rhs=wq.t[:, kc, c0:c1], start=(kc == 0), stop=(kc == 15))
                        return ins
                    P.op("pe", f, reads=[HT.res, wq.res], writes=[bk.res])
                    return bk
                b_cq = zmm(0, 512)
                P.op("act", lambda E, S=S, bk=b_cq: E.activation(out=junk.t[:], in_=bk.t[:], func=AF.Square, accum_out=S.t[:, 0:1]),
                     reads=[b_cq.res], writes=[junk.res, S.res])
                rstd_ops(P, S, 0, 1, 1, 1.0 / 512)
                CQ = cqn[i]
                P.op("dve", lambda E, S=S, bk=b_cq, CQ=CQ: E.scalar_tensor_tensor(out=CQ.t[:], in0=bk.t[:], scalar=S.t[:, 1:2], in1=qnb.t[:],
                                                                                   op0=ALU.mult, op1=ALU.mult),
                     reads=[b_cq.res, S.res, qnb.res], writes=[CQ.res])
                bk = pp.next()

                def tr2(E, CQ=CQ, bk=bk):
                    for j in range(4):
                        ins = E.transpose(out=bf(bk)[:, j * 128:(j + 1) * 128], in_=CQ.t[:, j * 128:(j + 1) * 128], identity=ident.t[:])
                    return ins
                P.op("pe", tr2, reads=[CQ.res, ident.res], writes=[bk.res])
                CT = cqnT[i]
                P.op("act", lambda E, CT=CT, bk=bk: E.copy(out=CT.t[:], in_=bf(bk, 512)), reads=[bk.res], writes=[CT.res])
                qb = []
                for q4 in range(4):
                    bk = pp.next()

                    def f(E, bk=bk, CT=CT, q4=q4):
                        for kc in range(4):
                            ins = E.matmul(bk.t[:], lhsT=CT.t[:, kc, :], rhs=wuq.t[:, kc, q4 * 512:(q4 + 1) * 512], start=(kc == 0), stop=(kc == 3))
                        return ins
                    P.op("pe", f, reads=[CT.res, wuq.res], writes=[bk.res])
                    qb.append(bk)
                for h in range(8):
                    P.op("act", lambda E, S=S, bk=qb[h // 4], h=h: E.activation(out=junk.t[:, 0:128], in_=bk.t[:, (h % 4) * 128:(h % 4 + 1) * 128],
                                                                               func=AF.Square, accum_out=S.t[:, 8 + h:9 + h]),
                         reads=[qb[h // 4].res], writes=[junk.res], awrites=[S.res])
                    P.op("act", lambda E, S=S, bk=qb[2], h=h: E.activation(out=junk.t[:, 0:64], in_=bk.t[:, h * 64:(h + 1) * 64],
                                                                          func=AF.Square, accum_out=S.t[:, 16 + h:17 + h]),
                         reads=[qb[2].res], writes=[junk.res], awrites=[S.res])
                P.op("dve", lambda E, S=S: E.tensor_tensor(out=S.t[:, 8:16], in0=S.t[:, 8:16], in1=S.t[:, 16:24], op=ALU.add),
                     reads=[S.res], writes=[S.res])
                rstd_ops(P, S, 8, 24, 8, 1.0 / 192)
                QN = qn16[i]
                for h in range(8):
                    P.op("dve", lambda E, S=S, QN=QN, bk=qb[h // 4], h=h: E.tensor_scalar(
                        out=QN.t[:, h * 128:(h + 1) * 128], in0=bk.t[:, (h % 4) * 128:(h % 4 + 1) * 128], scalar1=S.t[:, 24 + h:25 + h],
                        scalar2=None, op0=ALU.mult), reads=[qb[h // 4].res, S.res], awrites=[QN.res])
                RQ = rq[i]
                P.op("dve", lambda E, RQ=RQ, TQ=TQ, bk=qb[2]: E.tensor_tensor(out=RQ.t[:, 0:512], in0=bk.t[:], in1=TQ.t[:, 0:512], op=ALU.mult),
                     reads=[qb[2].res, TQ.res], awrites=[RQ.res])
                P.op("dve", lambda E, RQ=RQ, TQ=TQ, bk=qb[3]: E.tensor_tensor(out=RQ.t[:, 512:1024], in0=bk.t[:], in1=TQ.t[:, 512:1024], op=ALU.mult),
                     reads=[qb[3].res, TQ.res], awrites=[RQ.res])
                P.op("pool", lambda E, RQ=RQ: E.tensor_tensor(out=RQ.t[:], in0=RQ.t[:], in1=qgpe_s.t[:], op=ALU.mult),
                     reads=[RQ.res, qgpe_s.res], writes=[RQ.res])
                P.op("pool", lambda E, RQ=RQ: E.tensor_tensor(out=RQ.t[:, 0:512], in0=RQ.t[:, 0:512], in1=RQ.t[:, 512:1024], op=ALU.add),
                     reads=[RQ.res], writes=[RQ.res])
                QP = qpb[i]
                for h in range(8):
                    P.op("dve", lambda E, S=S, QP=QP, RQ=RQ, h=h: E.tensor_scalar(
                        out=QP.t[:, h * 64:(h + 1) * 64], in0=RQ.t[:, h * 64:(h + 1) * 64], scalar1=S.t[:, 24 + h:25 + h],
                        scalar2=None, op0=ALU.mult), reads=[RQ.res, S.res], awrites=[QP.res])
                bk = pp.next()

                def tr3(E, QN=QN, bk=bk):
                    for h in range(8):
                        ins = E.transpose(out=bf(bk)[:, h * 128:(h + 1) * 128], in_=QN.t[:, h * 128:(h + 1) * 128], identity=ident.t[:])
                    return ins
                P.op("pe", tr3, reads=[QN.res, ident.res], writes=[bk.res])
                QT = QnTt[i]
                P.op("dve", lambda E, QT=QT, bk=bk: E.tensor_scalar(out=QT.t[:].rearrange("p h t -> p (h t)"), in0=bf(bk), scalar1=qgn_s.t[:, 0:1],
                                                                     scalar2=None, op0=ALU.mult),
                     reads=[bk.res, qgn_s.res], writes=[QT.res])
                P.dma("sp", P.gs(0 + i), QnT[t], QT.t[:], reads=[QT.res], awrites=[R["QnT"]])
                bk = pp.next()

                def tr4(E, QP=QP, bk=bk):
                    for j in range(4):
                        ins = E.transpose(out=bf(bk)[:, j * 128:(j + 1) * 128], in_=QP.t[:, j * 128:(j + 1) * 128], identity=ident.t[:])
                    return ins
                P.op("pe", tr4, reads=[QP.res, ident.res], writes=[bk.res])
                QPT = QpeTt[i]
                P.op("act", lambda E, QPT=QPT, bk=bk: E.copy(out=QPT.t[:], in_=bf(bk, 512)), reads=[bk.res], writes=[QPT.res])
                P.dma("sp", P.gs(2 + i), QpeT[t], QPT.t[:], reads=[QPT.res], awrites=[R["QpeT"]])
                b_gq = zmm(512, 768)
                GQ = gqb[i]
                P.op("act", lambda E, GQ=GQ, bk=b_gq: E.activation(out=GQ.t[:], in_=bk.t[:, 0:256], func=AF.Copy, scale=0.125),
                     reads=[b_gq.res], writes=[GQ.res])
                P.dma("sp", P.gs(4 + i), glaQ[t * 128:(t + 1) * 128, :], GQ.t[:], reads=[GQ.res], awrites=[R["glaQ"]])
                b_gr = pz.next()

                def grT(E, bk=b_gr, HT=HT):
                    for g in range(4):
                        for kc in range(16):
                            ins = E.matmul(bk.t[:, g * 128:(g + 1) * 128], lhsT=wq.t[:, kc, 768 + g * 128:768 + (g + 1) * 128], rhs=HT.t[:, kc, :],
                                           start=(kc == 0), stop=(kc == 15))
                    return ins
                P.op("pe", grT, reads=[HT.res, wq.res], writes=[b_gr.res])
                GR = grb[i]
                P.op("act", lambda E, GR=GR, bk=b_gr: E.activation(out=GR.t[:], in_=bk.t[:], func=AF.Silu), reads=[b_gr.res], writes=[GR.res])
                P.dma("sp", P.gs(6 + i), glaR[t], GR.t[:].rearrange("p (g k) -> p g k", g=4), reads=[GR.res], awrites=[R["glaR"]])
                b_zv = zmm(1280, 1792)
                GV = gvv[i]
                P.op("act", lambda E, GV=GV, bk=b_zv: E.activation(out=GV.t[:], in_=bk.t[:], func=AF.Gelu_apprx_tanh), reads=[b_zv.res], writes=[GV.res])
                for g in range(4):
                    P.op("act", lambda E, S=S, GV=GV, g=g: E.activation(out=junk.t[:, 0:128], in_=GV.t[:, g * 128:(g + 1) * 128], func=AF.Square,
                                                                        accum_out=S.t[:, 32 + g:33 + g]),
                         reads=[GV.res], writes=[junk.res], awrites=[S.res])
                rstd_ops(P, S, 32, 36, 4, 1.0 / 128)
                for g in range(4):
                    P.op("dve", lambda E, S=S, GV=GV, g=g: E.tensor_scalar(out=GV.t[:, g * 128:(g + 1) * 128], in0=GV.t[:, g * 128:(g + 1) * 128],
                                                                           scalar1=S.t[:, 36 + g:37 + g], scalar2=None, op0=ALU.mult),
                         reads=[GV.res, S.res], writes=[GV.res])
                VN = vn[i]
                P.op("pool", lambda E, VN=VN, GV=GV: E.tensor_tensor(out=VN.t[:], in0=GV.t[:], in1=sgun_s.t[:], op=ALU.mult),
                     reads=[GV.res, sgun_s.res], writes=[VN.res])
                b_zu = pz.next()

                def zu(E, bk=b_zu, HT=HT):
                    for g in range(4):
                        for kc in range(16):
                            ins = E.matmul(bk.t[:, g * 128:(g + 1) * 128], lhsT=wq.t[:, kc, 1792 + g * 128:1792 + (g + 1) * 128], rhs=HT.t[:, kc, :],
                                           start=(kc == 0), stop=(kc == 15))
                    return ins
                P.op("pe", zu, reads=[HT.res, wq.res], writes=[b_zu.res])
                U = uT[i]
                P.op("act", lambda E, U=U, bk=b_zu: E.activation(out=U.t[:], in_=bk.t[:], func=AF.Gelu_apprx_tanh), reads=[b_zu.res], writes=[U.res])
                bk = pp.next()

                def sg(E, bk=bk, VN=VN):
                    for g in range(4):
                        ins = E.matmul(bk.t[:, g * 128:(g + 1) * 128], lhsT=VN.t[:, g * 128:(g + 1) * 128], rhs=wsT.t[:, g, :], start=True, stop=True)
                    return ins
                P.op("pe", sg, reads=[VN.res, wsT.res], writes=[bk.res])
                T2 = t2[i]
                P.op("dve", lambda E, T2=T2, bk=bk: E.tensor_tensor(out=T2.t[:], in0=bk.t[:], in1=sgub_s.t[:], op=ALU.add),
                     reads=[bk.res, sgub_s.res], writes=[T2.res])
                BX = bxT[i]
                P.op("pool", lambda E, T2=T2, U=U, BX=BX: E.tensor_tensor(out=BX.t[:].rearrange("p g t -> p (g t)"), in0=T2.t[:], in1=U.t[:], op=ALU.mult),
                     reads=[T2.res, U.res], writes=[BX.res])
                P.dma("sp", P.gs(8 + i), mixT[t, :, 8:12, :], BX.t[:], reads=[BX.res], awrites=[R["mixT"]])
            P.barrier()
            P.flush()

    cx.phaseA2 = phaseA2


    def phaseB(l, with_ctx, heads=range(8), qblocks=None, qsel=False):
        with ExitStack() as ps:
            Kn = sbn(ps, nc, "Kn", [128, NT, 128], BF16, 2)
            Vh = sbn(ps, nc, "Vh", [128, NT, 128], BF16, 2)
            Qn = sbn(ps, nc, "Qn", [128, NT, 128], BF16, 2)
            Qp = sbn(ps, nc, "Qp", [128, NT, 128], BF16, 2)
            KpeEO = sbn(ps, nc, "KpeEO", [128, NTOK], BF16, 2)
            rk = sb(ps, nc, "rk", [128, NT, 8], F32)
            pT = sbn(ps, nc, "pT", [128, 512], BF16, 3)
            rden = sbn(ps, nc, "rden", [128, 512], F32, 2)
            o16 = sbn(ps, nc, "o16", [128, 4, 128], BF16, 2)
            P.op("pool", lambda E: E.memset(KpeEO[0].t[64:128, :], 0.0), awrites=[KpeEO[0].res])
            P.op("pool", lambda E: E.memset(KpeEO[1].t[0:64, :], 0.0), awrites=[KpeEO[1].res])
            P.dma("sp", P.one(), KpeEO[0].t[0:64, :], KpeT[0:64, :], reads=[R["KpeT"]], awrites=[KpeEO[0].res])
            P.dma("sp", P.one(), KpeEO[1].t[64:128, :], KpeT[64:128, :], reads=[R["KpeT"]], awrites=[KpeEO[1].res])
            P.dma("sp", P.one(), rk.t[:], rkb.rearrange("(t p) h -> p t h", p=128), reads=[R["rkb"]], writes=[rk.res])
            KnS = KnT.rearrange("t d h k -> d t h k")
            QnSrc = (QnS if qsel else QnT).rearrange("t d h k -> d t h k")
            QpSrc = (QpS if qsel else QpeT).rearrange("t p j k -> p t j k")
            qres = [R["QnS"], R["QpS"]] if qsel else [R["QnT"], R["QpeT"]]
            nqt = NXT // 2 if qsel else NXT
            mixdst, mixres = (mixS, R["mixS"]) if qsel else (mixT, R["mixT"])
            VS = Vb.rearrange("(t p) c -> p t c", p=128)
            heads = list(heads)

            def loads(h):
                s_ = h % 2
                for a, b in ((0, 17), (17, NT)):
                    P.dma("sp", P.gs(0 + s_), Kn[s_].t[:, a:b, :], KnS[:, a:b, h, :], reads=[R["KnT"]], awrites=[Kn[s_].res])
                    P.dma("sp", P.gs(2 + s_), Vh[s_].t[:, a:b, :], VS[:, a:b, h * 128:(h + 1) * 128], reads=[R["Vb"]], awrites=[Vh[s_].res])
                    if qsel:
                        if a != 0:
                            continue
                        qa, qb_ = 0, nqt
                    else:
                        qa, qb_ = a, b
                    P.dma("sp", P.gs(4 + s_), Qn[s_].t[:, qa:qb_, :], QnSrc[:, qa:qb_, h, :], reads=qres, awrites=[Qn[s_].res])
                    P.dma("sp", P.gs(6 + s_), Qp[s_].t[:, qa:qb_, :], QpSrc[:, qa:qb_, h // 2, :], reads=qres, awrites=[Qp[s_].res])
            spool = Pool8(banks[0:4])
            cnt = [0]
            loads(heads[0])
            for hi, h in enumerate(heads):
                if hi + 1 < len(heads):
                    loads(heads[hi + 1])
                s_ = h % 2
                hp = h % 2
                KN, VH, QN, QP = Kn[s_], Vh[s_], Qn[s_], Qp[s_]
                blocks = [(q0, 4, list(range(NT))) for q0 in range(0, nqt, 4)]
                if with_ctx:
                    blocks.append((NXT, 2, [NXT, NXT + 1]))
                if qblocks is not None:
                    blocks = [blocks[j] for j in qblocks]
                for (q0, nq, keys) in blocks:
                    N = nq * 128
                    c = cnt[0]
                    cnt[0] += 2
                    ob, db = banks[4 + c % 2], banks[6 + c % 2]
                    qn_ap = QN.t[:, q0:q0 + nq, :].rearrange("p a b -> p (a b)")
                    qp_ap = QP.t[:, q0:q0 + nq, :].rearrange("p a b -> p (a b)")
                    Kpe = KpeEO[hp]
                    sb_ = {}

                    def score(j, N=N, KN=KN, QN=QN, QP=QP, Kpe=Kpe, qn_ap=qn_ap, qp_ap=qp_ap, keys=keys, sb_=sb_):
                        kt = keys[j]
                        bk = spool.next()

                        def f(E, bk=bk, kt=kt, N=N, KN=KN, Kpe=Kpe, qn_ap=qn_ap, qp_ap=qp_ap):
                            E.matmul(bk.t[:, 0:N], lhsT=KN.t[:, kt, :], rhs=qn_ap, start=True, stop=False)
                            return E.matmul(bk.t[:, 0:N], lhsT=Kpe.t[:, kt * 128:(kt + 1) * 128], rhs=qp_ap, start=False, stop=True)
                        P.op("pe", f, reads=[KN.res, QN.res, QP.res, Kpe.res], writes=[bk.res])
                        sb_[j] = bk
                    nk = len(keys)
                    score(0)
                    if nk > 1:
                        score(1)
                    for j in range(nk):
                        kt = keys[j]
                        bk = sb_.pop(j)
                        PT = pT[j % 3]
                        P.op("act", lambda E, PT=PT, bk=bk, kt=kt, N=N, h=h: E.activation(out=PT.t[:, 0:N], in_=bk.t[:, 0:N], func=AF.Exp,
                                                                                       scale=rk.t[:, kt, h:h + 1]),
                             reads=[bk.res, rk.res], writes=[PT.res])
                        if j + 2 < nk:
                            score(j + 2)

                        def acc(E, PT=PT, kt=kt, j=j, N=N, nk=nk, ob=ob, db=db, VH=VH):
                            E.matmul(ob.t[:, 0:N], lhsT=VH.t[:, kt, :], rhs=PT.t[:, 0:N], start=(j == 0), stop=(j == nk - 1))
                            return E.matmul(db.t[:, 0:N], lhsT=ones_bf.t[:], rhs=PT.t[:, 0:N], start=(j == 0), stop=(j == nk - 1))
                        P.op("pe", acc, reads=[VH.res, PT.res, ones_bf.res], writes=[ob.res, db.res])
                    RD = rden[c % 2]
                    P.op("dve", lambda E, RD=RD, db=db, N=N: E.reciprocal(out=RD.t[:, 0:N], in_=db.t[:, 0:N]), reads=[db.res], writes=[RD.res])
                    O = o16[c % 2]
                    P.op("dve", lambda E, RD=RD, O=O, ob=ob, N=N, nq=nq: E.tensor_tensor(out=O.t[:, 0:nq, :].rearrange("p a b -> p (a b)"), in0=ob.t[:, 0:N],
                                                                                      in1=RD.t[:, 0:N], op=ALU.mult),
                         reads=[ob.res, RD.res], writes=[O.res])
                    P.dma("sp", P.gs(8 + c % 2), mixdst[q0:q0 + nq, :, h, :].rearrange("t p k -> p t k"), O.t[:, 0:nq, :], reads=[O.res], awrites=[mixres])
            P.barrier()
            P.flush()

    cx.phaseB = phaseB


    def phaseC(l, ctx_out, xtiles=None, cstop=99):
        with ExitStack() as ps:
            gc = sb(ps, nc, "gc", [128, 1412], F32)
            mk = sb(ps, nc, "mk", [128, 4, 256], BF16)
            gcb = sb(ps, nc, "gcb", [128, 386], BF16)
            rh = sbn(ps, nc, "rh", [128, 256], BF16, 2)
            tdg = sbn(ps, nc, "tdg", [128, 128], F32, 2)
            rl = sbn(ps, nc, "rl", [128, 256], BF16, 2)
            gon = sb(ps, nc, "gon", [128, 1], F32)
            S2 = sb(ps, nc, "S2", [128, 2, 128], F32)
            Sb = sb(ps, nc, "Sb", [128, 2, 128], BF16)
            rr = sbn(ps, nc, "rr", [128, 256], F32, 2)
            kv = sbn(ps, nc, "kvg", [128, 768], BF16, 2)
            qq = sbn(ps, nc, "qq", [128, 256], BF16, 2)
            eb = sbn(ps, nc, "eb", [128, 256], F32, 2)
            enb = sbn(ps, nc, "enb", [128, 256], F32, 2)
            ebt = sbn(ps, nc, "ebt", [128, 256], F32, 2)
            edT = sbn(ps, nc, "edT", [128, 4], F32, 2)
            qe = sbn(ps, nc, "qe", [128, 256], BF16, 2)
            ke = sbn(ps, nc, "ke", [128, 256], BF16, 2)
            kw = sbn(ps, nc, "kw", [128, 256], F32, 2)
            kwz = sbn(ps, nc, "kwz", [128, 2, 256], BF16, 2)
            qeEO = sbn(ps, nc, "qeEO", [128, 2, 2, 128], BF16, 2)
            keT = sbn(ps, nc, "keT", [128, 2, 128], BF16, 2)
            aTm = sbn(ps, nc, "aTm", [128, 256], BF16, 2)
            oF = sbn(ps, nc, "oF", [128, 4, 128], F32, 2)
            oS = sbn(ps, nc, "oS", [128, 4, 128], F32, 2)
            sq = sbn(ps, nc, "sqo", [128, 512], BF16, 2)
            rs = sbn(ps, nc, "rso", [128, 512], F32, 2)
            srT = sbn(ps, nc, "srT", [128, 4, 128], BF16, 2)
            cxT = sbn(ps, nc, "cxT", [128, 4, 128], BF16, 2)
            P.dma("sp", P.one(), gc.t[:], gcon, writes=[gc.res])
            P.dma("pool", P.one(), mk.t[:].rearrange("p a b -> p (a b)"), gcon[:, 388:1412], writes=[mk.res])
            P.dma("pool", P.one(), gcb.t[:], gcon[:, 0:386], writes=[gcb.res])
            P.dma("sp", P.one(), gon.t[:], glaon[l, 0:128].rearrange("(p o) -> p o", o=1), writes=[gon.res])
            for i in range(2):
                P.op("pool", lambda E, i=i: E.memset(qeEO[i].t[:], 0.0), writes=[qeEO[i].res])
            TRI = {"F": gcb.t[:, 0:128], "B": gcb.t[:, 128:256]}
            ONES2 = gcb.t[:, 256:384]
            CIND = gcb.t[:, 384:386]
            pA = Pool8(banks[0:3])
            pB = Pool8(banks[3:5])
            pO = Pool8(banks[5:7])
            bKV = banks[7]
            xt_ = list(range(NXT)) if xtiles is None else list(xtiles)
            n = [0]

            def tile_pass(t, d, want_out, final):
                i = n[0] % 2
                n[0] += 1
                RR, KV, QQ = rr[i], kv[i], qq[i]
                P.dma("sp", P.gs(0 + i), RR.t[:], glaG[t * 128:(t + 1) * 128, (0 if d == "F" else 256):(256 if d == "F" else 512)], reads=[R["glaG"]], writes=[RR.res])
                P.dma("sp", P.gs(2 + i), KV.t[:], glaKV[t * 128:(t + 1) * 128, :], reads=[R["glaKV"]], writes=[KV.res])
                if want_out:
                    P.dma("sp", P.gs(4 + i), QQ.t[:], glaQ[t * 128:(t + 1) * 128, :], reads=[R["glaQ"]], writes=[QQ.res])
                b1, b2, b3 = pA.next(), pA.next(), pA.next()
                tri = TRI[d]
                RH, RL = rh[i], rl[i]
                P.op("dve", lambda E, RH=RH, RR=RR: E.tensor_copy(out=RH.t[:], in_=RR.t[:]), reads=[RR.res], writes=[RH.res])
                P.op("dve", lambda E, RH=RH, RL=RL, RR=RR: E.tensor_tensor(out=RL.t[:], in0=RR.t[:], in1=RH.t[:], op=ALU.subtract), reads=[RR.res, RH.res], writes=[RL.res])

                def f1(E, b1=b1, RH=RH, RL=RL, tri=tri):
                    E.matmul(b1.t[:, 0:256], lhsT=tri, rhs=RH.t[:], start=True, stop=False)
                    return E.matmul(b1.t[:, 0:256], lhsT=tri, rhs=RL.t[:], start=False, stop=True)
                P.op("pe", f1, reads=[gcb.res, RH.res, RL.res], writes=[b1.res])

                def f2(E, b2=b2, RH=RH, RL=RL):
                    E.matmul(b2.t[:, 0:256], lhsT=ONES2, rhs=RH.t[:], start=True, stop=False)
                    return E.matmul(b2.t[:, 0:256], lhsT=ONES2, rhs=RL.t[:], start=False, stop=True)
                P.op("pe", f2, reads=[gcb.res, RH.res, RL.res], writes=[b2.res])

                def f3(E, b3=b3, RH=RH, RL=RL):
                    for pr in range(2):
                        E.matmul(b3.t[:, pr * 2:pr * 2 + 2], lhsT=RH.t[:, pr * 128:(pr + 1) * 128], rhs=CIND, start=True, stop=False)
                        ins = E.matmul(b3.t[:, pr * 2:pr * 2 + 2], lhsT=RL.t[:, pr * 128:(pr + 1) * 128], rhs=CIND, start=False, stop=True)
                    return ins
                P.op("pe", f3, reads=[gcb.res, RH.res, RL.res], writes=[b3.res])
                EB, ENB, EBT, EDT = eb[i], enb[i], ebt[i], edT[i]
                if want_out:
                    P.op("act", lambda E, EB=EB, b1=b1: E.activation(out=EB.t[:], in_=b1.t[:, 0:256], func=AF.Exp), reads=[b1.res], writes=[EB.res])
                P.op("act", lambda E, ENB=ENB, b1=b1: E.activation(out=ENB.t[:], in_=b1.t[:, 0:256], func=AF.Exp, scale=-1.0), reads=[b1.res], writes=[ENB.res])
                P.op("act", lambda E, EBT=EBT, b2=b2: E.activation(out=EBT.t[:], in_=b2.t[:, 0:256], func=AF.Exp), reads=[b2.res], writes=[EBT.res])
                P.op("act", lambda E, EDT=EDT, b3=b3: E.activation(out=EDT.t[:], in_=b3.t[:, 0:4], func=AF.Exp), reads=[b3.res], writes=[EDT.res])
                if cstop < 1:
                    return
                QE, KE, KW, KWZ = qe[i], ke[i], kw[i], kwz[i]
                if want_out:
                    P.op("dve", lambda E, QE=QE, QQ=QQ, EB=EB: E.tensor_tensor(out=QE.t[:], in0=QQ.t[:], in1=EB.t[:], op=ALU.mult), reads=[QQ.res, EB.res], writes=[QE.res])
                    P.op("dve", lambda E, KE=KE, KV=KV, ENB=ENB: E.tensor_tensor(out=KE.t[:], in0=KV.t[:, 0:256], in1=ENB.t[:], op=ALU.mult), reads=[KV.res, ENB.res], writes=[KE.res])
                P.op("pool", lambda E, KW=KW, ENB=ENB, EBT=EBT: E.tensor_tensor(out=KW.t[:], in0=ENB.t[:], in1=EBT.t[:], op=ALU.mult), reads=[ENB.res, EBT.res], writes=[KW.res])
                P.op("pool", lambda E, KW=KW, KV=KV: E.tensor_tensor(out=KW.t[:], in0=KW.t[:], in1=KV.t[:, 0:256], op=ALU.mult), reads=[KW.res, KV.res], writes=[KW.res])
                for c in range(2):
                    P.op("dve", lambda E, KW=KW, KWZ=KWZ, c=c: E.tensor_scalar(out=KWZ.t[:, c, :], in0=KW.t[:], scalar1=gc.t[:, 386 + c:387 + c], scalar2=None, op0=ALU.mult),
                         reads=[KW.res, gc.res], awrites=[KWZ.res])
                if cstop < 2:
                    return
                QEO, KET = qeEO[i], keT[i]
                if want_out:
                    bt = pA.next()

                    def tr(E, bt=bt, QE=QE, KE=KE):
                        for j in range(2):
                            E.transpose(out=bf(bt)[:, j * 128:(j + 1) * 128], in_=QE.t[:, j * 128:(j + 1) * 128], identity=ident.t[:])
                        for j in range(2):
                            ins = E.transpose(out=bf(bt)[:, (2 + j) * 128:(3 + j) * 128], in_=KE.t[:, j * 128:(j + 1) * 128], identity=ident.t[:])
                        return ins
                    P.op("pe", tr, reads=[QE.res, KE.res, ident.res], writes=[bt.res])
                    for hh in range(2):
                        P.op("dve", lambda E, bt=bt, QEO=QEO, hh=hh: E.tensor_scalar(out=QEO.t[:, hh, :, :].rearrange("p a b -> p (a b)"), in0=bf(bt)[:, 0:256],
                                                                                    scalar1=gc.t[:, 386 + hh:387 + hh], scalar2=None, op0=ALU.mult),
                             reads=[bt.res, gc.res], awrites=[QEO.res])
                    P.op("dve", lambda E, bt=bt, KET=KET: E.tensor_copy(out=KET.t[:].rearrange("p a b -> p (a b)"), in_=bf(bt)[:, 256:512]), reads=[bt.res], writes=[KET.res])
                    OS = oS[i]
                    if final:
                        OFl = oF[i]
                        P.dma("sp", P.gs(6 + i), OFl.t[:], glaO[t], reads=[R["glaO"]], writes=[OFl.res])
                if cstop < 3:
                    return
                for c in ([0, 1] if d == "F" else [1, 0]):
                    if want_out and cstop >= 4:
                        ba = pB.next()

                        def fa(E, ba=ba, KET=KET, QEO=QEO, c=c):
                            for h in range(4):
                                ins = E.matmul(ba.t[:, h * 64:(h + 1) * 64], lhsT=KET.t[:, h // 2, :], rhs=QEO.t[:, h % 2, h // 2, c * 64:(c + 1) * 64], start=True, stop=True)
                            return ins
                        P.op("pe", fa, reads=[KET.res, QEO.res], writes=[ba.res])
                        AT = aTm[c]
                        mi = (0 if d == "F" else 2) + c
                        P.op("dve", lambda E, AT=AT, ba=ba, mi=mi: E.tensor_tensor(out=AT.t[:], in0=ba.t[:, 0:256], in1=mk.t[:, mi, :], op=ALU.mult), reads=[ba.res, mk.res], writes=[AT.res])
                        bo = pO.next()

                        def fo(E, bo=bo, QEO=QEO, AT=AT, KV=KV, c=c):
                            for h in range(4):
                                E.matmul(bo.t[:, h * 64:(h + 1) * 64], lhsT=Sb.t[:, h // 2, :], rhs=QEO.t[:, h % 2, h // 2, c * 64:(c + 1) * 64], start=True, stop=False)
                                ins = E.matmul(bo.t[:, h * 64:(h + 1) * 64], lhsT=KV.t[:, 256 + h * 128:256 + (h + 1) * 128], rhs=AT.t[:, h * 64:(h + 1) * 64], start=False, stop=True)
                            return ins
                        P.op("pe", fo, reads=[Sb.res, QEO.res, AT.res, KV.res], writes=[bo.res])
                        bo_v = bo.t[:, 0:256].rearrange("p (h k) -> p h k", h=4)
                        if final:
                            P.op("dve", lambda E, OS=OS, OFl=OFl, bo_v=bo_v, c=c: E.tensor_tensor(out=OS.t[:, :, c * 64:(c + 1) * 64], in0=bo_v, in1=OFl.t[:, :, c * 64:(c + 1) * 64], op=ALU.add),
                                 reads=[bo.res, OFl.res], awrites=[OS.res])
                        else:
                            P.op("act", lambda E, OS=OS, bo_v=bo_v, c=c: E.copy(out=OS.t[:, :, c * 64:(c + 1) * 64], in_=bo_v), reads=[bo.res], awrites=[OS.res])

                    def fk(E, KWZ=KWZ, KV=KV, c=c):
                        E.matmul(bKV.t[:, 0:256], lhsT=KWZ.t[:, c, 0:128], rhs=KV.t[:, 256:512], start=True, stop=True)
                        return E.matmul(bKV.t[:, 256:512], lhsT=KWZ.t[:, c, 128:256], rhs=KV.t[:, 512:768], start=True, stop=True)
                    P.op("pe", fk, reads=[KWZ.res, KV.res], writes=[bKV.res])
                    for pr in range(2):
                        TD = tdg[pr]
                        P.op("dve", lambda E, pr=pr, TD=TD: E.tensor_scalar(out=TD.t[:], in0=bKV.t[:, pr * 256:pr * 256 + 128], scalar1=gc.t[:, 386:387], scalar2=None, op0=ALU.mult),
                             reads=[bKV.res, gc.res], writes=[TD.res])
                        P.op("dve", lambda E, pr=pr, TD=TD: E.scalar_tensor_tensor(out=TD.t[:], in0=bKV.t[:, pr * 256 + 128:pr * 256 + 256], scalar=gc.t[:, 387:388], in1=TD.t[:],
                                                                                  op0=ALU.mult, op1=ALU.add), reads=[bKV.res, gc.res, TD.res], writes=[TD.res])
                        P.op("dve", lambda E, pr=pr, TD=TD, c=c, EDT=EDT: E.scalar_tensor_tensor(out=S2.t[:, pr, :], in0=S2.t[:, pr, :], scalar=EDT.t[:, pr * 2 + c:pr * 2 + c + 1], in1=TD.t[:],
                                                                                              op0=ALU.mult, op1=ALU.add), reads=[S2.res, EDT.res, TD.res], awrites=[S2.res])
                    P.op("act", lambda E: E.copy(out=Sb.t[:], in_=S2.t[:]), reads=[S2.res], writes=[Sb.res])
                if cstop < 5:
                    return
                if want_out and not final:
                    P.dma("sp", P.gs(8 + i), glaO[t], OS.t[:], reads=[OS.res], awrites=[R["glaO"]])
                if want_out and final:
                    SQ, RS, SR, CX = sq[i], rs[i], srT[i], cxT[i]
                    P.dma("sp", P.gs(10 + i), SR.t[:], glaR[t], reads=[R["glaR"]], writes=[SR.res])
                    osf = OS.t[:].rearrange("p h k -> p (h k)")
                    P.op("pool", lambda E, SQ=SQ, osf=osf: E.tensor_tensor(out=SQ.t[:], in0=osf, in1=osf, op=ALU.mult), reads=[OS.res], writes=[SQ.res])
                    bs = pB.next()
                    P.op("pe", lambda E, bs=bs, SQ=SQ: E.matmul(bs.t[:], lhsT=ones_bf.t[:], rhs=SQ.t[:], start=True, stop=True), reads=[SQ.res, ones_bf.res], writes=[bs.res])
                    P.op("act", lambda E, bs=bs, RS=RS: E.activation(out=RS.t[:], in_=bs.t[:], func=AF.Ln, scale=1.0 / 128, bias=EPS), reads=[bs.res], writes=[RS.res])
                    P.op("act", lambda E, RS=RS: E.activation(out=RS.t[:], in_=RS.t[:], func=AF.Exp, scale=-0.5), reads=[RS.res], writes=[RS.res])
                    P.op("dve", lambda E, RS=RS, osf=osf: E.scalar_tensor_tensor(out=RS.t[:], in0=osf, scalar=gon.t[:, 0:1], in1=RS.t[:], op0=ALU.mult, op1=ALU.mult),
                         reads=[OS.res, RS.res, gon.res], writes=[RS.res])
                    P.op("pool", lambda E, RS=RS, SR=SR, CX=CX: E.tensor_tensor(out=CX.t[:].rearrange("p h k -> p (h k)"), in0=RS.t[:], in1=SR.t[:].rearrange("p h k -> p (h k)"), op=ALU.mult),
                         reads=[RS.res, SR.res], writes=[CX.res])
                    P.dma("sp", P.gs(12 + i), mixT[t, :, 12:16, :], CX.t[:], reads=[CX.res], awrites=[R["mixT"]])

            for d in ("F", "B"):
                P.op("pool", lambda E: E.memset(S2.t[:], 0.0), writes=[S2.res])
                P.op("pool", lambda E: E.memset(Sb.t[:], 0.0), writes=[Sb.res])
                ct = [NXT, NXT + 1] if d == "F" else [NXT + 1, NXT]
                for t in ct:
                    tile_pass(t, d, ctx_out, d == "B")
                for t in (xt_ if d == "F" else xt_[::-1]):
                    tile_pass(t, d, True, d == "B")
            P.barrier()
            P.flush()

    cx.phaseC = phaseC


    def phaseD(l, tiles, sel=False):
        with ExitStack() as ps:
            wo = sb(ps, nc, "wo", [128, 16, D], BF16)
            wr = sb(ps, nc, "wrs", [128, 16, 20], BF16)
            brs = sb(ps, nc, "brs", [128, 20], F32)
            gt1 = sbn(ps, nc, "gt1", [128, D], F32, 2)
            G2 = sbn(ps, nc, "G2_", [128, D], F32, 2)
            S2m = sbn(ps, nc, "S2m", [128, D], F32, 2)
            mx = sbn(ps, nc, "mx", [128, 16, 128], BF16, 2)
            xt = sbn(ps, nc, "xd", [128, D], F32, 2)
            tmp = sb(ps, nc, "tmpd", [128, D], F32)
            junk = sb(ps, nc, "junkd", [128, D], BF16)
            h2 = sbn(ps, nc, "h2", [128, D], BF16, 2)
            h2T = sbn(ps, nc, "h2T", [128, 16, 128], BF16, 2)
            st = sbn(ps, nc, "std", [128, 8], F32, 2)
            rt = sbn(ps, nc, "rt", [128, 128], F32, 2)
            for kq in range(4):
                P.dma("pool", P.one(), wo.t[:, kq * 4:(kq + 1) * 4, :], wout_d[l, kq * 512:(kq + 1) * 512, :].rearrange("(kc p) n -> p kc n", p=128), writes=[wo.res])
            P.dma("pool", P.one(), wr.t[:], wr_d[l].rearrange("(kc p) n -> p kc n", p=128), writes=[wr.res])
            P.dma("sp", P.one(), brs.t[:], br_d[l].partition_broadcast(128), writes=[brs.res])
            for r in range(2):
                P.dma("sp", P.one(), gt1[r].t[:], modrow(l, r, 2), reads=[R["modbuf"]], writes=[gt1[r].res])
                P.dma("sp", P.one(), G2[r].t[:], modrow(l, r, 4), reads=[R["modbuf"]], writes=[G2[r].res])
                P.dma("sp", P.one(), S2m[r].t[:], modrow(l, r, 3), reads=[R["modbuf"]], writes=[S2m[r].res])
            pz = Pool8(banks[0:4])
            pp = Pool8(banks[4:8])
            for n, t in enumerate(tiles):
                i = n % 2
                r = 0 if t < NXT else 1
                MX, X, S, H2, HT, RT = mx[i], xt[i], st[i], h2[i], h2T[i], rt[i]
                if sel:
                    src, sres, msrc, mres = xS[t * 128:(t + 1) * 128, :], R["xS"], mixS[t], R["mixS"]
                else:
                    src, sres = tile_src(l, t)
                    msrc, mres = mixT[t], R["mixT"]
                P.dma("sp", P.gs(0 + i), MX.t[:], msrc, reads=[mres], writes=[MX.res])
                P.dma("sp", P.gs(2 + i), X.t[:], src, reads=([sres] if sres else []), writes=[X.res])
                for nb in range(4):
                    bk = pz.next()

                    def f(E, bk=bk, MX=MX, nb=nb):
                        for kc in range(16):
                            ins = E.matmul(bk.t[:], lhsT=MX.t[:, kc, :], rhs=wo.t[:, kc, nb * 512:(nb + 1) * 512], start=(kc == 0), stop=(kc == 15))
                        return ins
                    P.op("pe", f, reads=[MX.res, wo.res], writes=[bk.res])
                    P.op("dve", lambda E, bk=bk, nb=nb, r=r: E.tensor_tensor(out=tmp.t[:, nb * 512:(nb + 1) * 512], in0=bk.t[:], in1=gt1[r].t[:, nb * 512:(nb + 1) * 512], op=ALU.mult),
                         reads=[bk.res, gt1[r].res], awrites=[tmp.res])
                P.op("pool", lambda E, X=X: E.tensor_tensor(out=X.t[:], in0=X.t[:], in1=tmp.t[:], op=ALU.add), reads=[X.res, tmp.res], writes=[X.res])
                P.dma("sp", P.gs(4 + i), x1buf[t * 128:(t + 1) * 128, :], X.t[:], reads=[X.res], awrites=[R["x1buf"]])
                P.op("act", lambda E, X=X, S=S: E.activation(out=junk.t[:], in_=X.t[:], func=AF.Square, accum_out=S.t[:, 0:1]), reads=[X.res], writes=[junk.res, S.res])
                rstd_ops(P, S, 0, 1, 1, 1.0 / D)
                P.op("dve", lambda E, X=X, S=S, r=r: E.scalar_tensor_tensor(out=tmp.t[:], in0=X.t[:], scalar=S.t[:, 1:2], in1=G2[r].t[:], op0=ALU.mult, op1=ALU.mult),
                     reads=[X.res, S.res, G2[r].res], writes=[tmp.res])
                P.op("pool", lambda E, H2=H2, r=r: E.tensor_tensor(out=H2.t[:], in0=tmp.t[:], in1=S2m[r].t[:], op=ALU.add), reads=[tmp.res, S2m[r].res], writes=[H2.res])
                for half in range(2):
                    bk = pp.next()

                    def tr(E, H2=H2, bk=bk, half=half):
                        for j in range(8):
                            kc = half * 8 + j
                            ins = E.transpose(out=bf(bk)[:, j * 128:(j + 1) * 128], in_=H2.t[:, kc * 128:(kc + 1) * 128], identity=ident.t[:])
                        return ins
                    P.op("pe", tr, reads=[H2.res, ident.res], writes=[bk.res])
                    if half == 0:
                        P.op("act", lambda E, HT=HT, bk=bk: E.copy(out=HT.t[:, 0:8, :], in_=bf(bk)), reads=[bk.res], awrites=[HT.res])
                    else:
                        P.op("dve", lambda E, HT=HT, bk=bk: E.tensor_copy(out=HT.t[:, 8:16, :], in_=bf(bk)), reads=[bk.res], awrites=[HT.res])
                P.dma("sp", P.gs(6 + i), h2Tbuf[t], HT.t[:], reads=[HT.res], awrites=[R["h2Tbuf"]])
                bk = pp.next()

                def rm(E, bk=bk, HT=HT):
                    for kc in range(16):
                        ins = E.matmul(bk.t[:, 0:20], lhsT=HT.t[:, kc, :], rhs=wr.t[:, kc, :], start=(kc == 0), stop=(kc == 15))
                    return ins
                P.op("pe", rm, reads=[HT.res, wr.res], writes=[bk.res])
                dv = lambda f_, rd, wr_: P.op("dve", f_, reads=rd, writes=wr_)
                dv(lambda E, RT=RT, bk=bk: E.tensor_tensor(out=RT.t[:, 0:20], in0=bk.t[:, 0:20], in1=brs.t[:], op=ALU.add), [bk.res, brs.res], [RT.res])
                dv(lambda E, RT=RT, S=S: E.tensor_reduce(out=S.t[:, 2:3], in_=RT.t[:, 0:4], axis=AX.X, op=ALU.max), [RT.res], [S.res])
                dv(lambda E, S=S: E.tensor_scalar(out=S.t[:, 3:4], in0=S.t[:, 2:3], scalar1=-1.0, scalar2=None, op0=ALU.mult), [S.res], [S.res])
                P.op("act", lambda E, RT=RT, S=S: E.activation(out=RT.t[:, 20:24], in_=RT.t[:, 0:4], func=AF.Exp, bias=S.t[:, 3:4], accum_out=S.t[:, 4:5]),
                     reads=[RT.res, S.res], writes=[RT.res, S.res])
                dv(lambda E, S=S: E.reciprocal(out=S.t[:, 4:5], in_=S.t[:, 4:5]), [S.res], [S.res])
                dv(lambda E, RT=RT, S=S: E.tensor_scalar(out=RT.t[:, 24:28], in0=RT.t[:, 0:4], scalar1=S.t[:, 2:3], scalar2=None, op0=ALU.is_ge), [RT.res, S.res], [RT.res])
                dv(lambda E, RT=RT: E.tensor_scalar(out=RT.t[:, 24:28], in0=RT.t[:, 24:28], scalar1=-1.0, scalar2=1e30, op0=ALU.add, op1=ALU.mult), [RT.res], [RT.res])
                for g in range(4):
                    dv(lambda E, RT=RT, g=g: E.tensor_scalar(out=RT.t[:, 32 + 4 * g:36 + 4 * g], in0=RT.t[:, 4 + 4 * g:8 + 4 * g], scalar1=RT.t[:, 24 + g:25 + g], scalar2=None, op0=ALU.add),
                       [RT.res], [RT.res])
                dv(lambda E, RT=RT, S=S: E.tensor_reduce(out=S.t[:, 5:6], in_=RT.t[:, 32:48], axis=AX.X, op=ALU.max), [RT.res], [S.res])
                dv(lambda E, RT=RT, S=S: E.tensor_scalar(out=RT.t[:, 48:64], in0=RT.t[:, 32:48], scalar1=S.t[:, 5:6], scalar2=None, op0=ALU.is_ge), [RT.res, S.res], [RT.res])
                dv(lambda E, RT=RT: E.scalar_tensor_tensor(out=RT.t[:, 64:80], in0=RT.t[:, 48:64], scalar=-1e30, in1=RT.t[:, 32:48], op0=ALU.mult, op1=ALU.add), [RT.res], [RT.res])
                dv(lambda E, RT=RT, S=S: E.tensor_reduce(out=S.t[:, 6:7], in_=RT.t[:, 64:80], axis=AX.X, op=ALU.max), [RT.res], [S.res])
                dv(lambda E, RT=RT, S=S: E.tensor_scalar(out=RT.t[:, 80:96], in0=RT.t[:, 64:80], scalar1=S.t[:, 6:7], scalar2=None, op0=ALU.is_ge), [RT.res, S.res], [RT.res])
                dv(lambda E, S=S: E.tensor_tensor(out=S.t[:, 7:8], in0=S.t[:, 6:7], in1=S.t[:, 5:6], op=ALU.subtract), [S.res], [S.res])
                P.op("act", lambda E, S=S: E.activation(out=S.t[:, 7:8], in_=S.t[:, 7:8], func=AF.Exp), reads=[S.res], writes=[S.res])
                dv(lambda E, S=S: E.tensor_scalar(out=S.t[:, 6:7], in0=S.t[:, 7:8], scalar1=1.0, scalar2=None, op0=ALU.add), [S.res], [S.res])
                dv(lambda E, S=S: E.reciprocal(out=S.t[:, 6:7], in_=S.t[:, 6:7]), [S.res], [S.res])
                dv(lambda E, S=S: E.tensor_tensor(out=S.t[:, 7:8], in0=S.t[:, 7:8], in1=S.t[:, 6:7], op=ALU.mult), [S.res], [S.res])
                dv(lambda E, RT=RT, S=S: E.tensor_scalar(out=RT.t[:, 96:112], in0=RT.t[:, 48:64], scalar1=S.t[:, 6:7], scalar2=None, op0=ALU.mult), [RT.res, S.res], [RT.res])
                dv(lambda E, RT=RT, S=S: E.scalar_tensor_tensor(out=RT.t[:, 96:112], in0=RT.t[:, 80:96], scalar=S.t[:, 7:8], in1=RT.t[:, 96:112], op0=ALU.mult, op1=ALU.add),
                   [RT.res, S.res], [RT.res])
                dv(lambda E, RT=RT, S=S: E.tensor_scalar(out=RT.t[:, 96:112], in0=RT.t[:, 96:112], scalar1=S.t[:, 4:5], scalar2=None, op0=ALU.mult), [RT.res, S.res], [RT.res])
                P.dma("sp", P.gs(8 + i), combb[t * 128:(t + 1) * 128, :], RT.t[:, 96:112], reads=[RT.res], awrites=[R["combb"]])
            P.barrier()
            P.flush()

    def phaseE(l, tiles, experts=range(NE)):
        groups = []
        rest = list(tiles)
        ng = (len(rest) + 8) // 9
        base, extra = divmod(len(rest), ng)
        for g in range(ng):
            k = base + (1 if g < extra else 0)
            groups.append(rest[:k])
            rest = rest[k:]
        for grp in groups:
            with ExitStack() as ps:
                G = len(grp)
                hT = sb(ps, nc, "hTe", [128, 16, 9 * 128], BF16)
                acc = sb(ps, nc, "acc", [128, 9, D], F32)
                cmb = sb(ps, nc, "cmb", [128, 9, 16], F32)
                w1s = sbn(ps, nc, "w1s", [128, 16, 256], BF16, 2)
                w3s = sbn(ps, nc, "w3s", [128, 16, 256], BF16, 2)
                w2s = sbn(ps, nc, "w2s", [128, 2, D], BF16, 2)
                sg = sbn(ps, nc, "sg", [128, 512], F32, 2)
                aT = sbn(ps, nc, "aT", [128, 2, 512], BF16, 2)
                x1 = sb(ps, nc, "x1e", [128, D], F32)
                gt2 = sbn(ps, nc, "gt2", [128, D], F32, 2)
                for r in range(2):
                    P.dma("sp", P.one(), gt2[r].t[:], modrow(l, r, 5), reads=[R["modbuf"]], writes=[gt2[r].res])
                for j, t in enumerate(grp):
                    P.dma("sp", P.one(), hT.t[:, :, j * 128:(j + 1) * 128], h2Tbuf[t], reads=[R["h2Tbuf"]], awrites=[hT.res])
                    P.dma("sp", P.one(), cmb.t[:, j, :], combb[t * 128:(t + 1) * 128, :], reads=[R["combb"]], awrites=[cmb.res])
                P.op("pool", lambda E, acc=acc: E.memset(acc.t[:], 0.0), writes=[acc.res])
                pz = Pool8(banks[0:4])
                pp = Pool8(banks[4:8])
                blocks = [(a, min(4, G - a)) for a in range(0, G, 4)]
                u = 0
                for e in experts:
                    for q in range(4):
                        i = u % 2
                        u += 1
                        W1, W3, W2 = w1s[i], w3s[i], w2s[i]
                        P.dma("pool", P.gs(0 + i), W1.t[:], w1_d[l, e, :, q * 256:(q + 1) * 256].rearrange("(kc p) n -> p kc n", p=128), writes=[W1.res])
                        P.dma("pool", P.gs(2 + i), W3.t[:], w3_d[l, e, :, q * 256:(q + 1) * 256].rearrange("(kc p) n -> p kc n", p=128), writes=[W3.res])
                        P.dma("pool", P.gs(4 + i), W2.t[:], w2_d[l, e, q * 256:(q + 1) * 256, :].rearrange("(kc p) n -> p kc n", p=128), writes=[W2.res])
                        for bi, (a, nt) in enumerate(blocks):
                            N = nt * 128
                            gb = {}
                            for wi, W in enumerate((W1, W3)):
                                for dc in range(2):
                                    bk = pz.next()

                                    def f(E, bk=bk, W=W, dc=dc, a=a, N=N, hT=hT):
                                        for kc in range(16):
                                            ins = E.matmul(bk.t[:, 0:N], lhsT=W.t[:, kc, dc * 128:(dc + 1) * 128], rhs=hT.t[:, kc, a * 128:a * 128 + N], start=(kc == 0), stop=(kc == 15))
                                        return ins
                                    P.op("pe", f, reads=[W.res, hT.res], writes=[bk.res])
                                    gb[(wi, dc)] = bk
                            AT = aT[bi % 2]
                            for dc in range(2):
                                SG = sg[dc]
                                P.op("act", lambda E, SG=SG, bk=gb[(0, dc)], N=N: E.activation(out=SG.t[:, 0:N], in_=bk.t[:, 0:N], func=AF.Silu), reads=[gb[(0, dc)].res], writes=[SG.res])
                                P.op("dve", lambda E, SG=SG, AT=AT, bk=gb[(1, dc)], N=N, dc=dc: E.tensor_tensor(out=AT.t[:, dc, 0:N], in0=bk.t[:, 0:N], in1=SG.t[:, 0:N], op=ALU.mult),
                                     reads=[gb[(1, dc)].res, SG.res], awrites=[AT.res])
                            for j in range(nt):
                                jt = a + j
                                for nb in range(4):
                                    bk = pp.next()

                                    def fd(E, bk=bk, AT=AT, W2=W2, j=j, nb=nb):
                                        E.matmul(bk.t[:], lhsT=AT.t[:, 0, j * 128:(j + 1) * 128], rhs=W2.t[:, 0, nb * 512:(nb + 1) * 512], start=True, stop=False)
                                        return E.matmul(bk.t[:], lhsT=AT.t[:, 1, j * 128:(j + 1) * 128], rhs=W2.t[:, 1, nb * 512:(nb + 1) * 512], start=False, stop=True)
                                    P.op("pe", fd, reads=[AT.res, W2.res], writes=[bk.res])
                                    P.op("dve", lambda E, bk=bk, jt=jt, nb=nb, e=e, acc=acc, cmb=cmb: E.scalar_tensor_tensor(
                                        out=acc.t[:, jt, nb * 512:(nb + 1) * 512], in0=bk.t[:], scalar=cmb.t[:, jt, e:e + 1], in1=acc.t[:, jt, nb * 512:(nb + 1) * 512],
                                        op0=ALU.mult, op1=ALU.add), reads=[bk.res, cmb.res], awrites=[acc.res])
                for j, t in enumerate(grp):
                    r = 0 if t < NXT else 1
                    P.dma("sp", P.gs(6), x1.t[:], x1buf[t * 128:(t + 1) * 128, :], reads=[R["x1buf"]], writes=[x1.res])
                    P.op("dve", lambda E, j=j, r=r, acc=acc, gt2=gt2: E.tensor_tensor(out=acc.t[:, j, :], in0=acc.t[:, j, :], in1=gt2[r].t[:], op=ALU.mult),
                         reads=[acc.res, gt2[r].res], awrites=[acc.res])
                    P.op("pool", lambda E, j=j, acc=acc, x1=x1: E.tensor_tensor(out=acc.t[:, j, :], in0=acc.t[:, j, :], in1=x1.t[:], op=ALU.add),
                         reads=[acc.res, x1.res], awrites=[acc.res])
                    if t < NXT:
                        dst, dres = (xbuf, R["xbuf"]) if l < DEPTH - 1 else (y_out, R["y"])
                        dst = dst[t * 128:(t + 1) * 128, :]
                    else:
                        dst, dres = cbuf[(t - NXT) * 128:(t - NXT + 1) * 128, :], R["cbuf"]
                    P.dma("sp", P.gs(7 + j), dst, acc.t[:, j, :], reads=[acc.res], awrites=[dres])
                P.barrier()
                P.flush()

    cx.phaseD = phaseD
    cx.phaseE = phaseE


    def phaseSel(l):
        with ExitStack() as ps:
            hm = sb(ps, nc, "hm", [128, 2], F32)
            ma = sbn(ps, nc, "selma", [128, D], BF16, 2)
            mb = sbn(ps, nc, "selmb", [128, D], BF16, 2)
            xa = sbn(ps, nc, "selxa", [128, D], F32, 2)
            xb = sbn(ps, nc, "selxb", [128, D], F32, 2)
            P.dma("sp", P.one(), hm.t[:], hfm, writes=[hm.res])
            H = NXT // 2
            xs_ = x_in if l == 0 else xbuf
            xres = [R["xbuf"]] if l else []
            for j in range(H):
                i = j % 2
                MA, MB, XA, XB = ma[i], mb[i], xa[i], xb[i]
                P.dma("sp", P.gs(0 + i), MA.t[:], mixT[j].rearrange("p a b -> p (a b)"), reads=[R["mixT"]], writes=[MA.res])
                P.dma("sp", P.gs(2 + i), MB.t[:], mixT[H + j].rearrange("p a b -> p (a b)"), reads=[R["mixT"]], writes=[MB.res])
                P.dma("sp", P.gs(4 + i), XA.t[:], xs_[j * 128:(j + 1) * 128, :], reads=xres, writes=[XA.res])
                P.dma("sp", P.gs(6 + i), XB.t[:], xs_[(H + j) * 128:(H + j + 1) * 128, :], reads=xres, writes=[XB.res])
                P.op("dve", lambda E, MA=MA: E.tensor_scalar(out=MA.t[:], in0=MA.t[:], scalar1=hm.t[:, 0:1], scalar2=None, op0=ALU.mult), reads=[MA.res, hm.res], writes=[MA.res])
                P.op("dve", lambda E, MA=MA, MB=MB: E.scalar_tensor_tensor(out=MA.t[:], in0=MB.t[:], scalar=hm.t[:, 1:2], in1=MA.t[:], op0=ALU.mult, op1=ALU.add),
                     reads=[MA.res, MB.res, hm.res], writes=[MA.res])
                P.dma("sp", P.gs(8 + i), mixS[j].rearrange("p a b -> p (a b)"), MA.t[:], reads=[MA.res], awrites=[R["mixS"]])
                P.op("dve", lambda E, XA=XA: E.tensor_scalar(out=XA.t[:], in0=XA.t[:], scalar1=hm.t[:, 0:1], scalar2=None, op0=ALU.mult), reads=[XA.res, hm.res], writes=[XA.res])
                P.op("dve", lambda E, XA=XA, XB=XB: E.scalar_tensor_tensor(out=XA.t[:], in0=XB.t[:], scalar=hm.t[:, 1:2], in1=XA.t[:], op0=ALU.mult, op1=ALU.add),
                     reads=[XA.res, XB.res, hm.res], writes=[XA.res])
                P.dma("sp", P.gs(10 + i), xS[j * 128:(j + 1) * 128, :], XA.t[:], reads=[XA.res], awrites=[R["xS"]])
            P.barrier()
            P.flush()

    cx.phaseSel = phaseSel


    def phaseBlend(jobs):
        with ExitStack() as ps:
            hm = sb(ps, nc, "bhm", [128, 2], F32)
            bufs = {BF16: (sbn(ps, nc, "blA16", [128, D], BF16, 2), sbn(ps, nc, "blB16", [128, D], BF16, 2)),
                    F32: (sbn(ps, nc, "blA32", [128, D], F32, 2), sbn(ps, nc, "blB32", [128, D], F32, 2))}
            P.dma("sp", P.one(), hm.t[:], hfm, writes=[hm.res])
            cnt = {BF16: 0, F32: 0}
            for (a_ap, b_ap, d_ap, n_, dt_, rres, wres) in jobs:
                i = cnt[dt_] % 2
                cnt[dt_] += 1
                k = 0 if dt_ == BF16 else 6
                A_, B_ = bufs[dt_][0][i], bufs[dt_][1][i]
                P.dma("sp", P.gs(k + i), A_.t[:, 0:n_], a_ap, reads=rres, writes=[A_.res])
                P.dma("sp", P.gs(k + 2 + i), B_.t[:, 0:n_], b_ap, reads=rres, writes=[B_.res])
                P.op("dve", lambda E, A_=A_, n_=n_: E.tensor_scalar(out=A_.t[:, 0:n_], in0=A_.t[:, 0:n_], scalar1=hm.t[:, 0:1], scalar2=None, op0=ALU.mult),
                     reads=[A_.res, hm.res], writes=[A_.res])
                P.op("dve", lambda E, A_=A_, B_=B_, n_=n_: E.scalar_tensor_tensor(out=A_.t[:, 0:n_], in0=B_.t[:, 0:n_], scalar=hm.t[:, 1:2], in1=A_.t[:, 0:n_], op0=ALU.mult, op1=ALU.add),
                     reads=[A_.res, B_.res, hm.res], writes=[A_.res])
                P.dma("sp", P.gs(k + 4 + i), d_ap, A_.t[:, 0:n_], reads=[A_.res], awrites=[wres])
            P.barrier()
            P.flush()

    def sel_jobs_q():
        H = NXT // 2
        fl = lambda ap: ap.rearrange("p a b -> p (a b)")
        jobs = []
        for j in range(H):
            jobs.append((fl(QnT[j]), fl(QnT[H + j]), fl(QnS[j]), 1024, BF16, [R["QnT"]], R["QnS"]))
            jobs.append((fl(QpeT[j]), fl(QpeT[H + j]), fl(QpS[j]), 512, BF16, [R["QpeT"]], R["QpS"]))
        return jobs

    def sel_jobs_mix(l):
        H = NXT // 2
        fl = lambda ap: ap.rearrange("p a b -> p (a b)")
        xs_ = x_in if l == 0 else xbuf
        xres = [R["xbuf"]] if l else []
        jobs = []
        for j in range(H):
            jobs.append((fl(mixT[j, :, 8:16, :]), fl(mixT[H + j, :, 8:16, :]), fl(mixS[j, :, 8:16, :]), 1024, BF16, [R["mixT"]], R["mixS"]))
            jobs.append((xs_[j * 128:(j + 1) * 128, :], xs_[(H + j) * 128:(H + j + 1) * 128, :], xS[j * 128:(j + 1) * 128, :], D, F32, xres, R["xS"]))
        return jobs

    cx.phaseBlend = phaseBlend
    cx.sel_jobs_q = sel_jobs_q
    cx.sel_jobs_mix = sel_jobs_mix

    def phase_copy_out():
        with ExitStack() as ps:
            xt = sbn(ps, nc, "cpx", [128, D], F32, 2)
            ls = [P.gs(20), P.gs(21), P.gs(0), P.gs(1)]
            for t in range(NXT // 2):
                X = xt[t % 2]
                P.dma("sp", ls[t % 2], X.t[:], x_in[t * 128:(t + 1) * 128, :], writes=[X.res])
                P.dma("sp", ls[2 + t % 2], y_out[t * 128:(t + 1) * 128, :], X.t[:], reads=[X.res], awrites=[R["y"]])
            P.barrier()
            P.flush()

    cx.phase_copy_out = phase_copy_out
    cx.phase0 = phase0
    cx.phaseA1 = phaseA1
    cx.P = P
    cx.nc = nc
    cx.gs = gs
    cx.R = R
    cx.y_out = y_out
    return cx


def finish(cx):
    P = cx.P
    P.barrier()
    P.flush()
    cx.gs.close()
    P.es.close()
    return cx.nc


IN_OFF = dict(cq=0, ckv=512, kpe=1024, zu=1088, zv=1600, gq=2112, gk=2368, gv=2624, gr=3136, ggf=3648, ggb=3664)


def _rope_tables():
    rows = SEQ // 64
    row = np.repeat(np.arange(rows), 64).astype(np.float32)
    col = np.tile(np.arange(64), rows).astype(np.float32)
    inv = (10000.0 ** (-np.arange(16, dtype=np.float32) / 16)).astype(np.float32)
    ar = row[:, None] * inv
    ac = col[:, None] * inv
    cos64 = np.concatenate([np.cos(ar), np.cos(ar), np.cos(ac), np.cos(ac)], axis=1)
    sin64 = np.concatenate([-np.sin(ar), np.sin(ar), -np.sin(ac), np.sin(ac)], axis=1)
    cos64 = np.concatenate([cos64, np.ones((CTX, 64), np.float32)], axis=0).astype(np.float32)
    sin64 = np.concatenate([sin64, np.zeros((CTX, 64), np.float32)], axis=0).astype(np.float32)
    tabk = np.concatenate([cos64, sin64], axis=1).reshape(NT, 128, 128)
    tabq = np.concatenate([np.tile(cos64, (1, 8)), np.tile(sin64, (1, 8))], axis=1).reshape(NT, 128, 1024)
    return np.ascontiguousarray(tabk), np.ascontiguousarray(tabq)


def _swap64():
    idx = np.arange(64)
    blk = idx // 32
    j = idx % 32
    return blk * 32 + (j + 16) % 32


def prep_shared(inp):
    sw = _swap64()
    o = IN_OFF
    w_in = inp["w_in"]
    colk = np.concatenate([np.arange(o["ckv"], o["ckv"] + 512), np.arange(o["kpe"], o["kpe"] + 64), o["kpe"] + sw,
                           np.arange(o["gk"], o["gk"] + 256), np.arange(o["ggf"], o["ggf"] + 16), np.arange(o["ggb"], o["ggb"] + 16),
                           np.zeros(96, np.int64), np.arange(o["gv"], o["gv"] + 512)])
    colq = np.concatenate([np.arange(o["cq"], o["cq"] + 512), np.arange(o["gq"], o["gq"] + 256), np.arange(o["gr"], o["gr"] + 512),
                           np.arange(o["zv"], o["zv"] + 512), np.arange(o["zu"], o["zu"] + 512)])
    sh = {}
    sh["wk"] = np.ascontiguousarray(w_in[:, :, colk])
    sh["wq"] = np.ascontiguousarray(w_in[:, :, colq])
    ukv = inp["mla_w_ukv"].reshape(DEPTH, 512, 8, 2, 128)
    sh["wukv"] = np.ascontiguousarray(np.concatenate([ukv[:, :, :, 0, :].reshape(DEPTH, 512, 1024), ukv[:, :, :, 1, :].reshape(DEPTH, 512, 1024)], axis=2))
    uq = inp["mla_w_uq"].reshape(DEPTH, 512, 8, 192)
    sh["wuq"] = np.ascontiguousarray(np.concatenate([uq[..., :128].reshape(DEPTH, 512, 1024), uq[..., 128:].reshape(DEPTH, 512, 512),
                                                     uq[..., 128:][..., sw].reshape(DEPTH, 512, 512)], axis=2))
    sh["qnorm"] = inp["mla_q_norm"]
    sh["kvnorm"] = inp["mla_kv_norm"]
    qg, kg = inp["mla_q_gain"], inp["mla_k_gain"]
    sh["qgn"] = np.ascontiguousarray(qg[:, :128, None])
    sh["kgn"] = np.ascontiguousarray(kg[:, :128, None])
    sh["qgpe"] = np.ascontiguousarray(np.concatenate([np.tile(qg[:, 128:], (1, 8)), np.tile(qg[:, 128:][:, sw], (1, 8))], axis=1))
    sh["kgpe"] = np.ascontiguousarray(np.concatenate([kg[:, 128:], kg[:, 128:][:, sw]], axis=1))
    sh["sgun"] = np.ascontiguousarray(inp["sgu_norm"].reshape(DEPTH, 512))
    sh["wsT"] = np.ascontiguousarray(inp["sgu_w"].transpose(0, 3, 1, 2))
    sh["sgub"] = np.ascontiguousarray(inp["sgu_b"].reshape(DEPTH, 512))
    sh["wgF"] = np.ascontiguousarray(np.concatenate([inp["gla_wg_f"], inp["gla_bg_f"][:, None, :]], axis=1))
    sh["wgB"] = np.ascontiguousarray(np.concatenate([inp["gla_wg_b"], inp["gla_bg_b"][:, None, :]], axis=1))
    sh["glaon"] = np.ascontiguousarray(np.tile(inp["gla_out_norm"], (1, 4)))
    sh["w_out"] = inp["w_out"]
    sh["wr"] = np.ascontiguousarray(np.concatenate([inp["moe_w_group"], inp["moe_w_expert"]], axis=2))
    sh["br"] = np.ascontiguousarray(np.concatenate([inp["moe_b_group"], inp["moe_b_expert"]], axis=1))
    sh["w1"], sh["w3"], sh["w2"] = inp["moe_w1"], inp["moe_w3"], inp["moe_w2"]
    sh["ada_w"], sh["ada_b"] = inp["ada_w"], inp["ada_b"]
    sh["g1"], sh["g2"] = inp["norm1_g"], inp["norm2_g"]
    sh["tabk"], sh["tabq"] = _rope_tables()
    sh["ident"] = np.eye(128, dtype=np.float32)
    s_, t_ = np.meshgrid(np.arange(128), np.arange(128), indexing="ij")
    same = (s_ // 64) == (t_ // 64)
    triF = np.where(same & (s_ <= t_), -1.0 / 16, 0.0)
    triB = np.where(same & (s_ >= t_), -1.0 / 16, 0.0)
    ones2 = np.where(same, -1.0 / 16, 0.0)
    cind = np.stack([np.where(np.arange(128) // 64 == c, -1.0 / 16, 0.0) for c in range(2)], axis=1)
    cm = np.stack([(np.arange(128) // 64 == c).astype(np.float32) for c in range(2)], axis=1)
    sl_ = np.arange(128)[:, None]
    tl_ = np.arange(64)[None, :]
    masks = []
    for dF in (True, False):
        for c in range(2):
            inc = (sl_ // 64 == c) & (((sl_ % 64) <= tl_) if dF else ((sl_ % 64) >= tl_))
            masks.append(np.tile(inc.astype(np.float32), (1, 4)))
    sh["gcon"] = np.ascontiguousarray(np.concatenate([triF, triB, ones2, cind, cm] + masks, axis=1).astype(np.float32))
    return sh


def core_inputs(inp, sh, b):
    m = dict(sh)
    m["x"] = np.ascontiguousarray(inp["x"][b])
    m["ctx"] = np.ascontiguousarray(inp["ctx"][b])
    cv = np.stack([inp["c"][b], inp["c_ctx"]], axis=0)
    m["cT"] = np.ascontiguousarray(cv.reshape(2, 16, 128).transpose(2, 1, 0))
    return m


def build_full():
    cx = build()
    xt = list(range(NXT))
    allt = list(range(NT))
    for l in range(DEPTH):
        last = l == DEPTH - 1
        full = xt if last else allt
        cx.phase0(l)
        cx.phaseA1(l, allt)
        cx.phaseA2(l, full)
        if last:
            cx.phaseBlend(cx.sel_jobs_q())
            cx.phaseB(l, False, qsel=True)
        else:
            cx.phaseB(l, True)
        cx.phaseC(l, not last)
        if last:
            half = list(range(NXT // 2))
            cx.phaseBlend(cx.sel_jobs_mix(l))
            cx.phaseD(l, half, sel=True)
            cx.phaseE(l, half)
        else:
            cx.phaseD(l, full)
            cx.phaseE(l, full)
    return finish(cx)


def kernel(**inputs):
    inp = {k: np.asarray(v) for k, v in inputs.items()}
    sh = prep_shared(inp)
    nc = build_full()
    in_maps = []
    for c in range(NCORES):
        m = core_inputs(inp, sh, c // 2)
        hf = float(c % 2)
        m["hfm"] = np.ascontiguousarray(np.tile(np.array([[1.0 - hf, hf]], np.float32), (128, 1)))
        in_maps.append(m)
    res = run_bass_kernel_spmd(nc, in_maps, core_ids=list(range(NCORES)))
    B = NCORES // 2
    out = np.empty((B, SEQ, D), np.float32)
    for c in range(NCORES):
        hf = c % 2
        out[c // 2, hf * (SEQ // 2):(hf + 1) * (SEQ // 2)] = np.asarray(res.results[c]["y"], dtype=np.float32)
    return out
```
